# Optimizing a Trainium2 kernel written in Bass

```python
import jax, jax.numpy as jnp
from jax import lax
import numpy as np

D_MODEL = 1024
BATCH = 16
SEQ = 2048
DEPTH = 2

GRID_W = 64
CTX_LEN = 256
HEAD_DIM = 64
N_HEADS_NA = 8
D_NA = N_HEADS_NA * HEAD_DIM
N_HEADS_RW = 8
D_RW = N_HEADS_RW * HEAD_DIM
D_MIX = D_NA + D_RW
WIN_ROWS = 8
WIN_COLS = 16
N_DIR = 2
DECAY_LORA = 32
ICLR_LORA = 32
GATE_LORA = 96
D_RW_IN = 3 * D_RW + N_DIR * (DECAY_LORA + ICLR_LORA) + GATE_LORA
RW_SPLITS = (D_RW, 2 * D_RW, 3 * D_RW, 3 * D_RW + N_DIR * DECAY_LORA, 3 * D_RW + N_DIR * (DECAY_LORA + ICLR_LORA))
D_IN = 3 * D_NA + D_RW_IN
N_EXPERTS = 32
N_GROUPS = 8
EXPERTS_PER_GROUP = N_EXPERTS // N_GROUPS
TOP_K = 2
D_EXPERT = 512
MOE_BLOCK = 128
ROPE_BASE = 10000.0
LN_EPS = 1e-6
GN_EPS = 64e-5
ALPHA = (2 * DEPTH) ** 0.25
BETA = (8 * DEPTH) ** -0.25

kernel_name = 'hybrid_na_rwkv7_moe_diffusion_trunk'


def layer_norm(z, g, b):
    zf = z.astype(jnp.float32)
    mu = jnp.mean(zf, -1, keepdims=True)
    var = jnp.mean(jnp.square(zf - mu), -1, keepdims=True)
    return ((zf - mu) * lax.rsqrt(var + LN_EPS) * g + b).astype(z.dtype)


def modulate(z, shift, scale):
    return z * (1 + scale) + shift


def axial_rope_tables(seq_len):
    t = jnp.arange(seq_len)
    row = (t // GRID_W).astype(jnp.float32)
    col = (t % GRID_W).astype(jnp.float32)
    n_freq = HEAD_DIM // 4
    inv = ROPE_BASE ** (-jnp.arange(n_freq, dtype=jnp.float32) / n_freq)
    ar = row[:, None] * inv
    ac = col[:, None] * inv
    ang = jnp.concatenate([ar, ar, ac, ac], -1)
    return jnp.cos(ang)[:, None, :], jnp.sin(ang)[:, None, :]


def apply_axial_rope(z, cos, sin):
    a, b, cc, d = jnp.split(z, 4, axis=-1)
    rot = jnp.concatenate([-b, a, -d, cc], -1)
    return z * cos + rot * sin


def token_shift(z, mu_prev, mu_next):
    prev = jnp.pad(z[:, :-1], ((0, 0), (1, 0), (0, 0)))
    nxt = jnp.pad(z[:, 1:], ((0, 0), (0, 1), (0, 0)))
    return z + mu_prev * (prev - z) + mu_next * (nxt - z)


def wkv7_scan(s0, r, decay, k, v, kk, a, reverse):
    def step(s, inp):
        r_t, w_t, k_t, v_t, kk_t, a_t = inp
        s_kk = jnp.einsum('bhvk,bhk->bhv', s, kk_t)
        s = s * w_t[:, :, None, :] - s_kk[..., None] * (kk_t * a_t)[:, :, None, :] + v_t[..., None] * k_t[:, :, None, :]
        return s, jnp.einsum('bhvk,bhk->bhv', s, r_t)
    xs = tuple(jnp.moveaxis(z, 1, 0) for z in (r, decay, k, v, kk, a))
    s_fin, ys = lax.scan(step, s0, xs, reverse=reverse)
    return jnp.moveaxis(ys, 0, 1), s_fin


def rwkv7_bidir(p, rope, init_states, mu_prev, mu_next, w0, w2, a0, a2, g2, k_k, k_a, r_k, gn_g, gn_b):
    B, T = p.shape[0], p.shape[1]
    H, N = N_HEADS_RW, HEAD_DIM
    z = token_shift(p.astype(jnp.float32), mu_prev, mu_next)
    r, k, v, lw, la, lg = jnp.split(z, RW_SPLITS, axis=-1)
    r = r.reshape(B, T, H, N)
    k = k.reshape(B, T, H, N)
    v = v.reshape(B, T, H, N)
    if rope is not None:
        r = apply_axial_rope(r, rope[0], rope[1])
        k = apply_axial_rope(k, rope[0], rope[1])
    kk = k * k_k.reshape(H, N)
    kk = kk / jnp.maximum(jnp.sqrt(jnp.sum(kk * kk, -1, keepdims=True)), 1e-12)
    lw = lw.reshape(B, T, N_DIR, DECAY_LORA)
    la = la.reshape(B, T, N_DIR, ICLR_LORA)
    w_log = -jax.nn.softplus(-(w0 + jnp.einsum('btdr,drc->btdc', jnp.tanh(lw), w2))) - 0.5
    decay = jnp.exp(-jnp.exp(w_log)).reshape(B, T, N_DIR, H, N)
    a = jax.nn.sigmoid(a0 + jnp.einsum('btdr,drc->btdc', la, a2)).reshape(B, T, N_DIR, H, N)
    k_dir = k[:, :, None] * (1 + (a - 1) * k_a.reshape(H, N))
    g = jax.nn.sigmoid(lg) @ g2
    outs = []
    states = []
    for d in range(N_DIR):
        s0 = jnp.zeros((B, H, N, N), jnp.float32) if init_states is None else init_states[d]
        y_d, s_d = wkv7_scan(s0, r, decay[:, :, d], k_dir[:, :, d], v, kk, a[:, :, d], reverse=(d == 1))
        outs.append(y_d)
        states.append(s_d)
    y = outs[0] + outs[1]
    mu = jnp.mean(y, -1, keepdims=True)
    var = jnp.mean(jnp.square(y - mu), -1, keepdims=True)
    y = (y - mu) * lax.rsqrt(var + GN_EPS) * gn_g.reshape(H, N) + gn_b.reshape(H, N)
    bonus = jnp.sum(r[:, :, None] * k_dir * r_k, -1, keepdims=True) * v[:, :, None]
    y = y + jnp.sum(bonus, axis=2)
    out = y.reshape(B, T, D_RW) * g
    return out.astype(p.dtype), (states[0], states[1])


def neighbourhood_attention(q, k, v, k_ctx, v_ctx, rpb):
    B, S = q.shape[0], q.shape[1]
    rows = S // GRID_W
    kr = min(WIN_ROWS, rows)

    def grid(z):
        return z.reshape(B, rows, GRID_W, N_HEADS_NA, HEAD_DIM).transpose(1, 0, 3, 2, 4)

    qg, kg, vg = grid(q), grid(k), grid(v)
    cols = jnp.arange(GRID_W)
    col_start = jnp.clip(cols - WIN_COLS // 2, 0, GRID_W - WIN_COLS)
    col_idx = col_start[:, None] + jnp.arange(WIN_COLS)[None, :]
    col_off = col_idx - cols[:, None] + (WIN_COLS - 1)
    rpb_c = rpb[:, :, col_off]

    def one_row(i):
        rs = jnp.clip(i - kr // 2, 0, rows - kr)
        q_i = qg[i]
        k_win = lax.dynamic_slice_in_dim(kg, rs, kr, axis=0)[:, :, :, col_idx]
        v_win = lax.dynamic_slice_in_dim(vg, rs, kr, axis=0)[:, :, :, col_idx]
        row_off = rs + jnp.arange(kr) - i + (WIN_ROWS - 1)
        bias = rpb_c[:, row_off].transpose(0, 2, 1, 3)
        s_loc = jnp.einsum('bhqd,rbhqcd->bhqrc', q_i, k_win).astype(jnp.float32) + bias.astype(jnp.float32)
        s_ctx = jnp.einsum('bhqd,bnhd->bhqn', q_i, k_ctx).astype(jnp.float32)
        s = jnp.concatenate([s_loc.reshape(B, N_HEADS_NA, GRID_W, kr * WIN_COLS), s_ctx], -1)
        pr = jax.nn.softmax(s, axis=-1).astype(v.dtype)
        p_loc = pr[..., :kr * WIN_COLS].reshape(B, N_HEADS_NA, GRID_W, kr, WIN_COLS)
        p_ctx = pr[..., kr * WIN_COLS:]
        return jnp.einsum('bhqrc,rbhqcd->bhqd', p_loc, v_win) + jnp.einsum('bhqn,bnhd->bhqd', p_ctx, v_ctx)

    out = lax.map(one_row, jnp.arange(rows))
    return out.transpose(1, 0, 3, 2, 4).reshape(B, S, D_NA)


def context_attention(q, k, v):
    B, C = q.shape[0], q.shape[1]
    s = jnp.einsum('bqhd,bkhd->bhqk', q, k).astype(jnp.float32)
    pr = jax.nn.softmax(s, axis=-1).astype(v.dtype)
    return jnp.einsum('bhqk,bkhd->bqhd', pr, v).reshape(B, C, D_NA)


def moe_grouped_top2(h, router_w, router_bias, w1, w3, w2):
    N, D = h.shape
    scores = jax.nn.sigmoid(jnp.dot(h, router_w).astype(jnp.float32))
    sel = (scores + router_bias.astype(jnp.float32)).reshape(N, N_GROUPS, EXPERTS_PER_GROUP)
    group_score = jnp.sum(lax.top_k(sel, TOP_K)[0], -1)
    g_idx = jnp.argmax(group_score, -1)
    sel_in_group = jnp.take_along_axis(sel, g_idx[:, None, None], axis=1)[:, 0]
    _, local = lax.top_k(sel_in_group, TOP_K)
    e_idx = (g_idx[:, None] * EXPERTS_PER_GROUP + local).astype(jnp.int32)
    gate = jnp.take_along_axis(scores, e_idx, -1)
    gate = gate / jnp.sum(gate, -1, keepdims=True)
    flat_e = e_idx.reshape(-1)
    nk = flat_e.shape[0]
    order = jnp.argsort(flat_e)
    sorted_e = flat_e[order]
    counts = jnp.bincount(flat_e, length=N_EXPERTS)
    padded = (counts + MOE_BLOCK - 1) // MOE_BLOCK * MOE_BLOCK
    pad_end = jnp.cumsum(padded)
    pad_start = pad_end - padded
    seg_start = jnp.cumsum(counts) - counts
    dest_sorted = (pad_start[sorted_e] + jnp.arange(nk) - seg_start[sorted_e]).astype(jnp.int32)
    dest = jnp.zeros((nk,), jnp.int32).at[order].set(dest_sorted)
    n_blocks = -(-nk // MOE_BLOCK) + N_EXPERTS
    buf_len = n_blocks * MOE_BLOCK
    src_tok = jnp.full((buf_len,), N, jnp.int32).at[dest].set(jnp.arange(nk, dtype=jnp.int32) // TOP_K)
    h_pad = jnp.concatenate([h, jnp.zeros((1, D), h.dtype)], 0)
    xb = h_pad[src_tok].reshape(n_blocks, MOE_BLOCK, D)
    block_expert = jnp.minimum(jnp.searchsorted(pad_end, jnp.arange(n_blocks) * MOE_BLOCK, side='right'), N_EXPERTS - 1)

    def expert_block(args):
        xblk, e = args
        return (jax.nn.silu(xblk @ w1[e]) * (xblk @ w3[e])) @ w2[e]

    yb = lax.map(expert_block, (xb, block_expert)).reshape(buf_len, D)
    y_assign = yb[dest].reshape(N, TOP_K, D)
    return jnp.einsum('nk,nkd->nd', gate.astype(h.dtype), y_assign)


def setup_inputs(seed: int = 0) -> dict:
    key = jax.random.key(seed)
    ks = jax.random.split(key, 32)
    f32 = jnp.float32
    L = DEPTH

    def nrm(k, shape, s):
        return jax.random.normal(k, shape, f32) * s

    return {
        'x': nrm(ks[0], (BATCH, SEQ, D_MODEL), 1.0),
        'c': nrm(ks[1], (BATCH, D_MODEL), 1.0),
        'ctx': nrm(ks[2], (BATCH, CTX_LEN, D_MODEL), 1.0),
        'c_ctx': nrm(ks[3], (D_MODEL,), 1.0),
        'ada_w': nrm(ks[4], (L, D_MODEL, 6 * D_MODEL), 0.5 * D_MODEL ** -0.5),
        'ada_b': nrm(ks[5], (L, 6 * D_MODEL), 0.02),
        'w_in': nrm(ks[6], (L, D_MODEL, D_IN), D_MODEL ** -0.5),
        'na_rpb': nrm(ks[7], (L, N_HEADS_NA, 2 * WIN_ROWS - 1, 2 * WIN_COLS - 1), 0.1),
        'rw_mu_prev': jax.random.uniform(ks[8], (L, D_RW_IN), f32, 0.0, 0.5),
        'rw_mu_next': jax.random.uniform(ks[9], (L, D_RW_IN), f32, 0.0, 0.5),
        'rw_w0': jax.random.uniform(ks[10], (L, N_DIR, D_RW), f32, -5.0, 0.0),
        'rw_w2': nrm(ks[11], (L, N_DIR, DECAY_LORA, D_RW), 0.1 * DECAY_LORA ** -0.5),
        'rw_a0': nrm(ks[12], (L, N_DIR, D_RW), 0.1),
        'rw_a2': nrm(ks[13], (L, N_DIR, ICLR_LORA, D_RW), 0.5 * ICLR_LORA ** -0.5),
        'rw_g2': nrm(ks[14], (L, GATE_LORA, D_RW), GATE_LORA ** -0.5),
        'rw_k_k': 0.85 + nrm(ks[15], (L, D_RW), 0.05),
        'rw_k_a': 1.0 + nrm(ks[16], (L, D_RW), 0.05),
        'rw_r_k': nrm(ks[17], (L, N_HEADS_RW, HEAD_DIM), 0.1),
        'rw_gn_g': 1.0 + nrm(ks[18], (L, D_RW), 0.05),
        'rw_gn_b': nrm(ks[19], (L, D_RW), 0.02),
        'w_out': nrm(ks[20], (L, D_MIX, D_MODEL), BETA * D_MIX ** -0.5),
        'ln1_g': 1.0 + nrm(ks[21], (L, D_MODEL), 0.05),
        'ln1_b': nrm(ks[22], (L, D_MODEL), 0.02),
        'ln2_g': 1.0 + nrm(ks[23], (L, D_MODEL), 0.05),
        'ln2_b': nrm(ks[24], (L, D_MODEL), 0.02),
        'router_w': nrm(ks[25], (D_MODEL, N_EXPERTS), D_MODEL ** -0.5),
        'router_bias': nrm(ks[26], (N_EXPERTS,), 0.01),
        'exp_w1': nrm(ks[27], (L, N_EXPERTS, D_MODEL, D_EXPERT), D_MODEL ** -0.5),
        'exp_w3': nrm(ks[28], (L, N_EXPERTS, D_MODEL, D_EXPERT), D_MODEL ** -0.5),
        'exp_w2': nrm(ks[29], (L, N_EXPERTS, D_EXPERT, D_MODEL), BETA * D_EXPERT ** -0.5),
    }


def reference(x, c, ctx, c_ctx, ada_w, ada_b, w_in, na_rpb, rw_mu_prev, rw_mu_next, rw_w0, rw_w2, rw_a0, rw_a2, rw_g2, rw_k_k, rw_k_a, rw_r_k, rw_gn_g, rw_gn_b, w_out, ln1_g, ln1_b, ln2_g, ln2_b, router_w, router_bias, exp_w1, exp_w3, exp_w2):
    B, S, D = x.shape
    C = ctx.shape[1]
    rope = axial_rope_tables(S)
    q_scale = HEAD_DIM ** -0.5
    for l in range(DEPTH):
        last = l == DEPTH - 1
        mod_x = jnp.split((jax.nn.silu(c) @ ada_w[l] + ada_b[l])[:, None, :], 6, axis=-1)
        mod_c = jnp.split((jax.nn.silu(c_ctx) @ ada_w[l] + ada_b[l])[None, None, :], 6, axis=-1)
        px = modulate(x, mod_x[0], mod_x[1]) @ w_in[l]
        pc = modulate(ctx, mod_c[0], mod_c[1]) @ w_in[l]
        qx, kx, vx = (px[..., i * D_NA:(i + 1) * D_NA].reshape(B, S, N_HEADS_NA, HEAD_DIM) for i in range(3))
        qc, kc, vc = (pc[..., i * D_NA:(i + 1) * D_NA].reshape(B, C, N_HEADS_NA, HEAD_DIM) for i in range(3))
        rw_args = dict(mu_prev=rw_mu_prev[l], mu_next=rw_mu_next[l], w0=rw_w0[l], w2=rw_w2[l], a0=rw_a0[l], a2=rw_a2[l], g2=rw_g2[l], k_k=rw_k_k[l], k_a=rw_k_a[l], r_k=rw_r_k[l], gn_g=rw_gn_g[l], gn_b=rw_gn_b[l])
        rw_c, ctx_states = rwkv7_bidir(pc[..., 3 * D_NA:], None, None, **rw_args)
        rw_x, _ = rwkv7_bidir(px[..., 3 * D_NA:], rope, ctx_states, **rw_args)
        na_x = neighbourhood_attention(qx * q_scale, kx, vx, kc, vc, na_rpb[l])
        o_x = jnp.concatenate([na_x, rw_x], -1) @ w_out[l]
        x = layer_norm(ALPHA * x + mod_x[2] * o_x, ln1_g[l], ln1_b[l])
        hx = modulate(x, mod_x[3], mod_x[4])
        if last:
            y_x = moe_grouped_top2(hx.reshape(B * S, D), router_w, router_bias, exp_w1[l], exp_w3[l], exp_w2[l]).reshape(B, S, D)
        else:
            na_c = context_attention(qc * q_scale, kc, vc)
            o_c = jnp.concatenate([na_c, rw_c], -1) @ w_out[l]
            ctx = layer_norm(ALPHA * ctx + mod_c[2] * o_c, ln1_g[l], ln1_b[l])
            hc = modulate(ctx, mod_c[3], mod_c[4])
            y = moe_grouped_top2(jnp.concatenate([hx.reshape(B * S, D), hc.reshape(B * C, D)], 0), router_w, router_bias, exp_w1[l], exp_w3[l], exp_w2[l])
            y_x = y[:B * S].reshape(B, S, D)
            y_c = y[B * S:].reshape(B, C, D)
            ctx = layer_norm(ALPHA * ctx + mod_c[5] * y_c, ln2_g[l], ln2_b[l])
        x = layer_norm(ALPHA * x + mod_x[5] * y_x, ln2_g[l], ln2_b[l])
    return x
```

```python
from contextlib import ExitStack
import numpy as np
import concourse.bass as bass
import concourse.mybir as mybir
from concourse.bass_utils import run_bass_kernel_spmd

F32 = mybir.dt.float32
BF16 = mybir.dt.bfloat16
AF = mybir.ActivationFunctionType
ALU = mybir.AluOpType
AX = mybir.AxisListType

L = 2
D = 1024
S = 2048
CT = 256
TT = S + CT
NTT = TT // 128
DIN = 3296
LAM = float(np.exp(-0.5))
ALPHA = float((2 * L) ** 0.25)
LN_EPS = 1e-6
GN_EPS = 64e-5
NV = 70
DEBUG = False
INV_F32R = True
SPARSE_MOE = True
BS = 512
NBLK = (2 * 2 * TT + BS - 1) // BS + 32
CAST_IDMA = True
PRECONV = True
I32 = mybir.dt.int32
F32R = mybir.dt.float32r


class Buf:
    __slots__ = ("name", "w", "r")

    def __init__(self, name):
        self.name = name
        self.w = {}
        self.r = []


class T:
    __slots__ = ("ap", "buf")

    def __init__(self, ap, buf):
        self.ap = ap
        self.buf = buf

    def __getitem__(self, idx):
        return T(self.ap[idx], self.buf)

    def re(self, pat, **kw):
        return T(self.ap.rearrange(pat, **kw), self.buf)

    def bc(self, shape):
        return T(self.ap.to_broadcast(shape), self.buf)

    def pbc(self, n):
        return T(self.ap.partition_broadcast(n), self.buf)

    def cast(self, dt):
        return T(self.ap.bitcast(dt), self.buf)


class EM:
    NDS = 12

    def __init__(self, nc):
        self.nc = nc
        self.eng = {"pe": nc.tensor, "dve": nc.vector, "act": nc.scalar, "pool": nc.gpsimd, "sp": nc.sync}
        self.sem = {}
        self.cnt = {}
        self.waited = {e: {} for e in self.eng}
        for e in ("pe", "dve", "act", "pool"):
            self.sem[e] = nc.alloc_semaphore("s_" + e)
            self.cnt[e] = 0
        self.dsem = {}
        self.dcnt = {}
        for q in ("sp", "pool"):
            self.dsem[q] = [nc.alloc_semaphore(f"d_{q}{i}") for i in range(self.NDS)]
            self.dcnt[q] = 0
        self.ntens = 0
        self.ninst = 0
        self.stack = None

    def sb(self, shape, dt=F32, name=None):
        self.ntens += 1
        name = (name or "t") + f"_{self.ntens}"
        h = self.stack.enter_context(self.nc.sbuf_tensor(name, list(shape), dt))
        return T(h.ap(), Buf(name))

    def dram(self, name, shape, dt=F32, kind="Internal"):
        if DEBUG and kind == "Internal":
            kind = "ExternalOutput"
        h = self.nc.dram_tensor(name, list(shape), dt, kind=kind)
        return T(h.ap(), Buf(name))

    def _wait(self, e, tok):
        key = id(tok[0])
        if self.waited[e].get(key, 0) >= tok[1]:
            return
        self.waited[e][key] = tok[1]
        self.eng[e].wait_ge(tok[0], tok[1])
        self.ninst += 1

    def _deps(self, e, reads, writes, is_dma=False):
        for t in reads:
            for tok in t.buf.w.values():
                if e == "pe" and tok[2] == "pe":
                    continue
                self._wait(e, tok)
        for t in writes:
            b = t.buf
            for tok in b.w.values():
                if is_dma and tok[2] == "dma":
                    continue
                if (not is_dma) and tok[2] == e:
                    continue
                self._wait(e, tok)
            for tok in b.r:
                if (not is_dma) and tok[2] == e and e in ("pe", "dve", "act", "pool"):
                    continue
                self._wait(e, tok)

    def _done(self, tok, reads, writes, is_dma=False):
        wb = [t.buf for t in writes]
        for b in wb:
            if is_dma:
                b.w[id(tok[0])] = tok
            else:
                b.w = {id(tok[0]): tok}
            b.r = []
        for t in reads:
            b = t.buf
            if any(b is x for x in wb):
                continue
            b.r.append(tok)
            if len(b.r) > 16:
                best = {}
                for k in b.r:
                    kk = id(k[0])
                    if kk not in best or best[kk][1] < k[1]:
                        best[kk] = k
                b.r = list(best.values())

    def op(self, e, fn, reads=(), writes=()):
        reads = [t for t in reads if isinstance(t, T)]
        writes = [t for t in writes if isinstance(t, T)]
        self._deps(e, reads, writes)
        inst = fn(self.eng[e])
        self.cnt[e] += 1
        inst.then_inc(self.sem[e], 1)
        tok = (self.sem[e], self.cnt[e], e)
        self._done(tok, reads, writes)
        self.ninst += 1
        return tok

    def dma(self, out, in_, q="sp"):
        i = self.dcnt[q]
        self.dcnt[q] += 1
        s = self.dsem[q][i % self.NDS]
        val = 16 * (i // self.NDS + 1)
        if val > 16:
            self._wait(q, (s, val - 16, "dma"))
        self._deps(q, [in_], [out], True)
        self.eng[q].dma_start(out=out.ap, in_=in_.ap).then_inc(s, 16)
        tok = (s, val, "dma")
        self._done(tok, [in_], [out], True)
        self.ninst += 1
        return tok

    def idma(self, out, in_, idx, scatter, bound):
        q = "pool"
        i = self.dcnt[q]
        self.dcnt[q] += 1
        s = self.dsem[q][i % self.NDS]
        val = 16 * (i // self.NDS + 1)
        if val > 16:
            self._wait(q, (s, val - 16, "dma"))
        self._deps(q, [in_, idx], [out], True)
        off = bass.IndirectOffsetOnAxis(ap=idx.ap, axis=0)
        if scatter:
            inst = self.nc.gpsimd.indirect_dma_start(out=out.ap, out_offset=off, in_=in_.ap, in_offset=None)
        else:
            inst = self.nc.gpsimd.indirect_dma_start(out=out.ap, out_offset=None, in_=in_.ap, in_offset=off)
        inst.then_inc(s, 16)
        tok = (s, val, "dma")
        self._done(tok, [in_, idx], [out], True)
        self.ninst += 1
        return tok

    def barrier(self):
        toks = []
        for e in ("pe", "dve", "act", "pool"):
            if self.cnt[e] > 0:
                toks.append((self.sem[e], self.cnt[e], e))
        for q in self.dsem:
            n = self.dcnt[q]
            for j in range(min(n, self.NDS)):
                last = ((n - 1 - j) // self.NDS) if (n - 1 - j) >= 0 else -1
            for si in range(self.NDS):
                k = (n - si + self.NDS - 1) // self.NDS if n > si else 0
                if k > 0:
                    toks.append((self.dsem[q][si], 16 * k, "dma"))
        for e in self.eng:
            for tok in toks:
                if tok[2] == e and e in ("pe", "dve", "act"):
                    continue
                self._wait(e, tok)

    def mm(self, out, lhsT, rhs, start=True, stop=True):
        return self.op("pe", lambda g: g.matmul(out.ap, lhsT.ap, rhs.ap, start=start, stop=stop, skip_group_check=True),
                       [lhsT, rhs], [out])

    def tr(self, out, in_, ident):
        return self.op("pe", lambda g: g.transpose(out.ap, in_.ap, ident.ap), [in_, ident], [out])

    def act(self, out, in_, func, bias=None, scale=None):
        kw = {}
        rd = [in_]
        if bias is not None:
            kw["bias"] = bias.ap if isinstance(bias, T) else bias
            rd.append(bias)
        if scale is not None:
            kw["scale"] = scale.ap if isinstance(scale, T) else scale
            rd.append(scale)
        return self.op("act", lambda g: g.activation(out.ap, in_.ap, func, **kw), rd, [out])

    def tt(self, out, a, b, op, e="dve"):
        return self.op(e, lambda g: g.tensor_tensor(out.ap, a.ap, b.ap, op), [a, b], [out])

    def ts(self, out, a, s1, s2=None, op0=ALU.mult, op1=None, e="dve"):
        rd = [a, s1, s2]
        A1 = s1.ap if isinstance(s1, T) else s1
        A2 = s2.ap if isinstance(s2, T) else s2
        if op1 is None:
            return self.op(e, lambda g: g.tensor_scalar(out.ap, a.ap, A1, None, op0), rd, [out])
        return self.op(e, lambda g: g.tensor_scalar(out.ap, a.ap, A1, A2, op0, op1), rd, [out])

    def stt(self, out, a, s, b, op0, op1):
        rd = [a, b, s]
        Sx = s.ap if isinstance(s, T) else s
        return self.op("dve", lambda g: g.scalar_tensor_tensor(out.ap, a.ap, Sx, b.ap, op0, op1), rd, [out])

    def copy(self, out, in_, e="dve"):
        if e == "act":
            return self.op(e, lambda g: g.copy(out.ap, in_.ap), [in_], [out])
        return self.op(e, lambda g: g.tensor_copy(out.ap, in_.ap), [in_], [out])

    def memset(self, out, val, e="dve"):
        return self.op(e, lambda g: g.memset(out.ap, val), [], [out])

    def recip(self, out, in_):
        return self.op("dve", lambda g: g.reciprocal(out.ap, in_.ap), [in_], [out])

    def scan(self, out, d0, d1, init, op0, op1):
        return self.op("dve", lambda g: g.tensor_tensor_scan(out.ap, d0.ap, d1.ap, init, op0, op1), [d0, d1], [out])

    def reduce(self, out, in_, op, axis=AX.X):
        return self.op("dve", lambda g: g.tensor_reduce(out.ap, in_.ap, axis, op), [in_], [out])

    def bn_stats(self, out, in_):
        return self.op("dve", lambda g: g.bn_stats(out.ap, in_.ap), [in_], [out])

    def bn_aggr(self, out, in_):
        return self.op("dve", lambda g: g.bn_aggr(out.ap, in_.ap), [in_], [out])


def _na_variants():
    types = {0: [0, 1, 2, 3], 1: [-1, 0, 1, 2], 2: [-2, -1, 0, 1, 2], 3: [-2, -1, 0, 1], 4: [-3, -2, -1, 0]}
    rep = {0: 0, 1: 1, 2: 5, 3: 14, 4: 15}
    var = []
    for ty in range(5):
        for dc in types[ty]:
            var.append((ty, dc))
    return types, rep, var


def _qtype(qc):
    return {0: 0, 1: 1, 14: 3, 15: 4}.get(qc, 2)


def _consts_np():
    c = {}
    idx = np.arange(128)
    c["ident"] = np.eye(128, dtype=np.float32)
    blk = (idx[:, None] // 64 == idx[None, :] // 64).astype(np.float32)
    c["blockones"] = blk
    rm = np.zeros((128, 128), np.float32)
    for h in range(2):
        for m in range(64):
            q = m // 16
            if q in (0, 2):
                rm[h * 64 + m + 16, h * 64 + m] = -1.0
            else:
                rm[h * 64 + m - 16, h * 64 + m] = 1.0
    c["rmat"] = rm
    LT = (idx[:, None] < idx[None, :]).astype(np.float32)
    LE = (idx[:, None] <= idx[None, :]).astype(np.float32)
    GT = (idx[:, None] > idx[None, :]).astype(np.float32)
    GE = (idx[:, None] >= idx[None, :]).astype(np.float32)
    c["maskA"] = np.stack([np.concatenate([LT, LE], 1), np.concatenate([GT, GE], 1)], 1).astype(np.float32)
    c["maskN"] = np.stack([GT, GT, LT, LT], 1).astype(np.float32)
    t = np.arange(S)
    row = (t // 64).astype(np.float32)
    col = (t % 64).astype(np.float32)
    inv = (10000.0 ** (-np.arange(16, dtype=np.float32) / 16)).astype(np.float32)
    ar = row[:, None] * inv
    ac = col[:, None] * inv
    ang = np.concatenate([ar, ar, ac, ac], -1).astype(np.float32)
    cs = np.cos(ang).astype(np.float32).T
    sn = np.sin(ang).astype(np.float32).T
    c["cos"] = np.ascontiguousarray(np.concatenate([cs, cs], 0))
    c["sin"] = np.ascontiguousarray(np.concatenate([sn, sn], 0))
    types, rep, var = _na_variants()
    mk = np.zeros((128, len(var), 128), np.float32)
    for v, (ty, dc) in enumerate(var):
        qc = rep[ty]
        for kr in range(2):
            krow = 2 * (qc + dc) + kr
            for qr in range(2):
                i = 2 * qc + qr
                rs = min(max(i - 4, 0), 24)
                if not (rs <= krow < rs + 8):
                    continue
                for qcol in range(64):
                    cst = min(max(qcol - 8, 0), 48)
                    mk[kr * 64 + cst: kr * 64 + cst + 16, v, qr * 64 + qcol] = 1.0
    c["namask"] = mk
    c["thr"] = np.tile((float(BS) * np.arange(128, dtype=np.float32))[None, :], (128, 1))
    c["kp"] = np.tile(np.arange(128, dtype=np.float32)[:, None], (1, 8))
    return c


def _rb_gather(rpb):
    kr = np.arange(128)[:, None] // 64
    kc = np.arange(128)[:, None] % 64
    qr = np.arange(128)[None, :] // 64
    qc = np.arange(128)[None, :] % 64
    out = np.zeros((L, 7, 128, 8, 128), np.float32)
    ci = np.clip(kc - qc + 15, 0, 30)
    for d in range(7):
        ri = np.clip(2 * (d - 3) + kr - qr + 7, 0, 14)
        g = rpb[:, :, ri, ci]
        out[:, d] = np.transpose(g, (0, 2, 1, 3))
    return out


def _vecs_np(inp):
    v = np.zeros((L, 128, NV), np.float32)
    groups = [(g * 128, 128) for g in range(12)] + [(1536, 32), (1568, 32), (1600, 32), (1632, 32), (1664, 96)]
    for l in range(L):
        mp = inp["rw_mu_prev"][l]
        mn = inp["rw_mu_next"][l]
        for g, (a, n) in enumerate(groups):
            v[l, :n, g] = mp[a:a + n]
            v[l, :n, 17 + g] = mn[a:a + n]
        for d in range(2):
            for j in range(4):
                v[l, :, 34 + d * 4 + j] = inp["rw_w0"][l, d, j * 128:(j + 1) * 128]
                v[l, :, 42 + d * 4 + j] = inp["rw_a0"][l, d, j * 128:(j + 1) * 128]
        rk = inp["rw_r_k"][l].reshape(512)
        for j in range(4):
            sl = slice(j * 128, (j + 1) * 128)
            v[l, :, 50 + j] = inp["rw_k_k"][l, sl]
            v[l, :, 54 + j] = inp["rw_k_a"][l, sl]
            v[l, :, 58 + j] = rk[sl]
            v[l, :, 62 + j] = inp["rw_gn_g"][l, sl]
            v[l, :, 66 + j] = inp["rw_gn_b"][l, sl]
    return v


def build(nlayers=L, phases=None, na_stage=9, rw_stage=9, moe_stage=9):
    nc = bass.Bass("TRN2", target_bir_lowering=False)
    em = EM(nc)
    types, rep, variants = _na_variants()
    NVAR = len(variants)

    def din(name, shape, dt=F32):
        return em.dram(name, shape, dt, kind="ExternalInput")

    x_in = din("x", [2, S, D])
    ctx_in = din("ctx", [2, CT, D])
    cT_in = din("cT", [128, 8, 3])
    ada_w = din("ada_w", [L, D, 6 * D])
    ada_b = din("ada_b", [L, 6 * D])
    w_in = din("w_in", [L, D, DIN])
    vecs = din("vecs", [L, 128, NV])
    rw_w2 = din("rw_w2", [L, 2, 32, 512])
    rw_a2 = din("rw_a2", [L, 2, 32, 512])
    rw_g2 = din("rw_g2", [L, 96, 512])
    gnrow = din("gnrow", [L, 2, 512])
    w_out = din("w_out", [L, D, D])
    lnv = din("lnv", [L, 4, D])
    router_w = din("router_w", [D, 32])
    rbias = din("router_bias", [32])
    ew1 = din("exp_w1", [L * 32 * 128, 8 * 512])
    ew3 = din("exp_w3", [L * 32 * 128, 8 * 512])
    ew2 = din("exp_w2", [L * 32 * 128, 4 * D])
    c_ident = din("ident", [128, 128])
    c_blk = din("blockones", [128, 128])
    c_rmat = din("rmat", [128, 128])
    c_maskA = din("maskA", [128, 2, 256])
    c_maskN = din("maskN", [128, 4, 128])
    c_cos = din("cos", [128, S])
    c_sin = din("sin", [128, S])
    c_namask = din("namask", [128, NVAR, 128])
    c_thr = din("thr", [128, 128])
    c_kp = din("kp", [128, 8])
    rb_in = din("rb", [L, 7, 128, 8, 128])
    y_out = em.dram("y", [2, S, D], F32, kind="ExternalOutput")

    modD = em.dram("modD", [L, 3, 6 * D])
    sD = em.dram("sD", [2, TT, D])
    x1D = em.dram("x1D", [2, TT, D])
    mixD = em.dram("mixD", [2, 128, 8, TT], BF16)
    h2D = em.dram("h2D", [2, 128, 8, TT], BF16)
    hTD = em.dram("hTD", [2, 128, 8, TT], BF16)
    h2tokD = em.dram("h2tokD", [2 * TT, D], BF16)
    xbD = em.dram("xbD", [NBLK * BS, D], BF16)
    ybD = em.dram("ybD", [NBLK * BS, D], F32)
    wb1D = em.dram("wb1D", [32 * 128, 8 * 512], BF16)
    wb3D = em.dram("wb3D", [32 * 128, 8 * 512], BF16)
    wb2D = em.dram("wb2D", [32 * 128, 4 * D], BF16)
    KRD = em.dram("KRD", [2, 2, 4, 128, NTT, 2, 128], BF16)
    BKD = em.dram("BKD", [2, 2, 4, 128, NTT, 2, 128], BF16)
    KHD = em.dram("KHD", [2, 2, 4, 128, NTT, 2, 128], BF16)
    VFD = em.dram("VFD", [2, 4, 128, TT], BF16)
    BOND = em.dram("BOND", [2, 4, 128, TT], F32)
    GCD = em.dram("GCD", [2, 2, 4, 128, NTT], F32)

    pst = nc.alloc_psum_tensor("psum", [128, 8, 512], F32)
    ps = [T(pst.ap()[:, i, :], Buf(f"ps{i}")) for i in range(8)]

    def seq_tile(l, b, tt):
        if l == 0:
            if tt < 2:
                return ctx_in[b, tt * 128:(tt + 1) * 128, :]
            return x_in[b, (tt - 2) * 128:(tt - 1) * 128, :]
        return sD[b, tt * 128:(tt + 1) * 128, :]

    def want(name):
        return phases is None or name in phases

    pstack = ExitStack()
    em.stack = pstack
    ident = em.sb([128, 128], F32, "ident")
    em.dma(ident, c_ident)
    identb = em.sb([128, 128], BF16, "identb")
    em.copy(identb, ident)
    G_all2 = em.sb([128, 2, NTT, 32], F32, "G_all")

    def bcast_row(dst, row_ap_T):
        em.dma(dst, row_ap_T.pbc(128))

    def adaln(l):
        with ExitStack() as st:
            em.stack = st
            cTs = em.sb([128, 8, 3])
            em.dma(cTs, cT_in)
            sc = em.sb([128, 8, 3])
            em.act(sc, cTs, AF.Silu)
            adab = em.sb([3, 6 * D])
            em.dma(adab, ada_b[l].pbc(3))
            modsb = em.sb([3, 6 * D])
            wbuf = [em.sb([128, 8, 512]) for _ in range(2)]
            awv = ada_w[l].re("(k p) n -> p k n", p=128)
            for blk in range(12):
                wb = wbuf[blk % 2]
                em.dma(wb, awv[:, :, blk * 512:(blk + 1) * 512])
                pp = ps[blk % 2][0:3, :]
                for k in range(8):
                    em.mm(pp, sc[:, k, :], wb[:, k, :], start=(k == 0), stop=(k == 7))
                em.tt(modsb[:, blk * 512:(blk + 1) * 512], pp, adab[:, blk * 512:(blk + 1) * 512], ALU.add)
            em.dma(modD[l], modsb)
            em.barrier()
        em.stack = pstack

    def make_hT(l, b, hT, sc1, sh1):
        xt = [em.sb([128, D], F32, "xt") for _ in range(2)]
        for tt in range(NTT):
            xx = xt[tt % 2]
            em.dma(xx, seq_tile(l, b, tt))
            w = 0 if tt < 2 else 1
            em.tt(xx, xx, sc1[:, w, :], ALU.mult)
            em.tt(xx, xx, sh1[:, w, :], ALU.add)
            for half in range(2):
                pp = ps[6 + half]
                for kk in range(4):
                    k = half * 4 + kk
                    em.tr(pp[:, kk * 128:(kk + 1) * 128], xx[:, k * 128:(k + 1) * 128], ident)
                dst = hT[:, half * 4:(half + 1) * 4, tt * 128:(tt + 1) * 128]
                src = pp.re("p (a b) -> p a b", a=4)
                if half == 0:
                    em.copy(dst, src, e="act")
                else:
                    em.copy(dst, src, e="dve")

    def load_mod1(l, b):
        sc1 = em.sb([128, 2, D], F32, "sc1")
        sh1 = em.sb([128, 2, D], F32, "sh1")
        for w, row in enumerate((2, b)):
            bcast_row(sh1[:, w, :], modD[l, row, 0:D])
            bcast_row(sc1[:, w, :], modD[l, row, D:2 * D])
        em.ts(sc1, sc1, 1.0, None, ALU.add)
        return sc1, sh1

    def na_phase(l):
        with ExitStack() as st:
            em.stack = st
            Tb = em.sb([128, NVAR, 2, 4, 128], BF16, "Tb")
            with ExitStack() as st2:
                em.stack = st2
                mk = em.sb([128, NVAR, 128], F32, "mk")
                em.dma(mk, c_namask)
                for d in range(7):
                    rbt = em.sb([128, 8, 128], F32, "rbt")
                    em.dma(rbt, rb_in[l, d])
                    em.act(rbt, rbt, AF.Exp)
                    for v, (ty, dc) in enumerate(variants):
                        if dc + 3 == d:
                            for par in range(2):
                                em.tt(Tb[:, v, par], rbt.re("p (a two) q -> p a two q", two=2)[:, :, par, :],
                                      mk[:, v:v + 1, :].bc([128, 4, 128]), ALU.mult)
                em.barrier()
            em.stack = st
            ones_b = em.sb([128, 64], BF16, "ones_b")
            em.memset(ones_b, 1.0)
            wq = em.sb([128, 8, 1536], BF16, "wq")
            em.dma(wq, w_in[l].re("(k p) n -> p k n", p=128)[:, :, 0:1536], q="pool")
            for b in range(2):
                with ExitStack() as st3:
                    em.stack = st3
                    qT = em.sb([128, 4, TT], BF16, "qT")
                    kT = em.sb([128, 4, TT], BF16, "kT")
                    Vt = em.sb([128, NTT, 512], BF16, "Vt")
                    with ExitStack() as st4:
                        em.stack = st4
                        sc1, sh1 = load_mod1(l, b)
                        hT = em.sb([128, 8, TT], BF16, "hT")
                        make_hT(l, b, hT, sc1, sh1)
                        em.dma(hTD[b], hT)
                        cnt = 0
                        for ci in range(8):
                            dst = qT if ci < 4 else kT
                            for t0 in range(0, TT, 512):
                                w = min(512, TT - t0)
                                pp = ps[cnt % 2]
                                for k in range(8):
                                    em.mm(pp[:, 0:w], wq[:, k, ci * 128:(ci + 1) * 128], hT[:, k, t0:t0 + w],
                                          start=(k == 0), stop=(k == 7))
                                em.copy(dst[:, ci % 4, t0:t0 + w], pp[:, 0:w], e=("act" if cnt % 2 else "dve"))
                                cnt += 1
                        for tt in range(NTT):
                            pp = ps[cnt % 2]
                            for k in range(8):
                                em.mm(pp, hT[:, k, tt * 128:(tt + 1) * 128], wq[:, k, 1024:1536],
                                      start=(k == 0), stop=(k == 7))
                            em.copy(Vt[:, tt, :], pp, e=("act" if cnt % 2 else "dve"))
                            cnt += 1
                        em.barrier()
                    em.stack = st3
                    nab = [em.sb([128, 4, 128], BF16, "nab") for _ in range(2)]
                    if na_stage < 2:
                        em.barrier()
                        continue
                    Eb = [em.sb([128, 4, 128], BF16, "Eb") for _ in range(3)]
                    rec = em.sb([128, 512], F32, "rec")
                    qlist = [(2 + qc, True) for qc in range(16)]
                    if l == 0:
                        qlist += [(0, False), (1, False)]
                    its = []
                    for qi, (qt, is_x) in enumerate(qlist):
                        if is_x:
                            qc = qt - 2
                            ty = _qtype(qc)
                            keys = [(2 + qc + dc, variants.index((ty, dc))) for dc in types[ty]] + [(0, None), (1, None)]
                        else:
                            keys = [(0, None), (1, None)]
                        for ki, (kt, v) in enumerate(keys):
                            for hg in range(2):
                                its.append((qi, qt, ki, kt, v, hg, len(keys)))
                    NIT = len(its)

                    def emit_S(n):
                        qi, qt, ki, kt, v, hg, nk = its[n]
                        sp_ = ps[n % 4]
                        rows = slice(hg * 64, hg * 64 + 64)
                        for hh in range(4):
                            em.mm(sp_[:, hh * 128:(hh + 1) * 128], kT[rows, hh, kt * 128:(kt + 1) * 128],
                                  qT[rows, hh, qt * 128:(qt + 1) * 128])

                    def emit_E(n):
                        qi, qt, ki, kt, v, hg, nk = its[n]
                        E = Eb[n % 3]
                        em.act(E, ps[n % 4].re("p (a b) -> p a b", a=4), AF.Exp, scale=0.125)
                        if v is not None:
                            em.tt(E, E, Tb[:, v, hg, :, :], ALU.mult)

                    def emit_PV(n):
                        qi, qt, ki, kt, v, hg, nk = its[n]
                        E = Eb[n % 3]
                        num = ps[4 + 2 * (qi % 2)]
                        den = ps[5 + 2 * (qi % 2)]
                        rows = slice(hg * 64, hg * 64 + 64)
                        for hh in range(4):
                            h = 2 * hh + hg
                            cs = slice(hh * 128, hh * 128 + 128)
                            first = (ki == 0 and hh == 0)
                            em.mm(num[rows, cs], Vt[:, kt, h * 64:(h + 1) * 64], E[:, hh, :], start=first, stop=(ki == nk - 1))
                        em.mm(den[rows, :], ones_b, E.re("p a b -> p (a b)"), start=(ki == 0), stop=(ki == nk - 1))
                        if ki == nk - 1 and hg == 1:
                            em.recip(rec, den)
                            nb_ = nab[qi % 2]
                            em.tt(nb_, num.re("p (a b) -> p a b", a=4), rec.re("p (a b) -> p a b", a=4), ALU.mult)
                            em.dma(mixD[b, :, 0:4, qt * 128:(qt + 1) * 128], nb_)

                    emit_S(0)
                    emit_S(1)
                    emit_E(0)
                    for n in range(NIT):
                        if n + 2 < NIT:
                            emit_S(n + 2)
                        if n + 1 < NIT:
                            emit_E(n + 1)
                        emit_PV(n)
                    em.barrier()
            em.stack = st
            em.barrier()
        em.stack = pstack

    SEGS = [(0, 256, False, 0, 0)] + [(256 + i * 512, 512, True, int(i > 0), int(i < 3)) for i in range(4)]
    REV_ORDER = [1, 0] + list(range(NTT - 1, 1, -1))

    def rwkv_phase(l):
        with ExitStack() as st:
            em.stack = st
            vc = em.sb([128, NV], F32, "vc")
            em.dma(vc, vecs[l])
            c0v = em.sb([128, 17], F32, "c0v")
            em.tt(c0v, vc[:, 0:17], vc[:, 17:34], ALU.add)
            em.ts(c0v, c0v, -1.0, 1.0, ALU.mult, ALU.add)
            omka = em.sb([128, 4], F32, "omka")
            em.ts(omka, vc[:, 54:58], -1.0, 1.0, ALU.mult, ALU.add)
            blk = em.sb([128, 128], F32, "blk")
            em.dma(blk, c_blk)
            rmat = em.sb([128, 128], F32, "rmat")
            em.dma(rmat, c_rmat)
            blkrk = em.sb([128, 4, 128], F32, "blkrk")
            for j in range(4):
                em.ts(blkrk[:, j, :], blk, vc[:, 58 + j:59 + j], None, ALU.mult)
            maskA = em.sb([128, 2, 256], BF16, "maskA")
            maskN = em.sb([128, 4, 128], BF16, "maskN")
            w2b = em.sb([32, 2, 512], BF16, "w2b")
            a2b = em.sb([32, 2, 512], BF16, "a2b")
            g2b = em.sb([96, 512], BF16, "g2b")
            em.dma(maskA, c_maskA, q="pool")
            em.dma(maskN, c_maskN, q="pool")
            em.dma(w2b, rw_w2[l].re("d r c -> r d c"), q="pool")
            em.dma(a2b, rw_a2[l].re("d r c -> r d c"), q="pool")
            em.dma(g2b, rw_g2[l], q="pool")
            sgs = [em.sb([96, TT], BF16, "sg") for _ in range(2)]
            maskA32 = em.sb([128, 2, 128], F32, "maskA32")
            maskN32 = em.sb([128, 4, 128], F32, "maskN32")
            em.copy(maskA32, maskA[:, :, 0:128])
            em.copy(maskN32, maskN)
            stw = ExitStack()
            em.stack = stw
            wr = em.sb([128, 8, 1760], BF16, "wr")
            wv_ = w_in[l].re("(k p) n -> p k n", p=128)
            em.dma(wr[:, :, 1536:1760], wv_[:, :, 3072:DIN], q="pool")
            for j_ in range(4):
                for part in range(3):
                    c_ = part * 512 + j_ * 128
                    em.dma(wr[:, :, c_:c_ + 128], wv_[:, :, 1536 + c_:1536 + c_ + 128], q="pool")
            if PRECONV:
                for src_, dst_ in ((ew1, wb1D), (ew3, wb3D), (ew2, wb2D)):
                    for c8 in range(8):
                        em.dma(dst_[c8 * 512:(c8 + 1) * 512, :], src_[l * 4096 + c8 * 512:l * 4096 + (c8 + 1) * 512, :], q="pool")
            for b in range(2):
                with ExitStack() as st2:
                    em.stack = st2
                    sg = sgs[b]
                    rstm = em.sb([128, 512], F32, "rstm")
                    em.memset(rstm, 1.0)
                    em.memset(rstm.re("p (n t) -> p n t", t=128)[:, :, 0:1], 0.0)
                    tl = em.sb([32, 2, TT], BF16, "tl")
                    la = em.sb([32, 2, TT], BF16, "la")
                    hT = em.sb([128, 8, TT], BF16, "hT")
                    if want("na"):
                        em.dma(hT, hTD[b])
                    else:
                        with ExitStack() as st5:
                            em.stack = st5
                            sc1, sh1 = load_mod1(l, b)
                            make_hT(l, b, hT, sc1, sh1)
                            em.barrier()
                        em.stack = st2
                    cosb = em.sb([128, 512], F32, "cosb")
                    sinb = em.sb([128, 512], F32, "sinb")

                    class Lane:
                        pass

                    def mk_lane(li):
                        ln = Lane()
                        ln.B = ps[4 * li:4 * li + 4]
                        ln.c0 = 0
                        ln.c1 = 0
                        ln.Pb = em.sb([128, 514], F32, "Pb")
                        ln.tmp = [em.sb([128, 512], F32, f"tmp{i}") for i in range(3)]
                        for nm in ("r_", "k_", "v_", "kk_", "lw_", "a_", "cum_", "kd_", "be_", "rks"):
                            setattr(ln, nm, em.sb([128, 512], F32, nm))
                        ln.ob = [em.sb([128, 512], BF16, f"ob{i}") for i in range(7)]
                        ln.gct = em.sb([128, 4], F32, "gct")
                        return ln

                    def bankA(ln):
                        ln.c0 += 1
                        return ln.B[ln.c0 % 2]

                    def bankB(ln):
                        ln.c1 += 1
                        return ln.B[2 + ln.c1 % 2]

                    def proj_shift(ln, dst, g, m, c0, seg):
                        t0, n, rope, hl, hr = seg
                        Pb = ln.Pb
                        ta = t0 - hl
                        tb = t0 + n + hr
                        if not hl:
                            em.memset(Pb[0:m, 0:1], 0.0)
                        if not hr:
                            em.memset(Pb[0:m, n + 1:n + 2], 0.0)
                        for s0 in range(ta, tb, 512):
                            w = min(512, tb - s0)
                            pp = bankA(ln)
                            for k in range(8):
                                em.mm(pp[0:m, 0:w], wr[:, k, c0:c0 + m], hT[:, k, s0:s0 + w], start=(k == 0), stop=(k == 7))
                            o = 1 - hl + (s0 - ta)
                            em.copy(Pb[0:m, o:o + w], pp[0:m, 0:w], e="act")
                        em.ts(dst, Pb[0:m, 1:n + 1], c0v[0:m, g:g + 1], None, ALU.mult)
                        em.stt(dst, Pb[0:m, 0:n], vc[0:m, g:g + 1], dst, ALU.mult, ALU.add)
                        em.stt(dst, Pb[0:m, 2:n + 2], vc[0:m, 17 + g:18 + g], dst, ALU.mult, ALU.add)

                    lanes = [mk_lane(0), mk_lane(1)]
                    for si, seg in enumerate(SEGS):
                        t0, n = seg[0], seg[1]
                        ln = lanes[si % 2]
                        t_ = ln.tmp[0]
                        for d in range(2):
                            proj_shift(ln, t_[0:32, 0:n], 12 + d, 32, 1536 + d * 32, seg)
                            em.act(tl[:, d, t0:t0 + n], t_[0:32, 0:n], AF.Tanh)
                            proj_shift(ln, t_[0:32, 0:n], 14 + d, 32, 1600 + d * 32, seg)
                            em.copy(la[:, d, t0:t0 + n], t_[0:32, 0:n])
                        proj_shift(ln, t_[0:96, 0:n], 16, 96, 1664, seg)
                        em.act(sg[:, t0:t0 + n], t_[0:96, 0:n], AF.Sigmoid)

                    def rope_apply(ln, z, n):
                        pp = bankB(ln)
                        em.mm(pp[:, 0:n], rmat, z[:, 0:n])
                        em.tt(ln.tmp[0][:, 0:n], pp[:, 0:n], sinb[:, 0:n], ALU.mult)
                        em.tt(z[:, 0:n], z[:, 0:n], cosb[:, 0:n], ALU.mult)
                        em.tt(z[:, 0:n], z[:, 0:n], ln.tmp[0][:, 0:n], ALU.add)

                    def pair_gen(ln, seg, j):
                        t0, n, rope, hl, hr = seg
                        nch = n // 128
                        n0 = t0 // 128
                        tmp = ln.tmp
                        ob = ln.ob
                        r = ln.r_[:, 0:n]
                        k = ln.k_[:, 0:n]
                        v = ln.v_[:, 0:n]
                        kk = ln.kk_[:, 0:n]
                        rks = ln.rks
                        proj_shift(ln, r, j, 128, j * 128, seg)
                        yield
                        proj_shift(ln, k, 4 + j, 128, 512 + j * 128, seg)
                        yield
                        proj_shift(ln, v, 8 + j, 128, 1024 + j * 128, seg)
                        yield
                        if rope:
                            rope_apply(ln, ln.r_, n)
                            yield
                            rope_apply(ln, ln.k_, n)
                            yield
                        em.copy(ob[6][:, 0:n], v)
                        em.dma(VFD[b, j, :, t0:t0 + n], ob[6][:, 0:n])
                        em.ts(kk, k, vc[:, 50 + j:51 + j], None, ALU.mult)
                        em.tt(tmp[0][:, 0:n], kk, kk, ALU.mult)
                        pp = bankB(ln)
                        em.mm(pp[:, 0:n], blk, tmp[0][:, 0:n])
                        yield
                        em.act(tmp[1][:, 0:n], pp[:, 0:n], AF.Sqrt)
                        yield
                        em.ts(tmp[1][:, 0:n], tmp[1][:, 0:n], 1e-12, None, ALU.max)
                        em.recip(tmp[1][:, 0:n], tmp[1][:, 0:n])
                        em.tt(kk, kk, tmp[1][:, 0:n], ALU.mult)
                        yield
                        for d in range(2):
                            lw = ln.lw_[:, 0:n]
                            a = ln.a_[:, 0:n]
                            cum = ln.cum_[:, 0:n]
                            kd = ln.kd_[:, 0:n]
                            be = ln.be_[:, 0:n]
                            pp = bankB(ln)
                            em.mm(pp[:, 0:n], w2b[:, d, j * 128:(j + 1) * 128], tl[:, d, t0:t0 + n])
                            pp2 = bankB(ln)
                            em.mm(pp2[:, 0:n], a2b[:, d, j * 128:(j + 1) * 128], la[:, d, t0:t0 + n])
                            yield
                            em.act(lw, pp[:, 0:n], AF.Sigmoid, bias=vc[:, 34 + d * 4 + j:35 + d * 4 + j])
                            em.act(a, pp2[:, 0:n], AF.Sigmoid, bias=vc[:, 42 + d * 4 + j:43 + d * 4 + j])
                            yield
                            em.scan(tmp[0][:, 0:n], rstm[:, 0:n], lw, 0.0, ALU.mult, ALU.add)
                            pre3 = tmp[0][:, 0:n].re("p (n t) -> p n t", t=128)
                            totb = pre3[:, :, 127:128].bc([128, nch, 128])
                            cum3 = cum.re("p (n t) -> p n t", t=128)
                            if d == 0:
                                em.copy(cum, tmp[0][:, 0:n])
                            else:
                                em.tt(cum3, totb, pre3, ALU.subtract)
                                em.tt(cum, cum, lw, ALU.add)
                            em.act(ln.gct[:, 0:nch], pre3[:, :, 127], AF.Exp, scale=-LAM)
                            em.dma(GCD[b, d, j, :, n0:n0 + nch], ln.gct[:, 0:nch])
                            em.ts(tmp[1][:, 0:n], a, vc[:, 54 + j:55 + j], omka[:, j:j + 1], ALU.mult, ALU.add)
                            em.tt(kd, tmp[1][:, 0:n], k, ALU.mult)
                            em.tt(be, kk, a, ALU.mult)
                            if d == 0:
                                em.tt(rks[:, 0:n], r, kd, ALU.mult)
                            else:
                                em.tt(tmp[1][:, 0:n], r, kd, ALU.mult)
                                em.tt(rks[:, 0:n], rks[:, 0:n], tmp[1][:, 0:n], ALU.add)
                            yield
                            e1 = tmp[1][:, 0:n]
                            e2 = tmp[2][:, 0:n]
                            em.act(e1, cum, AF.Exp, scale=-LAM)
                            em.tt(e2, cum, lw, ALU.subtract)
                            yield
                            em.tt(ob[1][:, 0:n], r, e1, ALU.mult)
                            em.act(e1, cum, AF.Exp, scale=LAM)
                            em.act(e2, e2, AF.Exp, scale=-LAM)
                            yield
                            em.tt(ob[3][:, 0:n], kd, e1, ALU.mult)
                            em.stt(ob[2][:, 0:n], be, -1.0, e1, ALU.mult, ALU.mult)
                            em.tt(ob[0][:, 0:n], kk, e2, ALU.mult)
                            em.tt(e2.re("p (n t) -> p n t", t=128), totb, cum3, ALU.subtract)
                            em.act(e2, e2, AF.Exp, scale=-LAM)
                            yield
                            em.tt(ob[4][:, 0:n], kd, e2, ALU.mult)
                            em.stt(ob[5][:, 0:n], be, -1.0, e2, ALU.mult, ALU.mult)
                            for idx, (dst, slot) in enumerate(((KRD[b], 0), (KRD[b], 1), (BKD[b], 0), (BKD[b], 1), (KHD[b], 0), (KHD[b], 1))):
                                em.dma(dst[d, j, :, n0:n0 + nch, slot, :], ob[idx][:, 0:n].re("p (n t) -> p n t", t=128))
                            yield
                        pp = bankB(ln)
                        em.mm(pp[:, 0:n], blkrk[:, j, :], rks[:, 0:n])
                        yield
                        em.tt(tmp[0][:, 0:n], pp[:, 0:n], v, ALU.mult)
                        em.dma(BOND[b, j, :, t0:t0 + n], tmp[0][:, 0:n])

                    for seg in SEGS:
                        t0, n, rope, hl, hr = seg
                        if rope:
                            em.dma(cosb[:, 0:n], c_cos[:, t0 - CT:t0 - CT + n])
                            em.dma(sinb[:, 0:n], c_sin[:, t0 - CT:t0 - CT + n])
                        for jj in range(0, 4, 2):
                            gens = [pair_gen(lanes[0], seg, jj), pair_gen(lanes[1], seg, jj + 1)]
                            alive = [True, True]
                            while any(alive):
                                for gi, g in enumerate(gens):
                                    if alive[gi]:
                                        try:
                                            next(g)
                                        except StopIteration:
                                            alive[gi] = False
                    em.barrier()
                em.stack = stw
            stw.close()
            em.stack = st
            def rr(t):
                return t.cast(F32R) if INV_F32R else t

            def scan_gen(b, j, B, gi):
                a_, c_ = B
                sg = sgs[b]
                KRt = [em.sb([64, 4, 256], BF16, "KRt") for _ in range(2)]
                BKt = [em.sb([64, 4, 256], BF16, "BKt") for _ in range(2)]
                KHt = [em.sb([128, 2, 256], BF16, "KHt") for _ in range(2)]
                Vf = [em.sb([128, 2, 128], BF16, "Vf") for _ in range(2)]
                gC = em.sb([64, 4, NTT], F32, "gC")
                for d in range(2):
                    em.dma(gC[:, d * 2:d * 2 + 2, :], GCD[b, d, j].re("(h c) n -> c h n", h=2))
                TM = em.sb([128, 6, 128], BF16, "TM")
                A1 = em.sb([128, 4, 256], BF16, "A1")
                A2 = em.sb([128, 4, 256], BF16, "A2")
                PY = em.sb([128, 4, 2, 128], F32, "PY")
                Pq = PY[:, :, 0, :]
                Yq = PY[:, :, 1, :]
                Qq = em.sb([128, 4, 128], F32, "Qq")
                TTb = em.sb([128, 4, 128], BF16, "TTb")
                RH = em.sb([128, 4, 64], BF16, "RH")
                Ub = em.sb([128, 4, 64], BF16, "Ub")
                Zf = em.sb([64, 4, 64], F32, "Zf")
                Zb = em.sb([64, 4, 64], BF16, "Zb")
                ztmp = em.sb([64, 4, 64], F32, "ztmp")
                ytm = em.sb([128, NTT, 128], F32, "ytm")
                em.memset(Zf, 0.0)
                em.memset(Zb, 0.0)
                em.memset(ytm, 0.0, e="pool")
                e0 = "dve"
                e1 = "act"
                v4 = lambda t: t.re("p (a b) -> p a b", a=4)
                v2 = lambda t: t.re("p (a b) -> p a b", a=2)

                def load_step(s):
                    sl = s % 2
                    for d in range(2):
                        n = s if d == 0 else REV_ORDER[s]
                        em.dma(KRt[sl][:, d * 2:d * 2 + 2, :], KRD[b, d, j, :, n].re("(h c) s t -> c h (s t)", h=2))
                        em.dma(BKt[sl][:, d * 2:d * 2 + 2, :], BKD[b, d, j, :, n].re("(h c) s t -> c h (s t)", h=2))
                        em.dma(KHt[sl][:, d, :], KHD[b, d, j, :, n].re("p s t -> p (s t)"))
                        em.dma(Vf[sl][:, d, :], VFD[b, j, :, n * 128:(n + 1) * 128])

                def mmA(bank, d, lo):
                    for hp in range(2):
                        u = d * 2 + hp
                        em.mm(bank[:, hp * 256:(hp + 1) * 256], BKt[sl][:, u, lo:lo + 128], KRt[sl][:, u, :])

                load_step(0)
                yield
                for s in range(NTT):
                    sl = s % 2
                    if s + 1 < NTT:
                        load_step(s + 1)
                    ns = [s, REV_ORDER[s]]
                    pt = a_.cast(BF16)
                    for d in range(2):
                        em.tr(pt[:, (2 * d) * 128:(2 * d + 1) * 128], KHt[sl][:, d, 0:128], identb)
                        em.tr(pt[:, (2 * d + 1) * 128:(2 * d + 2) * 128], KHt[sl][:, d, 128:256], identb)
                        em.tr(pt[:, (4 + d) * 128:(5 + d) * 128], Vf[sl][:, d, :], identb)
                    mmA(c_, 0, 0)
                    yield
                    em.copy(TM, pt[:, 0:768].re("p (a b) -> p a b", a=6), e="act")
                    em.tt(A1[:, 0:2, 128:256], v2(c_)[:, :, 128:256], maskA[:, 0:1, 128:256].bc([128, 2, 128]), ALU.mult)
                    em.tt(rr(Pq[:, 0:2, :]), v2(c_)[:, :, 0:128], maskA32[:, 0:1, :].bc([128, 2, 128]), ALU.mult)
                    mmA(a_, 1, 0)
                    for u in range(4):
                        em.mm(c_[:, u * 128:(u + 1) * 128], KRt[sl][:, u, 0:128], BKt[sl][:, u, 0:128])
                    yield
                    em.tt(A1[:, 2:4, 128:256], v2(a_)[:, :, 128:256], maskA[:, 1:2, 128:256].bc([128, 2, 128]), ALU.mult)
                    em.tt(rr(Pq[:, 2:4, :]), v2(a_)[:, :, 0:128], maskA32[:, 1:2, :].bc([128, 2, 128]), ALU.mult)
                    em.tt(rr(Qq), v4(c_), maskN32, ALU.mult)
                    em.tt(rr(Yq), Pq, T(ident.ap.unsqueeze(1).to_broadcast([128, 4, 128]), ident.buf), ALU.add)
                    mmA(a_, 0, 128)
                    mmA(c_, 1, 128)
                    yield
                    em.tt(A2[:, 0:2, :], v2(a_), maskA[:, 0:1, :].bc([128, 2, 256]), ALU.mult)
                    em.tt(A2[:, 2:4, :], v2(c_), maskA[:, 1:2, :].bc([128, 2, 256]), ALU.mult)
                    for u in range(4):
                        em.mm(a_[:, u * 128:(u + 1) * 128], rr(Qq[:, u, :]), rr(Pq[:, u, :]))
                    for u in range(4):
                        em.mm(c_[:, u * 128:(u + 1) * 128], rr(Pq[:, u, :]), rr(Qq[:, u, :]))
                    yield
                    em.copy(rr(Pq), v4(a_), e=e0)
                    em.copy(rr(Qq), v4(c_), e=e1)
                    for lev in range(1, 6):
                        for u in (0, 1):
                            em.mm(a_[:, u * 256:(u + 1) * 256], rr(Qq[:, u, :]), rr(PY[:, u, :, :]))
                        for u in range(4):
                            em.mm(c_[:, u * 128:(u + 1) * 128], rr(Pq[:, u, :]), rr(Qq[:, u, :]))
                        yield
                        a3 = a_.re("p (u s t) -> p u s t", u=2, s=2)
                        em.tt(rr(Yq[:, 0:2, :]), a3[:, :, 1, :], Yq[:, 0:2, :], ALU.add)
                        em.copy(rr(Pq[:, 0:2, :]), a3[:, :, 0, :], e=e0)
                        for u in (2, 3):
                            em.mm(a_[:, (u - 2) * 256:(u - 1) * 256], rr(Qq[:, u, :]), rr(PY[:, u, :, :]))
                        yield
                        em.tt(rr(Yq[:, 2:4, :]), a3[:, :, 1, :], Yq[:, 2:4, :], ALU.add)
                        em.copy(rr(Pq[:, 2:4, :]), a3[:, :, 0, :], e=e0)
                        em.copy(rr(Qq), v4(c_), e=e1)
                    for u in range(4):
                        em.mm(a_[:, u * 128:(u + 1) * 128], rr(Qq[:, u, :]), rr(Yq[:, u, :]))
                    yield
                    em.tt(rr(Yq), v4(a_), Yq, ALU.add)
                    em.copy(TTb, Yq, e="act")
                    for u in range(4):
                        d, hp = u // 2, u % 2
                        vs = TM[:, 4 + d, hp * 64:(hp + 1) * 64]
                        em.mm(c_[:, u * 64:(u + 1) * 64], KRt[sl][:, u, 0:128], Zb[:, u, :], start=True, stop=False)
                        em.mm(c_[:, u * 64:(u + 1) * 64], A2[:, u, 0:128], vs, start=False, stop=True)
                    yield
                    em.copy(RH, v4(c_[:, 0:256]), e="act")
                    for u in range(4):
                        em.mm(a_[:, u * 64:(u + 1) * 64], TTb[:, u, :], RH[:, u, :])
                    yield
                    em.copy(Ub, v4(a_[:, 0:256]), e="act")
                    for u in range(4):
                        d, hp = u // 2, u % 2
                        vs = TM[:, 4 + d, hp * 64:(hp + 1) * 64]
                        yo = c_[:, u * 64:(u + 1) * 64]
                        em.mm(yo, KRt[sl][:, u, 128:256], Zb[:, u, :], start=True, stop=False)
                        em.mm(yo, A2[:, u, 128:256], vs, start=False, stop=False)
                        em.mm(yo, A1[:, u, 128:256], Ub[:, u, :], start=False, stop=True)
                        zo = a_[0:64, 256 + u * 64:256 + (u + 1) * 64]
                        em.mm(zo, TM[:, 2 * d, hp * 64:(hp + 1) * 64], vs, start=True, stop=False)
                        em.mm(zo, TM[:, 2 * d + 1, hp * 64:(hp + 1) * 64], Ub[:, u, :], start=False, stop=True)
                    yield
                    for d in range(2):
                        n = ns[d]
                        em.tt(ytm[:, n, :], ytm[:, n, :], c_[:, d * 128:(d + 1) * 128], ALU.add)
                        em.tt(ztmp[:, d * 2:d * 2 + 2, :], Zf[:, d * 2:d * 2 + 2, :],
                              gC[:, d * 2:d * 2 + 2, n:n + 1].bc([64, 2, 64]), ALU.mult)
                    em.tt(Zf, ztmp, v4(a_[0:64, 256:512]), ALU.add)
                    em.copy(Zb, Zf, e="act")
                st_s = em.sb([128, NTT * 2], F32, "st_s")
                st_q = em.sb([128, NTT * 2], F32, "st_q")
                msq = em.sb([128, NTT * 2], F32, "msq")
                fin = em.sb([128, 512], F32, "fin")
                bon = em.sb([128, 512], F32, "bon")
                rwc = em.sb([128, 512], BF16, "rwc")
                y4 = ytm.re("p n (h v) -> p (n h) v", h=2)
                em.reduce(st_s, y4, ALU.add)
                for q4 in range(0, NTT, 4):
                    nt = min(4, NTT - q4)
                    f3 = fin[:, 0:nt * 128].re("p (n c) -> p n c", c=128)
                    em.tt(f3, ytm[:, q4:q4 + nt, :], ytm[:, q4:q4 + nt, :], ALU.mult)
                    em.reduce(st_q[:, q4 * 2:(q4 + nt) * 2], fin[:, 0:nt * 128].re("p (n v) -> p n v", v=64), ALU.add)
                yield
                em.ts(st_s, st_s, 1.0 / 64, None, ALU.mult)
                em.ts(st_q, st_q, 1.0 / 64, None, ALU.mult)
                em.tt(msq, st_s, st_s, ALU.mult)
                em.tt(st_q, st_q, msq, ALU.subtract)
                em.ts(st_q, st_q, GN_EPS, None, ALU.add)
                em.act(st_q, st_q, AF.Sqrt)
                em.recip(st_q, st_q)
                yield
                em.tt(y4, y4, T(st_s.ap.unsqueeze(2).to_broadcast([128, NTT * 2, 64]), st_s.buf), ALU.subtract)
                em.tt(y4, y4, T(st_q.ap.unsqueeze(2).to_broadcast([128, NTT * 2, 64]), st_q.buf), ALU.mult)
                yield
                for q4 in range(0, NTT, 4):
                    nt = min(4, NTT - q4)
                    w = nt * 128
                    tsl = slice(q4 * 128, q4 * 128 + w)
                    em.dma(bon[:, 0:w], BOND[b, j, :, tsl])
                    for i in range(nt):
                        em.tr(a_[:, i * 128:(i + 1) * 128], ytm[:, q4 + i, :], ident)
                    em.mm(c_[:, 0:w], g2b[:, j * 128:(j + 1) * 128], sg[:, tsl])
                    yield
                    em.ts(fin[:, 0:w], a_[:, 0:w], vc[:, 62 + j:63 + j], vc[:, 66 + j:67 + j], ALU.mult, ALU.add)
                    em.tt(fin[:, 0:w], fin[:, 0:w], bon[:, 0:w], ALU.add)
                    em.tt(rwc[:, 0:w], fin[:, 0:w], c_[:, 0:w], ALU.mult)
                    em.dma(mixD[b, :, 4 + j, tsl], rwc[:, 0:w])

            for jj in range(0, 4 if rw_stage >= 2 else 0, 2):
                with ExitStack() as st3:
                    em.stack = st3
                    gens = []
                    for gi, (b_, j_) in enumerate(((0, jj), (1, jj), (0, jj + 1), (1, jj + 1))):
                        gens.append(scan_gen(b_, j_, (ps[2 * gi], ps[2 * gi + 1]), gi))
                    alive = [True] * len(gens)
                    while any(alive):
                        for gi, g in enumerate(gens):
                            if alive[gi]:
                                try:
                                    next(g)
                                except StopIteration:
                                    alive[gi] = False
                    em.barrier()
                em.stack = st
            em.barrier()
        em.stack = pstack

    def post_phase(l, b):
        G_all = G_all2[:, b]
        with ExitStack() as st:
            em.stack = st
            ntiles = NTT if l == 0 else NTT
            t_lo = 0 if l == 0 else 2
            wo = em.sb([128, 8, D], BF16, "wo")
            em.dma(wo, w_out[l].re("(k p) n -> p k n", p=128), q="pool")
            rw32 = em.sb([128, 8, 32], F32, "rw32")
            em.dma(rw32, router_w.re("(k p) n -> p k n", p=128))
            rb = em.sb([128, 32], F32, "rb")
            bcast_row(rb, rbias)
            g1 = em.sb([128, 2, D], F32, "g1")
            sc2 = em.sb([128, 2, D], F32, "sc2")
            sh2 = em.sb([128, 2, D], F32, "sh2")
            for w, row in enumerate((2, b)):
                bcast_row(g1[:, w, :], modD[l, row, 2 * D:3 * D])
                bcast_row(sh2[:, w, :], modD[l, row, 3 * D:4 * D])
                bcast_row(sc2[:, w, :], modD[l, row, 4 * D:5 * D])
            em.ts(sc2, sc2, 1.0, None, ALU.add)
            lg = em.sb([128, D], F32, "lg")
            lb = em.sb([128, D], F32, "lb")
            bcast_row(lg, lnv[l, 0])
            bcast_row(lb, lnv[l, 1])
            PAIRS = [(0, 1), (0, 2), (0, 3), (1, 2), (1, 3), (2, 3)]
            NLANE = 4

            def lane_gen(li, B):
                m = em.sb([128, 8, 128], BF16, "mt")
                xx = em.sb([128, D], F32, "xt")
                u_ = em.sb([128, D], F32, "u_")
                h2 = em.sb([128, D], F32, "h2")
                h2f = em.sb([128, 8, 128], F32, "h2f")
                hb = em.sb([128, 8, 128], BF16, "h2b")
                h2t = em.sb([128, D], BF16, "h2t")
                stt_ = em.sb([128, 2, 6], F32, "bst")
                mv = em.sb([128, 2], F32, "mv")
                rstd = em.sb([128, 1], F32, "rstd")
                sc_ = em.sb([128, 32], F32, "sc_")
                sel = em.sb([128, 32], F32, "sel")
                p6 = em.sb([128, 8, 6], F32, "p6")
                m6 = em.sb([128, 8, 6], F32, "m6")
                gs = em.sb([128, 8], F32, "gs")
                sec = em.sb([128, 8], F32, "sec")
                gmx = em.sb([128, 1], F32, "gmx")
                gmk = em.sb([128, 8], F32, "gmk")
                emk = em.sb([128, 32], F32, "emk")
                gsum = em.sb([128, 1], F32, "gsum")
                for tt in range(t_lo + li, NTT, NLANE):
                    w = 0 if tt < 2 else 1
                    tsl = slice(tt * 128, (tt + 1) * 128)
                    em.dma(m, mixD[b, :, :, tsl])
                    em.dma(xx, seq_tile(l, b, tt))
                    for half in range(2):
                        for k in range(8):
                            em.mm(B[half], m[:, k, :], wo[:, k, half * 512:(half + 1) * 512], start=(k == 0), stop=(k == 7))
                    yield
                    for half in range(2):
                        em.tt(u_[:, half * 512:(half + 1) * 512], B[half], g1[:, w, half * 512:(half + 1) * 512], ALU.mult)
                    em.stt(u_, xx, ALPHA, u_, ALU.mult, ALU.add)
                    for half in range(2):
                        em.bn_stats(stt_[:, half, :], u_[:, half * 512:(half + 1) * 512])
                    em.bn_aggr(mv, stt_.re("p a b -> p (a b)"))
                    em.ts(rstd, mv[:, 1:2], LN_EPS, None, ALU.add)
                    em.act(rstd, rstd, AF.Sqrt)
                    yield
                    em.recip(rstd, rstd)
                    em.ts(u_, u_, mv[:, 0:1], rstd, ALU.subtract, ALU.mult)
                    em.tt(u_, u_, lg, ALU.mult)
                    em.tt(u_, u_, lb, ALU.add)
                    em.dma(x1D[b, tsl, :], u_)
                    em.tt(h2, u_, sc2[:, w, :], ALU.mult, e="pool")
                    em.tt(h2, h2, sh2[:, w, :], ALU.add, e="pool")
                    if SPARSE_MOE:
                        em.copy(h2t, h2, e="pool")
                        em.dma(h2tokD[b * TT + tt * 128:b * TT + (tt + 1) * 128, :], h2t)
                    yield
                    for half in range(2):
                        pp = B[half]
                        for kk in range(4):
                            k = half * 4 + kk
                            em.tr(pp[:, kk * 128:(kk + 1) * 128], h2[:, k * 128:(k + 1) * 128], ident)
                    yield
                    for half in range(2):
                        em.copy(h2f[:, half * 4:(half + 1) * 4, :], B[half].re("p (a b) -> p a b", a=4), e="act")
                    em.copy(hb, h2f, e="pool")
                    em.dma(h2D[b, :, :, tsl], hb)
                    lgt = B[0][:, 0:32]
                    for k in range(8):
                        em.mm(lgt, h2f[:, k, :], rw32[:, k, :], start=(k == 0), stop=(k == 7))
                    yield
                    em.act(sc_, lgt, AF.Sigmoid)
                    em.tt(sel, sc_, rb, ALU.add)
                    s3 = sel.re("p (g e) -> p g e", e=4)
                    for pi, (i0_, i1_) in enumerate(PAIRS):
                        em.tt(p6[:, :, pi], s3[:, :, i0_], s3[:, :, i1_], ALU.add)
                        em.tt(m6[:, :, pi], s3[:, :, i0_], s3[:, :, i1_], ALU.min)
                    em.reduce(gs, p6, ALU.max)
                    em.reduce(sec, m6, ALU.max)
                    em.reduce(gmx, gs, ALU.max)
                    em.ts(gmk, gs, gmx, None, ALU.is_ge)
                    e3 = emk.re("p (g e) -> p g e", e=4)
                    em.tt(e3, s3, T(sec.ap.unsqueeze(2).to_broadcast([128, 8, 4]), sec.buf), ALU.is_ge)
                    em.tt(e3, e3, T(gmk.ap.unsqueeze(2).to_broadcast([128, 8, 4]), gmk.buf), ALU.mult)
                    em.tt(emk, emk, sc_, ALU.mult)
                    em.reduce(gsum, emk, ALU.add)
                    em.recip(gsum, gsum)
                    em.ts(G_all[:, tt, :], emk, gsum, None, ALU.mult)
                    yield

            gens = [lane_gen(li, ps[2 * li:2 * li + 2]) for li in range(NLANE)]
            alive = [True] * NLANE
            for li in range(1, NLANE):
                for _ in range(2 * li):
                    pass
            while any(alive):
                for gi, g in enumerate(gens):
                    if alive[gi]:
                        try:
                            next(g)
                        except StopIteration:
                            alive[gi] = False
            em.barrier()
        em.stack = pstack

    def moe_phase(l, b):
        G_all = G_all2[:, b]
        with ExitStack() as st:
            em.stack = st
            t_lo = 0 if l == 0 else 2
            tok0 = t_lo * 128
            NTOK = TT - tok0
            hT2 = em.sb([128, 8, TT], BF16, "hT2")
            em.dma(hT2[:, :, tok0:], h2D[b, :, :, tok0:])
            yacc = em.sb([128, NTT, D], F32, "yacc")
            em.memset(yacc[:, :, 0:512], 0.0)
            em.memset(yacc[:, :, 512:1024], 0.0, e="pool")
            w1b = [em.sb([128, 8, 512], BF16, "w1b") for _ in range(2)]
            w3b = [em.sb([128, 8, 512], BF16, "w3b") for _ in range(2)]
            w2b_ = [em.sb([128, 4, D], BF16, "w2b_") for _ in range(2)]
            sil = [em.sb([128, 512], BF16, "sil") for _ in range(2)]
            actT = [em.sb([128, 4, 512], BF16, "actT") for _ in range(2)]

            def load_w(e):
                sl = e % 2
                em.dma(w1b[sl], ew1[l, e].re("(k p) n -> p k n", p=128), q="pool")
                em.dma(w3b[sl], ew3[l, e].re("(k p) n -> p k n", p=128), q="pool")
                em.dma(w2b_[sl], ew2[l, e].re("(k p) n -> p k n", p=128), q="pool")

            load_w(0)
            it = 0
            for e in range(32):
                sl = e % 2
                if e + 1 < 32:
                    load_w(e + 1)
                for t0 in range(tok0, TT, 512):
                    w = min(512, TT - t0)
                    aT = actT[it % 2]
                    for f in range(4):
                        pa = ps[(it * 4 + f) % 2]
                        pb_ = ps[2 + (it * 4 + f) % 2]
                        for k in range(8):
                            em.mm(pa[:, 0:w], w1b[sl][:, k, f * 128:(f + 1) * 128], hT2[:, k, t0:t0 + w], start=(k == 0), stop=(k == 7))
                        for k in range(8):
                            em.mm(pb_[:, 0:w], w3b[sl][:, k, f * 128:(f + 1) * 128], hT2[:, k, t0:t0 + w], start=(k == 0), stop=(k == 7))
                        sb_ = sil[f % 2]
                        em.act(sb_[:, 0:w], pa[:, 0:w], AF.Silu)
                        em.tt(aT[:, f, 0:w], sb_[:, 0:w], pb_[:, 0:w], ALU.mult)
                    for sub in range(w // 128):
                        tt = (t0 + sub * 128) // 128
                        for half in range(2):
                            po = ps[4 + (sub * 2 + half) % 4]
                            for f in range(4):
                                em.mm(po, aT[:, f, sub * 128:(sub + 1) * 128], w2b_[sl][:, f, half * 512:(half + 1) * 512],
                                      start=(f == 0), stop=(f == 3))
                            ya = yacc[:, tt, half * 512:(half + 1) * 512]
                            em.stt(ya, po, G_all[:, tt, e:e + 1], ya, ALU.mult, ALU.add)
                    it += 1
            g2r = em.sb([128, 2, D], F32, "g2r")
            for w, row in enumerate((2, b)):
                bcast_row(g2r[:, w, :], modD[l, row, 5 * D:6 * D])
            lg = em.sb([128, D], F32, "lg2")
            lb = em.sb([128, D], F32, "lb2")
            bcast_row(lg, lnv[l, 2])
            bcast_row(lb, lnv[l, 3])
            xt = [em.sb([128, D], F32, "xt2") for _ in range(2)]
            stt_ = em.sb([128, 2, 6], F32, "bst2")
            mv = em.sb([128, 2], F32, "mv2")
            rstd = em.sb([128, 1], F32, "rstd2")
            for tt in range(t_lo, NTT):
                w = 0 if tt < 2 else 1
                xx = xt[tt % 2]
                tsl = slice(tt * 128, (tt + 1) * 128)
                em.dma(xx, x1D[b, tsl, :])
                u_ = yacc[:, tt, :]
                em.tt(u_, u_, g2r[:, w, :], ALU.mult)
                em.stt(u_, xx, ALPHA, u_, ALU.mult, ALU.add)
                for half in range(2):
                    em.bn_stats(stt_[:, half, :], u_[:, half * 512:(half + 1) * 512])
                em.bn_aggr(mv, stt_.re("p a b -> p (a b)"))
                em.ts(rstd, mv[:, 1:2], LN_EPS, None, ALU.add)
                em.act(rstd, rstd, AF.Sqrt)
                em.recip(rstd, rstd)
                em.ts(u_, u_, mv[:, 0:1], rstd, ALU.subtract, ALU.mult)
                em.tt(u_, u_, lg, ALU.mult)
                em.tt(xx, u_, lb, ALU.add)
                if l == L - 1:
                    em.dma(y_out[b, (tt - 2) * 128:(tt - 1) * 128, :], xx)
                else:
                    em.dma(sD[b, tsl, :], xx)
            em.barrier()
        em.stack = pstack

    def moe_sparse(l, stage=9):
        NT = 2 * NTT
        with ExitStack() as st:
            em.stack = st
            Gf = G_all2.re("p b t e -> p (b t) e")
            ones32 = em.sb([128, 128], F32, "ones32")
            em.memset(ones32, 1.0)
            lt32 = em.sb([128, 128], F32, "lt32")
            em.dma(lt32, c_maskA[:, 0, 0:128])
            thr = em.sb([128, 128], F32, "thr")
            em.dma(thr, c_thr)
            kp = em.sb([128, 8], F32, "kp")
            em.dma(kp, c_kp)
            glo = em.sb([128, NT], F32, "glo")
            ghi = em.sb([128, NT], F32, "ghi")
            dli = em.sb([128, NT], I32, "dli")
            dhi_i = em.sb([128, NT], I32, "dhi_i")
            widx = em.sb([128, NBLK], I32, "widx")
            with ExitStack() as st2:
                em.stack = st2
                m = em.sb([128, NT, 32], F32, "m")
                em.ts(m, Gf, 0.0, None, ALU.is_gt)
                if l == 1:
                    for b in range(2):
                        em.memset(m[:, b * NTT:b * NTT + 2, :], 0.0)
                rank = em.sb([128, NT, 32], F32, "rank")
                cnt = em.sb([128, 32], F32, "cnt")
                em.memset(cnt, 0.0)
                for i in range(NT):
                    em.mm(ps[i // 16][:, (i % 16) * 32:(i % 16 + 1) * 32], lt32, m[:, i, :])
                    em.mm(ps[3 + i // 16][:, (i % 16) * 32:(i % 16 + 1) * 32], ones32, m[:, i, :])
                csum = em.sb([128, NT, 32], F32, "csum")
                for bk in range(3):
                    n_ = min(16, NT - bk * 16)
                    em.copy(rank[:, bk * 16:bk * 16 + n_, :], ps[bk][:, 0:n_ * 32].re("p (a b) -> p a b", b=32), e="act")
                    em.copy(csum[:, bk * 16:bk * 16 + n_, :], ps[3 + bk][:, 0:n_ * 32].re("p (a b) -> p a b", b=32), e="dve")
                for i in range(NT):
                    if i > 0:
                        em.tt(rank[:, i, :], rank[:, i, :], cnt, ALU.add)
                    em.tt(cnt, cnt, csum[:, i, :], ALU.add)
                cmp = em.sb([128, 32, 40], F32, "cmp")
                em.tt(cmp, T(cnt.ap.unsqueeze(2).to_broadcast([128, 32, 40]), cnt.buf),
                      T(thr.ap[:, 0:40].unsqueeze(1).to_broadcast([128, 32, 40]), thr.buf), ALU.is_gt)
                nblk = em.sb([128, 32], F32, "nblk")
                em.reduce(nblk, cmp, ALU.add)
                incl = em.sb([128, 32], F32, "incl")
                em.scan(incl, ones32[:, 0:32], nblk, 0.0, ALU.mult, ALU.add)
                pstart = em.sb([128, 32], F32, "pstart")
                pend = em.sb([128, 32], F32, "pend")
                em.tt(pstart, incl, nblk, ALU.subtract)
                em.ts(pstart, pstart, float(BS), None, ALU.mult)
                em.ts(pend, incl, float(BS), None, ALU.mult)
                dest = em.sb([128, NT, 32], F32, "dest")
                em.tt(dest, rank, T(pstart.ap.unsqueeze(1).to_broadcast([128, NT, 32]), pstart.buf), ALU.add)
                tmpm = em.sb([128, NT, 32], F32, "tmpm")
                dlo = em.sb([128, NT], F32, "dlo")
                dhi = em.sb([128, NT], F32, "dhi")
                em.ts(tmpm, m, -1.0e6, 1.0e6, ALU.mult, ALU.add)
                em.tt(tmpm, tmpm, dest, ALU.add)
                em.reduce(dlo, tmpm, ALU.min)
                em.tt(tmpm, dest, m, ALU.mult)
                em.tt(tmpm, tmpm, m, ALU.add)
                em.ts(tmpm, tmpm, -1.0, None, ALU.add)
                em.reduce(dhi, tmpm, ALU.max)
                em.tt(tmpm, dest, T(dlo.ap.unsqueeze(2).to_broadcast([128, NT, 32]), dlo.buf), ALU.is_equal)
                em.tt(tmpm, tmpm, Gf, ALU.mult)
                em.reduce(glo, tmpm, ALU.add)
                em.tt(tmpm, dest, T(dhi.ap.unsqueeze(2).to_broadcast([128, NT, 32]), dhi.buf), ALU.is_equal)
                em.tt(tmpm, tmpm, Gf, ALU.mult)
                em.reduce(ghi, tmpm, ALU.add)
                em.copy(dli, dlo)
                em.copy(dhi_i, dhi)
                cmpb = em.sb([128, NBLK, 32], F32, "cmpb")
                em.tt(cmpb, T(pend.ap.unsqueeze(1).to_broadcast([128, NBLK, 32]), pend.buf),
                      T(thr.ap[:, 0:NBLK].unsqueeze(2).to_broadcast([128, NBLK, 32]), thr.buf), ALU.is_le)
                be = em.sb([128, NBLK], F32, "be")
                em.reduce(be, cmpb, ALU.add)
                em.ts(be, be, 31.0, None, ALU.min)
                em.ts(be, be, 128.0, float(0 if PRECONV else l * 32 * 128), ALU.mult, ALU.add)
                em.tt(be, be, T(kp.ap[:, 0:1].to_broadcast([128, NBLK]), kp.buf), ALU.add)
                em.copy(widx, be)
                xtk = [em.sb([128, D], BF16, "xtk") for _ in range(2)]
                for i in range(NT):
                    if l == 1 and (i % NTT) < 2:
                        continue
                    xk = xtk[i % 2]
                    em.dma(xk, h2tokD[i * 128:(i + 1) * 128, :])
                    em.idma(xbD, xk, dli[:, i:i + 1], True, NBLK * BS - 1)
                    em.idma(xbD, xk, dhi_i[:, i:i + 1], True, NBLK * BS - 1)
                em.barrier()
            em.stack = st
            if stage < 2:
                return
            NS = BS // 128
            with ExitStack() as st3:
                em.stack = st3
                WD = BF16 if CAST_IDMA else F32
                w1s = [em.sb([128, 8, 512], WD, "w1s") for _ in range(2)]
                w3s = [em.sb([128, 8, 512], WD, "w3s") for _ in range(2)]
                w2s = [em.sb([128, 4, D], WD, "w2s") for _ in range(2)]
                if not CAST_IDMA:
                    w1c = em.sb([128, 8, 512], BF16, "w1b")
                    w3c = em.sb([128, 8, 512], BF16, "w3b")
                    w2c = em.sb([128, 4, D], BF16, "w2b_")
                xblk = [em.sb([128, NS, D], BF16, "xblk") for _ in range(2)]
                xT = em.sb([128, 8, BS], BF16, "xT")
                sil = em.sb([128, BS], BF16, "sil")
                aT = em.sb([128, 4, BS], BF16, "aT")
                ysub = [em.sb([128, D], F32, "ysub") for _ in range(2)]

                def load_blk(blk):
                    sl = blk % 2
                    s1, s3, s2 = (wb1D, wb3D, wb2D) if PRECONV else (ew1, ew3, ew2)
                    em.idma(w1s[sl].re("p k n -> p (k n)"), s1, widx[:, blk:blk + 1], False, 0)
                    em.idma(w3s[sl].re("p k n -> p (k n)"), s3, widx[:, blk:blk + 1], False, 0)
                    em.idma(w2s[sl].re("p k n -> p (k n)"), s2, widx[:, blk:blk + 1], False, 0)
                    em.dma(xblk[sl], xbD[blk * BS:(blk + 1) * BS, :].re("(s p) n -> p s n", p=128))

                load_blk(0)
                yc = 0
                for blk in range(NBLK):
                    sl = blk % 2
                    if blk + 1 < NBLK:
                        load_blk(blk + 1)
                    if CAST_IDMA:
                        w1b, w3b, w2b_ = w1s[sl], w3s[sl], w2s[sl]
                    else:
                        w1b, w3b, w2b_ = w1c, w3c, w2c
                        em.copy(w1b, w1s[sl], e="act")
                        em.copy(w3b, w3s[sl], e="dve")
                        em.copy(w2b_[:, 0:2, :], w2s[sl][:, 0:2, :], e="dve")
                        em.copy(w2b_[:, 2:4, :], w2s[sl][:, 2:4, :], e="act")
                    for s_ in range(NS):
                        ptb = ps[6 + s_ % 2].cast(BF16)
                        for k in range(8):
                            em.tr(ptb[:, k * 128:(k + 1) * 128], xblk[sl][:, s_, k * 128:(k + 1) * 128], identb)
                        em.copy(xT[:, :, s_ * 128:(s_ + 1) * 128], ptb.re("p (a b) -> p a b", a=8), e=("dve" if s_ % 2 else "act"))
                    for f in range(4):
                        pa = ps[f % 2]
                        pb_ = ps[2 + f % 2]
                        for k in range(8):
                            em.mm(pa, w1b[:, k, f * 128:(f + 1) * 128], xT[:, k, :], start=(k == 0), stop=(k == 7))
                        for k in range(8):
                            em.mm(pb_, w3b[:, k, f * 128:(f + 1) * 128], xT[:, k, :], start=(k == 0), stop=(k == 7))
                        em.act(sil, pa, AF.Silu)
                        em.tt(aT[:, f, :], sil, pb_, ALU.mult)
                    for s_ in range(NS):
                        yb_ = ysub[yc % 2]
                        yc += 1
                        for half in range(2):
                            po = ps[4 + half]
                            for f in range(4):
                                em.mm(po, aT[:, f, s_ * 128:(s_ + 1) * 128], w2b_[:, f, half * 512:(half + 1) * 512],
                                      start=(f == 0), stop=(f == 3))
                            em.copy(yb_[:, half * 512:(half + 1) * 512], po, e=("act" if half else "dve"))
                        em.dma(ybD[blk * BS + s_ * 128:blk * BS + (s_ + 1) * 128, :], yb_)
                em.barrier()
            em.stack = st
            if stage < 3:
                return
            with ExitStack() as st4:
                em.stack = st4
                g2r = em.sb([128, 3, D], F32, "g2r")
                for w, row in enumerate((2, 0, 1)):
                    bcast_row(g2r[:, w, :], modD[l, row, 5 * D:6 * D])
                lg = em.sb([128, D], F32, "lg2")
                lb = em.sb([128, D], F32, "lb2")
                bcast_row(lg, lnv[l, 2])
                bcast_row(lb, lnv[l, 3])
                xt = [em.sb([128, D], F32, "xt2") for _ in range(2)]
                yl = [em.sb([128, D], F32, "yl") for _ in range(2)]
                yh = [em.sb([128, D], F32, "yh") for _ in range(2)]
                stt_ = em.sb([128, 2, 6], F32, "bst2")
                mv = em.sb([128, 2], F32, "mv2")
                rstd = em.sb([128, 1], F32, "rstd2")
                for i in range(NT):
                    b, tt = i // NTT, i % NTT
                    if l == 1 and tt < 2:
                        continue
                    w = 0 if tt < 2 else 1 + b
                    xx = xt[i % 2]
                    ylo_ = yl[i % 2]
                    yhi_ = yh[i % 2]
                    tsl = slice(tt * 128, (tt + 1) * 128)
                    em.dma(xx, x1D[b, tsl, :])
                    em.idma(ylo_, ybD, dli[:, i:i + 1], False, 0)
                    em.idma(yhi_, ybD, dhi_i[:, i:i + 1], False, 0)
                    u_ = ylo_
                    em.ts(ylo_, ylo_, glo[:, i:i + 1], None, ALU.mult)
                    em.stt(u_, yhi_, ghi[:, i:i + 1], ylo_, ALU.mult, ALU.add)
                    em.tt(u_, u_, g2r[:, w, :], ALU.mult)
                    em.stt(u_, xx, ALPHA, u_, ALU.mult, ALU.add)
                    for half in range(2):
                        em.bn_stats(stt_[:, half, :], u_[:, half * 512:(half + 1) * 512])
                    em.bn_aggr(mv, stt_.re("p a b -> p (a b)"))
                    em.ts(rstd, mv[:, 1:2], LN_EPS, None, ALU.add)
                    em.act(rstd, rstd, AF.Sqrt)
                    em.recip(rstd, rstd)
                    em.ts(u_, u_, mv[:, 0:1], rstd, ALU.subtract, ALU.mult)
                    em.tt(u_, u_, lg, ALU.mult)
                    em.tt(xx, u_, lb, ALU.add)
                    if l == L - 1:
                        em.dma(y_out[b, (tt - 2) * 128:(tt - 1) * 128, :], xx)
                    else:
                        em.dma(sD[b, tsl, :], xx)
                em.barrier()
        em.stack = pstack

    for l in range(nlayers):
        if want("adaln"):
            adaln(l)
        if want("na"):
            na_phase(l)
        if want("rwkv"):
            rwkv_phase(l)
        if SPARSE_MOE:
            for b in range(2):
                if want("post"):
                    post_phase(l, b)
            if want("moe"):
                moe_sparse(l, moe_stage)
        else:
            for b in range(2):
                if want("post"):
                    post_phase(l, b)
                if want("moe"):
                    moe_phase(l, b)
    em.barrier()
    return nc, em


_CONSTS = None


def make_in_maps(inp, ncores=8):
    global _CONSTS
    if _CONSTS is None:
        _CONSTS = _consts_np()
    f = lambda a: np.ascontiguousarray(np.asarray(a, dtype=np.float32))
    shared = {k: f(inp[k]) for k in ("ada_w", "ada_b", "w_in", "rw_w2", "rw_a2", "rw_g2", "w_out", "router_w",
                                     "router_bias")}
    for nm, kc in (("exp_w1", 8), ("exp_w3", 8), ("exp_w2", 4)):
        w = f(inp[nm])
        n = w.shape[-1]
        shared[nm] = np.ascontiguousarray(w.reshape(L, 32, kc, 128, n).transpose(0, 1, 3, 2, 4)).reshape(L * 32 * 128, kc * n)
    shared["vecs"] = _vecs_np(inp)
    shared["gnrow"] = f(np.stack([inp["rw_gn_g"], inp["rw_gn_b"]], 1))
    shared["lnv"] = f(np.stack([inp["ln1_g"], inp["ln1_b"], inp["ln2_g"], inp["ln2_b"]], 1))
    shared["rb"] = _rb_gather(f(inp["na_rpb"]))
    for k, v in _CONSTS.items():
        shared[k] = v
    maps = []
    x = f(inp["x"])
    ctx = f(inp["ctx"])
    c = f(inp["c"])
    cc = f(inp["c_ctx"])
    for i in range(ncores):
        m = dict(shared)
        m["x"] = np.ascontiguousarray(x[2 * i:2 * i + 2])
        m["ctx"] = np.ascontiguousarray(ctx[2 * i:2 * i + 2])
        rows = np.stack([c[2 * i], c[2 * i + 1], cc], 0)
        m["cT"] = np.ascontiguousarray(rows.reshape(3, 8, 128).transpose(2, 1, 0))
        maps.append(m)
    return maps


def kernel(**inputs):
    nc, em = build()
    maps = make_in_maps(inputs, 8)
    res = run_bass_kernel_spmd(nc, maps, core_ids=list(range(8)))
    out = np.concatenate([np.asarray(r["y"], dtype=np.float32) for r in res.results], axis=0)
    return out
```

```python
from contextlib import ExitStack
import numpy as np
import concourse.bass as bass
import concourse.mybir as mybir
from concourse.bass_utils import run_bass_kernel_spmd

F32 = mybir.dt.float32
BF16 = mybir.dt.bfloat16
AF = mybir.ActivationFunctionType
ALU = mybir.AluOpType
AX = mybir.AxisListType

L = 2
D = 1024
S = 2048
CT = 256
TT = S + CT
NTT = TT // 128
DIN = 3296
LAM = float(np.exp(-0.5))
ALPHA = float((2 * L) ** 0.25)
LN_EPS = 1e-6
GN_EPS = 64e-5
NV = 70
DEBUG = False
INV_F32R = True
SPARSE_MOE = True
BS = 512
NBLK = (2 * 2 * TT + BS - 1) // BS + 32
CAST_IDMA = True
I32 = mybir.dt.int32
F32R = mybir.dt.float32r


class Buf:
    __slots__ = ("name", "w", "r")

    def __init__(self, name):
        self.name = name
        self.w = {}
        self.r = []


class T:
    __slots__ = ("ap", "buf")

    def __init__(self, ap, buf):
        self.ap = ap
        self.buf = buf

    def __getitem__(self, idx):
        return T(self.ap[idx], self.buf)

    def re(self, pat, **kw):
        return T(self.ap.rearrange(pat, **kw), self.buf)

    def bc(self, shape):
        return T(self.ap.to_broadcast(shape), self.buf)

    def pbc(self, n):
        return T(self.ap.partition_broadcast(n), self.buf)

    def cast(self, dt):
        return T(self.ap.bitcast(dt), self.buf)


class EM:
    NDS = 12

    def __init__(self, nc):
        self.nc = nc
        self.eng = {"pe": nc.tensor, "dve": nc.vector, "act": nc.scalar, "pool": nc.gpsimd, "sp": nc.sync}
        self.sem = {}
        self.cnt = {}
        self.waited = {e: {} for e in self.eng}
        for e in ("pe", "dve", "act", "pool"):
            self.sem[e] = nc.alloc_semaphore("s_" + e)
            self.cnt[e] = 0
        self.dsem = {}
        self.dcnt = {}
        for q in ("sp", "pool"):
            self.dsem[q] = [nc.alloc_semaphore(f"d_{q}{i}") for i in range(self.NDS)]
            self.dcnt[q] = 0
        self.ntens = 0
        self.ninst = 0
        self.stack = None

    def sb(self, shape, dt=F32, name=None):
        self.ntens += 1
        name = (name or "t") + f"_{self.ntens}"
        h = self.stack.enter_context(self.nc.sbuf_tensor(name, list(shape), dt))
        return T(h.ap(), Buf(name))

    def dram(self, name, shape, dt=F32, kind="Internal"):
        if DEBUG and kind == "Internal":
            kind = "ExternalOutput"
        h = self.nc.dram_tensor(name, list(shape), dt, kind=kind)
        return T(h.ap(), Buf(name))

    def _wait(self, e, tok):
        key = id(tok[0])
        if self.waited[e].get(key, 0) >= tok[1]:
            return
        self.waited[e][key] = tok[1]
        self.eng[e].wait_ge(tok[0], tok[1])
        self.ninst += 1

    def _deps(self, e, reads, writes, is_dma=False):
        for t in reads:
            for tok in t.buf.w.values():
                if e == "pe" and tok[2] == "pe":
                    continue
                self._wait(e, tok)
        for t in writes:
            b = t.buf
            for tok in b.w.values():
                if is_dma and tok[2] == "dma":
                    continue
                if (not is_dma) and tok[2] == e:
                    continue
                self._wait(e, tok)
            for tok in b.r:
                if (not is_dma) and tok[2] == e and e in ("pe", "dve", "act", "pool"):
                    continue
                self._wait(e, tok)

    def _done(self, tok, reads, writes, is_dma=False):
        wb = [t.buf for t in writes]
        for b in wb:
            if is_dma:
                b.w[id(tok[0])] = tok
            else:
                b.w = {id(tok[0]): tok}
            b.r = []
        for t in reads:
            b = t.buf
            if any(b is x for x in wb):
                continue
            b.r.append(tok)
            if len(b.r) > 16:
                best = {}
                for k in b.r:
                    kk = id(k[0])
                    if kk not in best or best[kk][1] < k[1]:
                        best[kk] = k
                b.r = list(best.values())

    def op(self, e, fn, reads=(), writes=()):
        reads = [t for t in reads if isinstance(t, T)]
        writes = [t for t in writes if isinstance(t, T)]
        self._deps(e, reads, writes)
        inst = fn(self.eng[e])
        self.cnt[e] += 1
        inst.then_inc(self.sem[e], 1)
        tok = (self.sem[e], self.cnt[e], e)
        self._done(tok, reads, writes)
        self.ninst += 1
        return tok

    def dma(self, out, in_, q="sp"):
        i = self.dcnt[q]
        self.dcnt[q] += 1
        s = self.dsem[q][i % self.NDS]
        val = 16 * (i // self.NDS + 1)
        if val > 16:
            self._wait(q, (s, val - 16, "dma"))
        self._deps(q, [in_], [out], True)
        self.eng[q].dma_start(out=out.ap, in_=in_.ap).then_inc(s, 16)
        tok = (s, val, "dma")
        self._done(tok, [in_], [out], True)
        self.ninst += 1
        return tok

    def idma(self, out, in_, idx, scatter, bound):
        q = "pool"
        i = self.dcnt[q]
        self.dcnt[q] += 1
        s = self.dsem[q][i % self.NDS]
        val = 16 * (i // self.NDS + 1)
        if val > 16:
            self._wait(q, (s, val - 16, "dma"))
        self._deps(q, [in_, idx], [out], True)
        off = bass.IndirectOffsetOnAxis(ap=idx.ap, axis=0)
        if scatter:
            inst = self.nc.gpsimd.indirect_dma_start(out=out.ap, out_offset=off, in_=in_.ap, in_offset=None)
        else:
            inst = self.nc.gpsimd.indirect_dma_start(out=out.ap, out_offset=None, in_=in_.ap, in_offset=off)
        inst.then_inc(s, 16)
        tok = (s, val, "dma")
        self._done(tok, [in_, idx], [out], True)
        self.ninst += 1
        return tok

    def barrier(self):
        toks = []
        for e in ("pe", "dve", "act", "pool"):
            if self.cnt[e] > 0:
                toks.append((self.sem[e], self.cnt[e], e))
        for q in self.dsem:
            n = self.dcnt[q]
            for j in range(min(n, self.NDS)):
                last = ((n - 1 - j) // self.NDS) if (n - 1 - j) >= 0 else -1
            for si in range(self.NDS):
                k = (n - si + self.NDS - 1) // self.NDS if n > si else 0
                if k > 0:
                    toks.append((self.dsem[q][si], 16 * k, "dma"))
        for e in self.eng:
            for tok in toks:
                if tok[2] == e and e in ("pe", "dve", "act"):
                    continue
                self._wait(e, tok)

    def mm(self, out, lhsT, rhs, start=True, stop=True):
        return self.op("pe", lambda g: g.matmul(out.ap, lhsT.ap, rhs.ap, start=start, stop=stop, skip_group_check=True),
                       [lhsT, rhs], [out])

    def tr(self, out, in_, ident):
        return self.op("pe", lambda g: g.transpose(out.ap, in_.ap, ident.ap), [in_, ident], [out])

    def act(self, out, in_, func, bias=None, scale=None):
        kw = {}
        rd = [in_]
        if bias is not None:
            kw["bias"] = bias.ap if isinstance(bias, T) else bias
            rd.append(bias)
        if scale is not None:
            kw["scale"] = scale.ap if isinstance(scale, T) else scale
            rd.append(scale)
        return self.op("act", lambda g: g.activation(out.ap, in_.ap, func, **kw), rd, [out])

    def tt(self, out, a, b, op, e="dve"):
        return self.op(e, lambda g: g.tensor_tensor(out.ap, a.ap, b.ap, op), [a, b], [out])

    def ts(self, out, a, s1, s2=None, op0=ALU.mult, op1=None, e="dve"):
        rd = [a, s1, s2]
        A1 = s1.ap if isinstance(s1, T) else s1
        A2 = s2.ap if isinstance(s2, T) else s2
        if op1 is None:
            return self.op(e, lambda g: g.tensor_scalar(out.ap, a.ap, A1, None, op0), rd, [out])
        return self.op(e, lambda g: g.tensor_scalar(out.ap, a.ap, A1, A2, op0, op1), rd, [out])

    def stt(self, out, a, s, b, op0, op1):
        rd = [a, b, s]
        Sx = s.ap if isinstance(s, T) else s
        return self.op("dve", lambda g: g.scalar_tensor_tensor(out.ap, a.ap, Sx, b.ap, op0, op1), rd, [out])

    def copy(self, out, in_, e="dve"):
        if e == "act":
            return self.op(e, lambda g: g.copy(out.ap, in_.ap), [in_], [out])
        return self.op(e, lambda g: g.tensor_copy(out.ap, in_.ap), [in_], [out])

    def memset(self, out, val, e="dve"):
        return self.op(e, lambda g: g.memset(out.ap, val), [], [out])

    def recip(self, out, in_):
        return self.op("dve", lambda g: g.reciprocal(out.ap, in_.ap), [in_], [out])

    def scan(self, out, d0, d1, init, op0, op1):
        return self.op("dve", lambda g: g.tensor_tensor_scan(out.ap, d0.ap, d1.ap, init, op0, op1), [d0, d1], [out])

    def reduce(self, out, in_, op, axis=AX.X):
        return self.op("dve", lambda g: g.tensor_reduce(out.ap, in_.ap, axis, op), [in_], [out])

    def bn_stats(self, out, in_):
        return self.op("dve", lambda g: g.bn_stats(out.ap, in_.ap), [in_], [out])

    def bn_aggr(self, out, in_):
        return self.op("dve", lambda g: g.bn_aggr(out.ap, in_.ap), [in_], [out])


def _na_variants():
    types = {0: [0, 1, 2, 3], 1: [-1, 0, 1, 2], 2: [-2, -1, 0, 1, 2], 3: [-2, -1, 0, 1], 4: [-3, -2, -1, 0]}
    rep = {0: 0, 1: 1, 2: 5, 3: 14, 4: 15}
    var = []
    for ty in range(5):
        for dc in types[ty]:
            var.append((ty, dc))
    return types, rep, var


def _qtype(qc):
    return {0: 0, 1: 1, 14: 3, 15: 4}.get(qc, 2)


def _consts_np():
    c = {}
    idx = np.arange(128)
    c["ident"] = np.eye(128, dtype=np.float32)
    blk = (idx[:, None] // 64 == idx[None, :] // 64).astype(np.float32)
    c["blockones"] = blk
    rm = np.zeros((128, 128), np.float32)
    for h in range(2):
        for m in range(64):
            q = m // 16
            if q in (0, 2):
                rm[h * 64 + m + 16, h * 64 + m] = -1.0
            else:
                rm[h * 64 + m - 16, h * 64 + m] = 1.0
    c["rmat"] = rm
    LT = (idx[:, None] < idx[None, :]).astype(np.float32)
    LE = (idx[:, None] <= idx[None, :]).astype(np.float32)
    GT = (idx[:, None] > idx[None, :]).astype(np.float32)
    GE = (idx[:, None] >= idx[None, :]).astype(np.float32)
    c["maskA"] = np.stack([np.concatenate([LT, LE], 1), np.concatenate([GT, GE], 1)], 1).astype(np.float32)
    c["maskN"] = np.stack([GT, GT, LT, LT], 1).astype(np.float32)
    t = np.arange(S)
    row = (t // 64).astype(np.float32)
    col = (t % 64).astype(np.float32)
    inv = (10000.0 ** (-np.arange(16, dtype=np.float32) / 16)).astype(np.float32)
    ar = row[:, None] * inv
    ac = col[:, None] * inv
    ang = np.concatenate([ar, ar, ac, ac], -1).astype(np.float32)
    cs = np.cos(ang).astype(np.float32).T
    sn = np.sin(ang).astype(np.float32).T
    c["cos"] = np.ascontiguousarray(np.concatenate([cs, cs], 0))
    c["sin"] = np.ascontiguousarray(np.concatenate([sn, sn], 0))
    types, rep, var = _na_variants()
    mk = np.zeros((128, len(var), 128), np.float32)
    for v, (ty, dc) in enumerate(var):
        qc = rep[ty]
        for kr in range(2):
            krow = 2 * (qc + dc) + kr
            for qr in range(2):
                i = 2 * qc + qr
                rs = min(max(i - 4, 0), 24)
                if not (rs <= krow < rs + 8):
                    continue
                for qcol in range(64):
                    cst = min(max(qcol - 8, 0), 48)
                    mk[kr * 64 + cst: kr * 64 + cst + 16, v, qr * 64 + qcol] = 1.0
    c["namask"] = mk
    c["thr"] = np.tile((float(BS) * np.arange(128, dtype=np.float32))[None, :], (128, 1))
    c["kp"] = np.tile(np.arange(128, dtype=np.float32)[:, None], (1, 8))
    return c


def _rb_gather(rpb):
    kr = np.arange(128)[:, None] // 64
    kc = np.arange(128)[:, None] % 64
    qr = np.arange(128)[None, :] // 64
    qc = np.arange(128)[None, :] % 64
    out = np.zeros((L, 7, 128, 8, 128), np.float32)
    ci = np.clip(kc - qc + 15, 0, 30)
    for d in range(7):
        ri = np.clip(2 * (d - 3) + kr - qr + 7, 0, 14)
        g = rpb[:, :, ri, ci]
        out[:, d] = np.transpose(g, (0, 2, 1, 3))
    return out


def _vecs_np(inp):
    v = np.zeros((L, 128, NV), np.float32)
    groups = [(g * 128, 128) for g in range(12)] + [(1536, 32), (1568, 32), (1600, 32), (1632, 32), (1664, 96)]
    for l in range(L):
        mp = inp["rw_mu_prev"][l]
        mn = inp["rw_mu_next"][l]
        for g, (a, n) in enumerate(groups):
            v[l, :n, g] = mp[a:a + n]
            v[l, :n, 17 + g] = mn[a:a + n]
        for d in range(2):
            for j in range(4):
                v[l, :, 34 + d * 4 + j] = inp["rw_w0"][l, d, j * 128:(j + 1) * 128]
                v[l, :, 42 + d * 4 + j] = inp["rw_a0"][l, d, j * 128:(j + 1) * 128]
        rk = inp["rw_r_k"][l].reshape(512)
        for j in range(4):
            sl = slice(j * 128, (j + 1) * 128)
            v[l, :, 50 + j] = inp["rw_k_k"][l, sl]
            v[l, :, 54 + j] = inp["rw_k_a"][l, sl]
            v[l, :, 58 + j] = rk[sl]
            v[l, :, 62 + j] = inp["rw_gn_g"][l, sl]
            v[l, :, 66 + j] = inp["rw_gn_b"][l, sl]
    return v


def build(nlayers=L, phases=None, na_stage=9, rw_stage=9, moe_stage=9):
    nc = bass.Bass("TRN2", target_bir_lowering=False)
    em = EM(nc)
    types, rep, variants = _na_variants()
    NVAR = len(variants)

    def din(name, shape, dt=F32):
        return em.dram(name, shape, dt, kind="ExternalInput")

    x_in = din("x", [2, S, D])
    ctx_in = din("ctx", [2, CT, D])
    cT_in = din("cT", [128, 8, 3])
    ada_w = din("ada_w", [L, D, 6 * D])
    ada_b = din("ada_b", [L, 6 * D])
    w_in = din("w_in", [L, D, DIN])
    vecs = din("vecs", [L, 128, NV])
    rw_w2 = din("rw_w2", [L, 2, 32, 512])
    rw_a2 = din("rw_a2", [L, 2, 32, 512])
    rw_g2 = din("rw_g2", [L, 96, 512])
    gnrow = din("gnrow", [L, 2, 512])
    w_out = din("w_out", [L, D, D])
    lnv = din("lnv", [L, 4, D])
    router_w = din("router_w", [D, 32])
    rbias = din("router_bias", [32])
    ew1 = din("exp_w1", [L * 32 * 128, 8 * 512])
    ew3 = din("exp_w3", [L * 32 * 128, 8 * 512])
    ew2 = din("exp_w2", [L * 32 * 128, 4 * D])
    c_ident = din("ident", [128, 128])
    c_blk = din("blockones", [128, 128])
    c_rmat = din("rmat", [128, 128])
    c_maskA = din("maskA", [128, 2, 256])
    c_maskN = din("maskN", [128, 4, 128])
    c_cos = din("cos", [128, S])
    c_sin = din("sin", [128, S])
    c_namask = din("namask", [128, NVAR, 128])
    c_thr = din("thr", [128, 128])
    c_kp = din("kp", [128, 8])
    rb_in = din("rb", [L, 7, 128, 8, 128])
    y_out = em.dram("y", [2, S, D], F32, kind="ExternalOutput")

    modD = em.dram("modD", [L, 3, 6 * D])
    sD = em.dram("sD", [2, TT, D])
    x1D = em.dram("x1D", [2, TT, D])
    mixD = em.dram("mixD", [2, 128, 8, TT], BF16)
    h2D = em.dram("h2D", [2, 128, 8, TT], BF16)
    hTD = em.dram("hTD", [2, 128, 8, TT], BF16)
    h2tokD = em.dram("h2tokD", [2 * TT, D], BF16)
    xbD = em.dram("xbD", [NBLK * BS, D], BF16)
    ybD = em.dram("ybD", [NBLK * BS, D], F32)
    KRD = em.dram("KRD", [2, 2, 4, 128, NTT, 2, 128], BF16)
    BKD = em.dram("BKD", [2, 2, 4, 128, NTT, 2, 128], BF16)
    KHD = em.dram("KHD", [2, 2, 4, 128, NTT, 2, 128], BF16)
    VFD = em.dram("VFD", [2, 4, 128, TT], BF16)
    BOND = em.dram("BOND", [2, 4, 128, TT], F32)
    GCD = em.dram("GCD", [2, 2, 4, 128, NTT], F32)

    pst = nc.alloc_psum_tensor("psum", [128, 8, 512], F32)
    ps = [T(pst.ap()[:, i, :], Buf(f"ps{i}")) for i in range(8)]

    def seq_tile(l, b, tt):
        if l == 0:
            if tt < 2:
                return ctx_in[b, tt * 128:(tt + 1) * 128, :]
            return x_in[b, (tt - 2) * 128:(tt - 1) * 128, :]
        return sD[b, tt * 128:(tt + 1) * 128, :]

    def want(name):
        return phases is None or name in phases

    pstack = ExitStack()
    em.stack = pstack
    ident = em.sb([128, 128], F32, "ident")
    em.dma(ident, c_ident)
    identb = em.sb([128, 128], BF16, "identb")
    em.copy(identb, ident)
    G_all2 = em.sb([128, 2, NTT, 32], F32, "G_all")

    def bcast_row(dst, row_ap_T):
        em.dma(dst, row_ap_T.pbc(128))

    def adaln(l):
        with ExitStack() as st:
            em.stack = st
            cTs = em.sb([128, 8, 3])
            em.dma(cTs, cT_in)
            sc = em.sb([128, 8, 3])
            em.act(sc, cTs, AF.Silu)
            adab = em.sb([3, 6 * D])
            em.dma(adab, ada_b[l].pbc(3))
            modsb = em.sb([3, 6 * D])
            wbuf = [em.sb([128, 8, 512]) for _ in range(2)]
            awv = ada_w[l].re("(k p) n -> p k n", p=128)
            for blk in range(12):
                wb = wbuf[blk % 2]
                em.dma(wb, awv[:, :, blk * 512:(blk + 1) * 512])
                pp = ps[blk % 2][0:3, :]
                for k in range(8):
                    em.mm(pp, sc[:, k, :], wb[:, k, :], start=(k == 0), stop=(k == 7))
                em.tt(modsb[:, blk * 512:(blk + 1) * 512], pp, adab[:, blk * 512:(blk + 1) * 512], ALU.add)
            em.dma(modD[l], modsb)
            em.barrier()
        em.stack = pstack

    def make_hT(l, b, hT, sc1, sh1):
        xt = [em.sb([128, D], F32, "xt") for _ in range(2)]
        for tt in range(NTT):
            xx = xt[tt % 2]
            em.dma(xx, seq_tile(l, b, tt))
            w = 0 if tt < 2 else 1
            em.tt(xx, xx, sc1[:, w, :], ALU.mult)
            em.tt(xx, xx, sh1[:, w, :], ALU.add)
            for half in range(2):
                pp = ps[6 + half]
                for kk in range(4):
                    k = half * 4 + kk
                    em.tr(pp[:, kk * 128:(kk + 1) * 128], xx[:, k * 128:(k + 1) * 128], ident)
                dst = hT[:, half * 4:(half + 1) * 4, tt * 128:(tt + 1) * 128]
                src = pp.re("p (a b) -> p a b", a=4)
                if half == 0:
                    em.copy(dst, src, e="act")
                else:
                    em.copy(dst, src, e="dve")

    def load_mod1(l, b):
        sc1 = em.sb([128, 2, D], F32, "sc1")
        sh1 = em.sb([128, 2, D], F32, "sh1")
        for w, row in enumerate((2, b)):
            bcast_row(sh1[:, w, :], modD[l, row, 0:D])
            bcast_row(sc1[:, w, :], modD[l, row, D:2 * D])
        em.ts(sc1, sc1, 1.0, None, ALU.add)
        return sc1, sh1

    def na_phase(l):
        with ExitStack() as st:
            em.stack = st
            Tb = em.sb([128, NVAR, 2, 4, 128], BF16, "Tb")
            with ExitStack() as st2:
                em.stack = st2
                mk = em.sb([128, NVAR, 128], F32, "mk")
                em.dma(mk, c_namask)
                for d in range(7):
                    rbt = em.sb([128, 8, 128], F32, "rbt")
                    em.dma(rbt, rb_in[l, d])
                    em.act(rbt, rbt, AF.Exp)
                    for v, (ty, dc) in enumerate(variants):
                        if dc + 3 == d:
                            for par in range(2):
                                em.tt(Tb[:, v, par], rbt.re("p (a two) q -> p a two q", two=2)[:, :, par, :],
                                      mk[:, v:v + 1, :].bc([128, 4, 128]), ALU.mult)
                em.barrier()
            em.stack = st
            ones_b = em.sb([128, 64], BF16, "ones_b")
            em.memset(ones_b, 1.0)
            wq = em.sb([128, 8, 1536], BF16, "wq")
            em.dma(wq, w_in[l].re("(k p) n -> p k n", p=128)[:, :, 0:1536], q="pool")
            for b in range(2):
                with ExitStack() as st3:
                    em.stack = st3
                    qT = em.sb([128, 4, TT], BF16, "qT")
                    kT = em.sb([128, 4, TT], BF16, "kT")
                    Vt = em.sb([128, NTT, 512], BF16, "Vt")
                    with ExitStack() as st4:
                        em.stack = st4
                        sc1, sh1 = load_mod1(l, b)
                        hT = em.sb([128, 8, TT], BF16, "hT")
                        make_hT(l, b, hT, sc1, sh1)
                        em.dma(hTD[b], hT)
                        cnt = 0
                        for ci in range(8):
                            dst = qT if ci < 4 else kT
                            for t0 in range(0, TT, 512):
                                w = min(512, TT - t0)
                                pp = ps[cnt % 2]
                                for k in range(8):
                                    em.mm(pp[:, 0:w], wq[:, k, ci * 128:(ci + 1) * 128], hT[:, k, t0:t0 + w],
                                          start=(k == 0), stop=(k == 7))
                                em.copy(dst[:, ci % 4, t0:t0 + w], pp[:, 0:w], e=("act" if cnt % 2 else "dve"))
                                cnt += 1
                        for tt in range(NTT):
                            pp = ps[cnt % 2]
                            for k in range(8):
                                em.mm(pp, hT[:, k, tt * 128:(tt + 1) * 128], wq[:, k, 1024:1536],
                                      start=(k == 0), stop=(k == 7))
                            em.copy(Vt[:, tt, :], pp, e=("act" if cnt % 2 else "dve"))
                            cnt += 1
                        em.barrier()
                    em.stack = st3
                    nab = [em.sb([128, 4, 128], BF16, "nab") for _ in range(2)]
                    if na_stage < 2:
                        em.barrier()
                        continue
                    Eb = [em.sb([128, 4, 128], BF16, "Eb") for _ in range(3)]
                    rec = em.sb([128, 512], F32, "rec")
                    qlist = [(2 + qc, True) for qc in range(16)]
                    if l == 0:
                        qlist += [(0, False), (1, False)]
                    its = []
                    for qi, (qt, is_x) in enumerate(qlist):
                        if is_x:
                            qc = qt - 2
                            ty = _qtype(qc)
                            keys = [(2 + qc + dc, variants.index((ty, dc))) for dc in types[ty]] + [(0, None), (1, None)]
                        else:
                            keys = [(0, None), (1, None)]
                        for ki, (kt, v) in enumerate(keys):
                            for hg in range(2):
                                its.append((qi, qt, ki, kt, v, hg, len(keys)))
                    NIT = len(its)

                    def emit_S(n):
                        qi, qt, ki, kt, v, hg, nk = its[n]
                        sp_ = ps[n % 4]
                        rows = slice(hg * 64, hg * 64 + 64)
                        for hh in range(4):
                            em.mm(sp_[:, hh * 128:(hh + 1) * 128], kT[rows, hh, kt * 128:(kt + 1) * 128],
                                  qT[rows, hh, qt * 128:(qt + 1) * 128])

                    def emit_E(n):
                        qi, qt, ki, kt, v, hg, nk = its[n]
                        E = Eb[n % 3]
                        em.act(E, ps[n % 4].re("p (a b) -> p a b", a=4), AF.Exp, scale=0.125)
                        if v is not None:
                            em.tt(E, E, Tb[:, v, hg, :, :], ALU.mult)

                    def emit_PV(n):
                        qi, qt, ki, kt, v, hg, nk = its[n]
                        E = Eb[n % 3]
                        num = ps[4 + 2 * (qi % 2)]
                        den = ps[5 + 2 * (qi % 2)]
                        rows = slice(hg * 64, hg * 64 + 64)
                        for hh in range(4):
                            h = 2 * hh + hg
                            cs = slice(hh * 128, hh * 128 + 128)
                            first = (ki == 0 and hh == 0)
                            em.mm(num[rows, cs], Vt[:, kt, h * 64:(h + 1) * 64], E[:, hh, :], start=first, stop=(ki == nk - 1))
                        em.mm(den[rows, :], ones_b, E.re("p a b -> p (a b)"), start=(ki == 0), stop=(ki == nk - 1))
                        if ki == nk - 1 and hg == 1:
                            em.recip(rec, den)
                            nb_ = nab[qi % 2]
                            em.tt(nb_, num.re("p (a b) -> p a b", a=4), rec.re("p (a b) -> p a b", a=4), ALU.mult)
                            em.dma(mixD[b, :, 0:4, qt * 128:(qt + 1) * 128], nb_)

                    emit_S(0)
                    emit_S(1)
                    emit_E(0)
                    for n in range(NIT):
                        if n + 2 < NIT:
                            emit_S(n + 2)
                        if n + 1 < NIT:
                            emit_E(n + 1)
                        emit_PV(n)
                    em.barrier()
            em.stack = st
            em.barrier()
        em.stack = pstack

    SEGS = [(0, 256, False, 0, 0)] + [(256 + i * 512, 512, True, int(i > 0), int(i < 3)) for i in range(4)]
    REV_ORDER = [1, 0] + list(range(NTT - 1, 1, -1))

    def rwkv_phase(l):
        with ExitStack() as st:
            em.stack = st
            vc = em.sb([128, NV], F32, "vc")
            em.dma(vc, vecs[l])
            c0v = em.sb([128, 17], F32, "c0v")
            em.tt(c0v, vc[:, 0:17], vc[:, 17:34], ALU.add)
            em.ts(c0v, c0v, -1.0, 1.0, ALU.mult, ALU.add)
            omka = em.sb([128, 4], F32, "omka")
            em.ts(omka, vc[:, 54:58], -1.0, 1.0, ALU.mult, ALU.add)
            blk = em.sb([128, 128], F32, "blk")
            em.dma(blk, c_blk)
            rmat = em.sb([128, 128], F32, "rmat")
            em.dma(rmat, c_rmat)
            blkrk = em.sb([128, 4, 128], F32, "blkrk")
            for j in range(4):
                em.ts(blkrk[:, j, :], blk, vc[:, 58 + j:59 + j], None, ALU.mult)
            maskA = em.sb([128, 2, 256], BF16, "maskA")
            maskN = em.sb([128, 4, 128], BF16, "maskN")
            w2b = em.sb([32, 2, 512], BF16, "w2b")
            a2b = em.sb([32, 2, 512], BF16, "a2b")
            g2b = em.sb([96, 512], BF16, "g2b")
            em.dma(maskA, c_maskA, q="pool")
            em.dma(maskN, c_maskN, q="pool")
            em.dma(w2b, rw_w2[l].re("d r c -> r d c"), q="pool")
            em.dma(a2b, rw_a2[l].re("d r c -> r d c"), q="pool")
            em.dma(g2b, rw_g2[l], q="pool")
            sgs = [em.sb([96, TT], BF16, "sg") for _ in range(2)]
            maskA32 = em.sb([128, 2, 128], F32, "maskA32")
            maskN32 = em.sb([128, 4, 128], F32, "maskN32")
            em.copy(maskA32, maskA[:, :, 0:128])
            em.copy(maskN32, maskN)
            stw = ExitStack()
            em.stack = stw
            wr = em.sb([128, 8, 1760], BF16, "wr")
            wv_ = w_in[l].re("(k p) n -> p k n", p=128)
            em.dma(wr[:, :, 1536:1760], wv_[:, :, 3072:DIN], q="pool")
            for j_ in range(4):
                for part in range(3):
                    c_ = part * 512 + j_ * 128
                    em.dma(wr[:, :, c_:c_ + 128], wv_[:, :, 1536 + c_:1536 + c_ + 128], q="pool")
            for b in range(2):
                with ExitStack() as st2:
                    em.stack = st2
                    sg = sgs[b]
                    rstm = em.sb([128, 512], F32, "rstm")
                    em.memset(rstm, 1.0)
                    em.memset(rstm.re("p (n t) -> p n t", t=128)[:, :, 0:1], 0.0)
                    tl = em.sb([32, 2, TT], BF16, "tl")
                    la = em.sb([32, 2, TT], BF16, "la")
                    hT = em.sb([128, 8, TT], BF16, "hT")
                    if want("na"):
                        em.dma(hT, hTD[b])
                    else:
                        with ExitStack() as st5:
                            em.stack = st5
                            sc1, sh1 = load_mod1(l, b)
                            make_hT(l, b, hT, sc1, sh1)
                            em.barrier()
                        em.stack = st2
                    cosb = em.sb([128, 512], F32, "cosb")
                    sinb = em.sb([128, 512], F32, "sinb")

                    class Lane:
                        pass

                    def mk_lane(li):
                        ln = Lane()
                        ln.B = ps[4 * li:4 * li + 4]
                        ln.c0 = 0
                        ln.c1 = 0
                        ln.Pb = em.sb([128, 514], F32, "Pb")
                        ln.tmp = [em.sb([128, 512], F32, f"tmp{i}") for i in range(3)]
                        for nm in ("r_", "k_", "v_", "kk_", "lw_", "a_", "cum_", "kd_", "be_", "rks"):
                            setattr(ln, nm, em.sb([128, 512], F32, nm))
                        ln.ob = [em.sb([128, 512], BF16, f"ob{i}") for i in range(7)]
                        ln.gct = em.sb([128, 4], F32, "gct")
                        return ln

                    def bankA(ln):
                        ln.c0 += 1
                        return ln.B[ln.c0 % 2]

                    def bankB(ln):
                        ln.c1 += 1
                        return ln.B[2 + ln.c1 % 2]

                    def proj_shift(ln, dst, g, m, c0, seg):
                        t0, n, rope, hl, hr = seg
                        Pb = ln.Pb
                        ta = t0 - hl
                        tb = t0 + n + hr
                        if not hl:
                            em.memset(Pb[0:m, 0:1], 0.0)
                        if not hr:
                            em.memset(Pb[0:m, n + 1:n + 2], 0.0)
                        for s0 in range(ta, tb, 512):
                            w = min(512, tb - s0)
                            pp = bankA(ln)
                            for k in range(8):
                                em.mm(pp[0:m, 0:w], wr[:, k, c0:c0 + m], hT[:, k, s0:s0 + w], start=(k == 0), stop=(k == 7))
                            o = 1 - hl + (s0 - ta)
                            em.copy(Pb[0:m, o:o + w], pp[0:m, 0:w], e="act")
                        em.ts(dst, Pb[0:m, 1:n + 1], c0v[0:m, g:g + 1], None, ALU.mult)
                        em.stt(dst, Pb[0:m, 0:n], vc[0:m, g:g + 1], dst, ALU.mult, ALU.add)
                        em.stt(dst, Pb[0:m, 2:n + 2], vc[0:m, 17 + g:18 + g], dst, ALU.mult, ALU.add)

                    lanes = [mk_lane(0), mk_lane(1)]
                    for si, seg in enumerate(SEGS):
                        t0, n = seg[0], seg[1]
                        ln = lanes[si % 2]
                        t_ = ln.tmp[0]
                        for d in range(2):
                            proj_shift(ln, t_[0:32, 0:n], 12 + d, 32, 1536 + d * 32, seg)
                            em.act(tl[:, d, t0:t0 + n], t_[0:32, 0:n], AF.Tanh)
                            proj_shift(ln, t_[0:32, 0:n], 14 + d, 32, 1600 + d * 32, seg)
                            em.copy(la[:, d, t0:t0 + n], t_[0:32, 0:n])
                        proj_shift(ln, t_[0:96, 0:n], 16, 96, 1664, seg)
                        em.act(sg[:, t0:t0 + n], t_[0:96, 0:n], AF.Sigmoid)

                    def rope_apply(ln, z, n):
                        pp = bankB(ln)
                        em.mm(pp[:, 0:n], rmat, z[:, 0:n])
                        em.tt(ln.tmp[0][:, 0:n], pp[:, 0:n], sinb[:, 0:n], ALU.mult)
                        em.tt(z[:, 0:n], z[:, 0:n], cosb[:, 0:n], ALU.mult)
                        em.tt(z[:, 0:n], z[:, 0:n], ln.tmp[0][:, 0:n], ALU.add)

                    def pair_gen(ln, seg, j):
                        t0, n, rope, hl, hr = seg
                        nch = n // 128
                        n0 = t0 // 128
                        tmp = ln.tmp
                        ob = ln.ob
                        r = ln.r_[:, 0:n]
                        k = ln.k_[:, 0:n]
                        v = ln.v_[:, 0:n]
                        kk = ln.kk_[:, 0:n]
                        rks = ln.rks
                        proj_shift(ln, r, j, 128, j * 128, seg)
                        yield
                        proj_shift(ln, k, 4 + j, 128, 512 + j * 128, seg)
                        yield
                        proj_shift(ln, v, 8 + j, 128, 1024 + j * 128, seg)
                        yield
                        if rope:
                            rope_apply(ln, ln.r_, n)
                            yield
                            rope_apply(ln, ln.k_, n)
                            yield
                        em.copy(ob[6][:, 0:n], v)
                        em.dma(VFD[b, j, :, t0:t0 + n], ob[6][:, 0:n])
                        em.ts(kk, k, vc[:, 50 + j:51 + j], None, ALU.mult)
                        em.tt(tmp[0][:, 0:n], kk, kk, ALU.mult)
                        pp = bankB(ln)
                        em.mm(pp[:, 0:n], blk, tmp[0][:, 0:n])
                        yield
                        em.act(tmp[1][:, 0:n], pp[:, 0:n], AF.Sqrt)
                        yield
                        em.ts(tmp[1][:, 0:n], tmp[1][:, 0:n], 1e-12, None, ALU.max)
                        em.recip(tmp[1][:, 0:n], tmp[1][:, 0:n])
                        em.tt(kk, kk, tmp[1][:, 0:n], ALU.mult)
                        yield
                        for d in range(2):
                            lw = ln.lw_[:, 0:n]
                            a = ln.a_[:, 0:n]
                            cum = ln.cum_[:, 0:n]
                            kd = ln.kd_[:, 0:n]
                            be = ln.be_[:, 0:n]
                            pp = bankB(ln)
                            em.mm(pp[:, 0:n], w2b[:, d, j * 128:(j + 1) * 128], tl[:, d, t0:t0 + n])
                            pp2 = bankB(ln)
                            em.mm(pp2[:, 0:n], a2b[:, d, j * 128:(j + 1) * 128], la[:, d, t0:t0 + n])
                            yield
                            em.act(lw, pp[:, 0:n], AF.Sigmoid, bias=vc[:, 34 + d * 4 + j:35 + d * 4 + j])
                            em.act(a, pp2[:, 0:n], AF.Sigmoid, bias=vc[:, 42 + d * 4 + j:43 + d * 4 + j])
                            yield
                            em.scan(tmp[0][:, 0:n], rstm[:, 0:n], lw, 0.0, ALU.mult, ALU.add)
                            pre3 = tmp[0][:, 0:n].re("p (n t) -> p n t", t=128)
                            totb = pre3[:, :, 127:128].bc([128, nch, 128])
                            cum3 = cum.re("p (n t) -> p n t", t=128)
                            if d == 0:
                                em.copy(cum, tmp[0][:, 0:n])
                            else:
                                em.tt(cum3, totb, pre3, ALU.subtract)
                                em.tt(cum, cum, lw, ALU.add)
                            em.act(ln.gct[:, 0:nch], pre3[:, :, 127], AF.Exp, scale=-LAM)
                            em.dma(GCD[b, d, j, :, n0:n0 + nch], ln.gct[:, 0:nch])
                            em.ts(tmp[1][:, 0:n], a, vc[:, 54 + j:55 + j], omka[:, j:j + 1], ALU.mult, ALU.add)
                            em.tt(kd, tmp[1][:, 0:n], k, ALU.mult)
                            em.tt(be, kk, a, ALU.mult)
                            if d == 0:
                                em.tt(rks[:, 0:n], r, kd, ALU.mult)
                            else:
                                em.tt(tmp[1][:, 0:n], r, kd, ALU.mult)
                                em.tt(rks[:, 0:n], rks[:, 0:n], tmp[1][:, 0:n], ALU.add)
                            yield
                            e1 = tmp[1][:, 0:n]
                            e2 = tmp[2][:, 0:n]
                            em.act(e1, cum, AF.Exp, scale=-LAM)
                            em.tt(e2, cum, lw, ALU.subtract)
                            yield
                            em.tt(ob[1][:, 0:n], r, e1, ALU.mult)
                            em.act(e1, cum, AF.Exp, scale=LAM)
                            em.act(e2, e2, AF.Exp, scale=-LAM)
                            yield
                            em.tt(ob[3][:, 0:n], kd, e1, ALU.mult)
                            em.stt(ob[2][:, 0:n], be, -1.0, e1, ALU.mult, ALU.mult)
                            em.tt(ob[0][:, 0:n], kk, e2, ALU.mult)
                            em.tt(e2.re("p (n t) -> p n t", t=128), totb, cum3, ALU.subtract)
                            em.act(e2, e2, AF.Exp, scale=-LAM)
                            yield
                            em.tt(ob[4][:, 0:n], kd, e2, ALU.mult)
                            em.stt(ob[5][:, 0:n], be, -1.0, e2, ALU.mult, ALU.mult)
                            for idx, (dst, slot) in enumerate(((KRD[b], 0), (KRD[b], 1), (BKD[b], 0), (BKD[b], 1), (KHD[b], 0), (KHD[b], 1))):
                                em.dma(dst[d, j, :, n0:n0 + nch, slot, :], ob[idx][:, 0:n].re("p (n t) -> p n t", t=128))
                            yield
                        pp = bankB(ln)
                        em.mm(pp[:, 0:n], blkrk[:, j, :], rks[:, 0:n])
                        yield
                        em.tt(tmp[0][:, 0:n], pp[:, 0:n], v, ALU.mult)
                        em.dma(BOND[b, j, :, t0:t0 + n], tmp[0][:, 0:n])

                    for seg in SEGS:
                        t0, n, rope, hl, hr = seg
                        if rope:
                            em.dma(cosb[:, 0:n], c_cos[:, t0 - CT:t0 - CT + n])
                            em.dma(sinb[:, 0:n], c_sin[:, t0 - CT:t0 - CT + n])
                        for jj in range(0, 4, 2):
                            gens = [pair_gen(lanes[0], seg, jj), pair_gen(lanes[1], seg, jj + 1)]
                            alive = [True, True]
                            while any(alive):
                                for gi, g in enumerate(gens):
                                    if alive[gi]:
                                        try:
                                            next(g)
                                        except StopIteration:
                                            alive[gi] = False
                    em.barrier()
                em.stack = stw
            stw.close()
            em.stack = st
            def rr(t):
                return t.cast(F32R) if INV_F32R else t

            def scan_gen(b, j, B, gi):
                a_, c_ = B
                sg = sgs[b]
                KRt = [em.sb([64, 4, 256], BF16, "KRt") for _ in range(2)]
                BKt = [em.sb([64, 4, 256], BF16, "BKt") for _ in range(2)]
                KHt = [em.sb([128, 2, 256], BF16, "KHt") for _ in range(2)]
                Vf = [em.sb([128, 2, 128], BF16, "Vf") for _ in range(2)]
                gC = em.sb([64, 4, NTT], F32, "gC")
                for d in range(2):
                    em.dma(gC[:, d * 2:d * 2 + 2, :], GCD[b, d, j].re("(h c) n -> c h n", h=2))
                TM = em.sb([128, 6, 128], BF16, "TM")
                A1 = em.sb([128, 4, 256], BF16, "A1")
                A2 = em.sb([128, 4, 256], BF16, "A2")
                PY = em.sb([128, 4, 2, 128], F32, "PY")
                Pq = PY[:, :, 0, :]
                Yq = PY[:, :, 1, :]
                Qq = em.sb([128, 4, 128], F32, "Qq")
                TTb = em.sb([128, 4, 128], BF16, "TTb")
                RH = em.sb([128, 4, 64], BF16, "RH")
                Ub = em.sb([128, 4, 64], BF16, "Ub")
                Zf = em.sb([64, 4, 64], F32, "Zf")
                Zb = em.sb([64, 4, 64], BF16, "Zb")
                ztmp = em.sb([64, 4, 64], F32, "ztmp")
                ytm = em.sb([128, NTT, 128], F32, "ytm")
                em.memset(Zf, 0.0)
                em.memset(Zb, 0.0)
                em.memset(ytm, 0.0, e="pool")
                e0 = "dve"
                e1 = "act"
                v4 = lambda t: t.re("p (a b) -> p a b", a=4)
                v2 = lambda t: t.re("p (a b) -> p a b", a=2)

                def load_step(s):
                    sl = s % 2
                    for d in range(2):
                        n = s if d == 0 else REV_ORDER[s]
                        em.dma(KRt[sl][:, d * 2:d * 2 + 2, :], KRD[b, d, j, :, n].re("(h c) s t -> c h (s t)", h=2))
                        em.dma(BKt[sl][:, d * 2:d * 2 + 2, :], BKD[b, d, j, :, n].re("(h c) s t -> c h (s t)", h=2))
                        em.dma(KHt[sl][:, d, :], KHD[b, d, j, :, n].re("p s t -> p (s t)"))
                        em.dma(Vf[sl][:, d, :], VFD[b, j, :, n * 128:(n + 1) * 128])

                def mmA(bank, d, lo):
                    for hp in range(2):
                        u = d * 2 + hp
                        em.mm(bank[:, hp * 256:(hp + 1) * 256], BKt[sl][:, u, lo:lo + 128], KRt[sl][:, u, :])

                load_step(0)
                yield
                for s in range(NTT):
                    sl = s % 2
                    if s + 1 < NTT:
                        load_step(s + 1)
                    ns = [s, REV_ORDER[s]]
                    pt = a_.cast(BF16)
                    for d in range(2):
                        em.tr(pt[:, (2 * d) * 128:(2 * d + 1) * 128], KHt[sl][:, d, 0:128], identb)
                        em.tr(pt[:, (2 * d + 1) * 128:(2 * d + 2) * 128], KHt[sl][:, d, 128:256], identb)
                        em.tr(pt[:, (4 + d) * 128:(5 + d) * 128], Vf[sl][:, d, :], identb)
                    mmA(c_, 0, 0)
                    yield
                    em.copy(TM, pt[:, 0:768].re("p (a b) -> p a b", a=6), e="act")
                    em.tt(A1[:, 0:2, 128:256], v2(c_)[:, :, 128:256], maskA[:, 0:1, 128:256].bc([128, 2, 128]), ALU.mult)
                    em.tt(rr(Pq[:, 0:2, :]), v2(c_)[:, :, 0:128], maskA32[:, 0:1, :].bc([128, 2, 128]), ALU.mult)
                    mmA(a_, 1, 0)
                    for u in range(4):
                        em.mm(c_[:, u * 128:(u + 1) * 128], KRt[sl][:, u, 0:128], BKt[sl][:, u, 0:128])
                    yield
                    em.tt(A1[:, 2:4, 128:256], v2(a_)[:, :, 128:256], maskA[:, 1:2, 128:256].bc([128, 2, 128]), ALU.mult)
                    em.tt(rr(Pq[:, 2:4, :]), v2(a_)[:, :, 0:128], maskA32[:, 1:2, :].bc([128, 2, 128]), ALU.mult)
                    em.tt(rr(Qq), v4(c_), maskN32, ALU.mult)
                    em.tt(rr(Yq), Pq, T(ident.ap.unsqueeze(1).to_broadcast([128, 4, 128]), ident.buf), ALU.add)
                    mmA(a_, 0, 128)
                    mmA(c_, 1, 128)
                    yield
                    em.tt(A2[:, 0:2, :], v2(a_), maskA[:, 0:1, :].bc([128, 2, 256]), ALU.mult)
                    em.tt(A2[:, 2:4, :], v2(c_), maskA[:, 1:2, :].bc([128, 2, 256]), ALU.mult)
                    for u in range(4):
                        em.mm(a_[:, u * 128:(u + 1) * 128], rr(Qq[:, u, :]), rr(Pq[:, u, :]))
                    for u in range(4):
                        em.mm(c_[:, u * 128:(u + 1) * 128], rr(Pq[:, u, :]), rr(Qq[:, u, :]))
                    yield
                    em.copy(rr(Pq), v4(a_), e=e0)
                    em.copy(rr(Qq), v4(c_), e=e1)
                    for lev in range(1, 6):
                        for u in (0, 1):
                            em.mm(a_[:, u * 256:(u + 1) * 256], rr(Qq[:, u, :]), rr(PY[:, u, :, :]))
                        for u in range(4):
                            em.mm(c_[:, u * 128:(u + 1) * 128], rr(Pq[:, u, :]), rr(Qq[:, u, :]))
                        yield
                        a3 = a_.re("p (u s t) -> p u s t", u=2, s=2)
                        em.tt(rr(Yq[:, 0:2, :]), a3[:, :, 1, :], Yq[:, 0:2, :], ALU.add)
                        em.copy(rr(Pq[:, 0:2, :]), a3[:, :, 0, :], e=e0)
                        for u in (2, 3):
                            em.mm(a_[:, (u - 2) * 256:(u - 1) * 256], rr(Qq[:, u, :]), rr(PY[:, u, :, :]))
                        yield
                        em.tt(rr(Yq[:, 2:4, :]), a3[:, :, 1, :], Yq[:, 2:4, :], ALU.add)
                        em.copy(rr(Pq[:, 2:4, :]), a3[:, :, 0, :], e=e0)
                        em.copy(rr(Qq), v4(c_), e=e1)
                    for u in range(4):
                        em.mm(a_[:, u * 128:(u + 1) * 128], rr(Qq[:, u, :]), rr(Yq[:, u, :]))
                    yield
                    em.tt(rr(Yq), v4(a_), Yq, ALU.add)
                    em.copy(TTb, Yq, e="act")
                    for u in range(4):
                        d, hp = u // 2, u % 2
                        vs = TM[:, 4 + d, hp * 64:(hp + 1) * 64]
                        em.mm(c_[:, u * 64:(u + 1) * 64], KRt[sl][:, u, 0:128], Zb[:, u, :], start=True, stop=False)
                        em.mm(c_[:, u * 64:(u + 1) * 64], A2[:, u, 0:128], vs, start=False, stop=True)
                    yield
                    em.copy(RH, v4(c_[:, 0:256]), e="act")
                    for u in range(4):
                        em.mm(a_[:, u * 64:(u + 1) * 64], TTb[:, u, :], RH[:, u, :])
                    yield
                    em.copy(Ub, v4(a_[:, 0:256]), e="act")
                    for u in range(4):
                        d, hp = u // 2, u % 2
                        vs = TM[:, 4 + d, hp * 64:(hp + 1) * 64]
                        yo = c_[:, u * 64:(u + 1) * 64]
                        em.mm(yo, KRt[sl][:, u, 128:256], Zb[:, u, :], start=True, stop=False)
                        em.mm(yo, A2[:, u, 128:256], vs, start=False, stop=False)
                        em.mm(yo, A1[:, u, 128:256], Ub[:, u, :], start=False, stop=True)
                        zo = a_[0:64, 256 + u * 64:256 + (u + 1) * 64]
                        em.mm(zo, TM[:, 2 * d, hp * 64:(hp + 1) * 64], vs, start=True, stop=False)
                        em.mm(zo, TM[:, 2 * d + 1, hp * 64:(hp + 1) * 64], Ub[:, u, :], start=False, stop=True)
                    yield
                    for d in range(2):
                        n = ns[d]
                        em.tt(ytm[:, n, :], ytm[:, n, :], c_[:, d * 128:(d + 1) * 128], ALU.add)
                        em.tt(ztmp[:, d * 2:d * 2 + 2, :], Zf[:, d * 2:d * 2 + 2, :],
                              gC[:, d * 2:d * 2 + 2, n:n + 1].bc([64, 2, 64]), ALU.mult)
                    em.tt(Zf, ztmp, v4(a_[0:64, 256:512]), ALU.add)
                    em.copy(Zb, Zf, e="act")
                st_s = em.sb([128, NTT * 2], F32, "st_s")
                st_q = em.sb([128, NTT * 2], F32, "st_q")
                msq = em.sb([128, NTT * 2], F32, "msq")
                fin = em.sb([128, 512], F32, "fin")
                bon = em.sb([128, 512], F32, "bon")
                rwc = em.sb([128, 512], BF16, "rwc")
                y4 = ytm.re("p n (h v) -> p (n h) v", h=2)
                em.reduce(st_s, y4, ALU.add)
                for q4 in range(0, NTT, 4):
                    nt = min(4, NTT - q4)
                    f3 = fin[:, 0:nt * 128].re("p (n c) -> p n c", c=128)
                    em.tt(f3, ytm[:, q4:q4 + nt, :], ytm[:, q4:q4 + nt, :], ALU.mult)
                    em.reduce(st_q[:, q4 * 2:(q4 + nt) * 2], fin[:, 0:nt * 128].re("p (n v) -> p n v", v=64), ALU.add)
                yield
                em.ts(st_s, st_s, 1.0 / 64, None, ALU.mult)
                em.ts(st_q, st_q, 1.0 / 64, None, ALU.mult)
                em.tt(msq, st_s, st_s, ALU.mult)
                em.tt(st_q, st_q, msq, ALU.subtract)
                em.ts(st_q, st_q, GN_EPS, None, ALU.add)
                em.act(st_q, st_q, AF.Sqrt)
                em.recip(st_q, st_q)
                yield
                em.tt(y4, y4, T(st_s.ap.unsqueeze(2).to_broadcast([128, NTT * 2, 64]), st_s.buf), ALU.subtract)
                em.tt(y4, y4, T(st_q.ap.unsqueeze(2).to_broadcast([128, NTT * 2, 64]), st_q.buf), ALU.mult)
                yield
                for q4 in range(0, NTT, 4):
                    nt = min(4, NTT - q4)
                    w = nt * 128
                    tsl = slice(q4 * 128, q4 * 128 + w)
                    em.dma(bon[:, 0:w], BOND[b, j, :, tsl])
                    for i in range(nt):
                        em.tr(a_[:, i * 128:(i + 1) * 128], ytm[:, q4 + i, :], ident)
                    em.mm(c_[:, 0:w], g2b[:, j * 128:(j + 1) * 128], sg[:, tsl])
                    yield
                    em.ts(fin[:, 0:w], a_[:, 0:w], vc[:, 62 + j:63 + j], vc[:, 66 + j:67 + j], ALU.mult, ALU.add)
                    em.tt(fin[:, 0:w], fin[:, 0:w], bon[:, 0:w], ALU.add)
                    em.tt(rwc[:, 0:w], fin[:, 0:w], c_[:, 0:w], ALU.mult)
                    em.dma(mixD[b, :, 4 + j, tsl], rwc[:, 0:w])

            for jj in range(0, 4 if rw_stage >= 2 else 0, 2):
                with ExitStack() as st3:
                    em.stack = st3
                    gens = []
                    for gi, (b_, j_) in enumerate(((0, jj), (1, jj), (0, jj + 1), (1, jj + 1))):
                        gens.append(scan_gen(b_, j_, (ps[2 * gi], ps[2 * gi + 1]), gi))
                    alive = [True] * len(gens)
                    while any(alive):
                        for gi, g in enumerate(gens):
                            if alive[gi]:
                                try:
                                    next(g)
                                except StopIteration:
                                    alive[gi] = False
                    em.barrier()
                em.stack = st
            em.barrier()
        em.stack = pstack

    def post_phase(l, b):
        G_all = G_all2[:, b]
        with ExitStack() as st:
            em.stack = st
            ntiles = NTT if l == 0 else NTT
            t_lo = 0 if l == 0 else 2
            wo = em.sb([128, 8, D], BF16, "wo")
            em.dma(wo, w_out[l].re("(k p) n -> p k n", p=128), q="pool")
            rw32 = em.sb([128, 8, 32], F32, "rw32")
            em.dma(rw32, router_w.re("(k p) n -> p k n", p=128))
            rb = em.sb([128, 32], F32, "rb")
            bcast_row(rb, rbias)
            g1 = em.sb([128, 2, D], F32, "g1")
            sc2 = em.sb([128, 2, D], F32, "sc2")
            sh2 = em.sb([128, 2, D], F32, "sh2")
            for w, row in enumerate((2, b)):
                bcast_row(g1[:, w, :], modD[l, row, 2 * D:3 * D])
                bcast_row(sh2[:, w, :], modD[l, row, 3 * D:4 * D])
                bcast_row(sc2[:, w, :], modD[l, row, 4 * D:5 * D])
            em.ts(sc2, sc2, 1.0, None, ALU.add)
            lg = em.sb([128, D], F32, "lg")
            lb = em.sb([128, D], F32, "lb")
            bcast_row(lg, lnv[l, 0])
            bcast_row(lb, lnv[l, 1])
            PAIRS = [(0, 1), (0, 2), (0, 3), (1, 2), (1, 3), (2, 3)]
            NLANE = 4

            def lane_gen(li, B):
                m = em.sb([128, 8, 128], BF16, "mt")
                xx = em.sb([128, D], F32, "xt")
                u_ = em.sb([128, D], F32, "u_")
                h2 = em.sb([128, D], F32, "h2")
                h2f = em.sb([128, 8, 128], F32, "h2f")
                hb = em.sb([128, 8, 128], BF16, "h2b")
                h2t = em.sb([128, D], BF16, "h2t")
                stt_ = em.sb([128, 2, 6], F32, "bst")
                mv = em.sb([128, 2], F32, "mv")
                rstd = em.sb([128, 1], F32, "rstd")
                sc_ = em.sb([128, 32], F32, "sc_")
                sel = em.sb([128, 32], F32, "sel")
                p6 = em.sb([128, 8, 6], F32, "p6")
                m6 = em.sb([128, 8, 6], F32, "m6")
                gs = em.sb([128, 8], F32, "gs")
                sec = em.sb([128, 8], F32, "sec")
                gmx = em.sb([128, 1], F32, "gmx")
                gmk = em.sb([128, 8], F32, "gmk")
                emk = em.sb([128, 32], F32, "emk")
                gsum = em.sb([128, 1], F32, "gsum")
                for tt in range(t_lo + li, NTT, NLANE):
                    w = 0 if tt < 2 else 1
                    tsl = slice(tt * 128, (tt + 1) * 128)
                    em.dma(m, mixD[b, :, :, tsl])
                    em.dma(xx, seq_tile(l, b, tt))
                    for half in range(2):
                        for k in range(8):
                            em.mm(B[half], m[:, k, :], wo[:, k, half * 512:(half + 1) * 512], start=(k == 0), stop=(k == 7))
                    yield
                    for half in range(2):
                        em.tt(u_[:, half * 512:(half + 1) * 512], B[half], g1[:, w, half * 512:(half + 1) * 512], ALU.mult)
                    em.stt(u_, xx, ALPHA, u_, ALU.mult, ALU.add)
                    for half in range(2):
                        em.bn_stats(stt_[:, half, :], u_[:, half * 512:(half + 1) * 512])
                    em.bn_aggr(mv, stt_.re("p a b -> p (a b)"))
                    em.ts(rstd, mv[:, 1:2], LN_EPS, None, ALU.add)
                    em.act(rstd, rstd, AF.Sqrt)
                    yield
                    em.recip(rstd, rstd)
                    em.ts(u_, u_, mv[:, 0:1], rstd, ALU.subtract, ALU.mult)
                    em.tt(u_, u_, lg, ALU.mult)
                    em.tt(u_, u_, lb, ALU.add)
                    em.dma(x1D[b, tsl, :], u_)
                    em.tt(h2, u_, sc2[:, w, :], ALU.mult, e="pool")
                    em.tt(h2, h2, sh2[:, w, :], ALU.add, e="pool")
                    if SPARSE_MOE:
                        em.copy(h2t, h2, e="pool")
                        em.dma(h2tokD[b * TT + tt * 128:b * TT + (tt + 1) * 128, :], h2t)
                    yield
                    for half in range(2):
                        pp = B[half]
                        for kk in range(4):
                            k = half * 4 + kk
                            em.tr(pp[:, kk * 128:(kk + 1) * 128], h2[:, k * 128:(k + 1) * 128], ident)
                    yield
                    for half in range(2):
                        em.copy(h2f[:, half * 4:(half + 1) * 4, :], B[half].re("p (a b) -> p a b", a=4), e="act")
                    em.copy(hb, h2f, e="pool")
                    em.dma(h2D[b, :, :, tsl], hb)
                    lgt = B[0][:, 0:32]
                    for k in range(8):
                        em.mm(lgt, h2f[:, k, :], rw32[:, k, :], start=(k == 0), stop=(k == 7))
                    yield
                    em.act(sc_, lgt, AF.Sigmoid)
                    em.tt(sel, sc_, rb, ALU.add)
                    s3 = sel.re("p (g e) -> p g e", e=4)
                    for pi, (i0_, i1_) in enumerate(PAIRS):
                        em.tt(p6[:, :, pi], s3[:, :, i0_], s3[:, :, i1_], ALU.add)
                        em.tt(m6[:, :, pi], s3[:, :, i0_], s3[:, :, i1_], ALU.min)
                    em.reduce(gs, p6, ALU.max)
                    em.reduce(sec, m6, ALU.max)
                    em.reduce(gmx, gs, ALU.max)
                    em.ts(gmk, gs, gmx, None, ALU.is_ge)
                    e3 = emk.re("p (g e) -> p g e", e=4)
                    em.tt(e3, s3, T(sec.ap.unsqueeze(2).to_broadcast([128, 8, 4]), sec.buf), ALU.is_ge)
                    em.tt(e3, e3, T(gmk.ap.unsqueeze(2).to_broadcast([128, 8, 4]), gmk.buf), ALU.mult)
                    em.tt(emk, emk, sc_, ALU.mult)
                    em.reduce(gsum, emk, ALU.add)
                    em.recip(gsum, gsum)
                    em.ts(G_all[:, tt, :], emk, gsum, None, ALU.mult)
                    yield

            gens = [lane_gen(li, ps[2 * li:2 * li + 2]) for li in range(NLANE)]
            alive = [True] * NLANE
            for li in range(1, NLANE):
                for _ in range(2 * li):
                    pass
            while any(alive):
                for gi, g in enumerate(gens):
                    if alive[gi]:
                        try:
                            next(g)
                        except StopIteration:
                            alive[gi] = False
            em.barrier()
        em.stack = pstack

    def moe_phase(l, b):
        G_all = G_all2[:, b]
        with ExitStack() as st:
            em.stack = st
            t_lo = 0 if l == 0 else 2
            tok0 = t_lo * 128
            NTOK = TT - tok0
            hT2 = em.sb([128, 8, TT], BF16, "hT2")
            em.dma(hT2[:, :, tok0:], h2D[b, :, :, tok0:])
            yacc = em.sb([128, NTT, D], F32, "yacc")
            em.memset(yacc[:, :, 0:512], 0.0)
            em.memset(yacc[:, :, 512:1024], 0.0, e="pool")
            w1b = [em.sb([128, 8, 512], BF16, "w1b") for _ in range(2)]
            w3b = [em.sb([128, 8, 512], BF16, "w3b") for _ in range(2)]
            w2b_ = [em.sb([128, 4, D], BF16, "w2b_") for _ in range(2)]
            sil = [em.sb([128, 512], BF16, "sil") for _ in range(2)]
            actT = [em.sb([128, 4, 512], BF16, "actT") for _ in range(2)]

            def load_w(e):
                sl = e % 2
                em.dma(w1b[sl], ew1[l, e].re("(k p) n -> p k n", p=128), q="pool")
                em.dma(w3b[sl], ew3[l, e].re("(k p) n -> p k n", p=128), q="pool")
                em.dma(w2b_[sl], ew2[l, e].re("(k p) n -> p k n", p=128), q="pool")

            load_w(0)
            it = 0
            for e in range(32):
                sl = e % 2
                if e + 1 < 32:
                    load_w(e + 1)
                for t0 in range(tok0, TT, 512):
                    w = min(512, TT - t0)
                    aT = actT[it % 2]
                    for f in range(4):
                        pa = ps[(it * 4 + f) % 2]
                        pb_ = ps[2 + (it * 4 + f) % 2]
                        for k in range(8):
                            em.mm(pa[:, 0:w], w1b[sl][:, k, f * 128:(f + 1) * 128], hT2[:, k, t0:t0 + w], start=(k == 0), stop=(k == 7))
                        for k in range(8):
                            em.mm(pb_[:, 0:w], w3b[sl][:, k, f * 128:(f + 1) * 128], hT2[:, k, t0:t0 + w], start=(k == 0), stop=(k == 7))
                        sb_ = sil[f % 2]
                        em.act(sb_[:, 0:w], pa[:, 0:w], AF.Silu)
                        em.tt(aT[:, f, 0:w], sb_[:, 0:w], pb_[:, 0:w], ALU.mult)
                    for sub in range(w // 128):
                        tt = (t0 + sub * 128) // 128
                        for half in range(2):
                            po = ps[4 + (sub * 2 + half) % 4]
                            for f in range(4):
                                em.mm(po, aT[:, f, sub * 128:(sub + 1) * 128], w2b_[sl][:, f, half * 512:(half + 1) * 512],
                                      start=(f == 0), stop=(f == 3))
                            ya = yacc[:, tt, half * 512:(half + 1) * 512]
                            em.stt(ya, po, G_all[:, tt, e:e + 1], ya, ALU.mult, ALU.add)
                    it += 1
            g2r = em.sb([128, 2, D], F32, "g2r")
            for w, row in enumerate((2, b)):
                bcast_row(g2r[:, w, :], modD[l, row, 5 * D:6 * D])
            lg = em.sb([128, D], F32, "lg2")
            lb = em.sb([128, D], F32, "lb2")
            bcast_row(lg, lnv[l, 2])
            bcast_row(lb, lnv[l, 3])
            xt = [em.sb([128, D], F32, "xt2") for _ in range(2)]
            stt_ = em.sb([128, 2, 6], F32, "bst2")
            mv = em.sb([128, 2], F32, "mv2")
            rstd = em.sb([128, 1], F32, "rstd2")
            for tt in range(t_lo, NTT):
                w = 0 if tt < 2 else 1
                xx = xt[tt % 2]
                tsl = slice(tt * 128, (tt + 1) * 128)
                em.dma(xx, x1D[b, tsl, :])
                u_ = yacc[:, tt, :]
                em.tt(u_, u_, g2r[:, w, :], ALU.mult)
                em.stt(u_, xx, ALPHA, u_, ALU.mult, ALU.add)
                for half in range(2):
                    em.bn_stats(stt_[:, half, :], u_[:, half * 512:(half + 1) * 512])
                em.bn_aggr(mv, stt_.re("p a b -> p (a b)"))
                em.ts(rstd, mv[:, 1:2], LN_EPS, None, ALU.add)
                em.act(rstd, rstd, AF.Sqrt)
                em.recip(rstd, rstd)
                em.ts(u_, u_, mv[:, 0:1], rstd, ALU.subtract, ALU.mult)
                em.tt(u_, u_, lg, ALU.mult)
                em.tt(xx, u_, lb, ALU.add)
                if l == L - 1:
                    em.dma(y_out[b, (tt - 2) * 128:(tt - 1) * 128, :], xx)
                else:
                    em.dma(sD[b, tsl, :], xx)
            em.barrier()
        em.stack = pstack

    def moe_sparse(l, stage=9):
        NT = 2 * NTT
        with ExitStack() as st:
            em.stack = st
            Gf = G_all2.re("p b t e -> p (b t) e")
            ones32 = em.sb([128, 128], F32, "ones32")
            em.memset(ones32, 1.0)
            lt32 = em.sb([128, 128], F32, "lt32")
            em.dma(lt32, c_maskA[:, 0, 0:128])
            thr = em.sb([128, 128], F32, "thr")
            em.dma(thr, c_thr)
            kp = em.sb([128, 8], F32, "kp")
            em.dma(kp, c_kp)
            glo = em.sb([128, NT], F32, "glo")
            ghi = em.sb([128, NT], F32, "ghi")
            dli = em.sb([128, NT], I32, "dli")
            dhi_i = em.sb([128, NT], I32, "dhi_i")
            widx = em.sb([128, NBLK], I32, "widx")
            with ExitStack() as st2:
                em.stack = st2
                m = em.sb([128, NT, 32], F32, "m")
                em.ts(m, Gf, 0.0, None, ALU.is_gt)
                if l == 1:
                    for b in range(2):
                        em.memset(m[:, b * NTT:b * NTT + 2, :], 0.0)
                rank = em.sb([128, NT, 32], F32, "rank")
                cnt = em.sb([128, 32], F32, "cnt")
                em.memset(cnt, 0.0)
                for i in range(NT):
                    em.mm(ps[i // 16][:, (i % 16) * 32:(i % 16 + 1) * 32], lt32, m[:, i, :])
                    em.mm(ps[3 + i // 16][:, (i % 16) * 32:(i % 16 + 1) * 32], ones32, m[:, i, :])
                csum = em.sb([128, NT, 32], F32, "csum")
                for bk in range(3):
                    n_ = min(16, NT - bk * 16)
                    em.copy(rank[:, bk * 16:bk * 16 + n_, :], ps[bk][:, 0:n_ * 32].re("p (a b) -> p a b", b=32), e="act")
                    em.copy(csum[:, bk * 16:bk * 16 + n_, :], ps[3 + bk][:, 0:n_ * 32].re("p (a b) -> p a b", b=32), e="dve")
                for i in range(NT):
                    if i > 0:
                        em.tt(rank[:, i, :], rank[:, i, :], cnt, ALU.add)
                    em.tt(cnt, cnt, csum[:, i, :], ALU.add)
                cmp = em.sb([128, 32, 40], F32, "cmp")
                em.tt(cmp, T(cnt.ap.unsqueeze(2).to_broadcast([128, 32, 40]), cnt.buf),
                      T(thr.ap[:, 0:40].unsqueeze(1).to_broadcast([128, 32, 40]), thr.buf), ALU.is_gt)
                nblk = em.sb([128, 32], F32, "nblk")
                em.reduce(nblk, cmp, ALU.add)
                incl = em.sb([128, 32], F32, "incl")
                em.scan(incl, ones32[:, 0:32], nblk, 0.0, ALU.mult, ALU.add)
                pstart = em.sb([128, 32], F32, "pstart")
                pend = em.sb([128, 32], F32, "pend")
                em.tt(pstart, incl, nblk, ALU.subtract)
                em.ts(pstart, pstart, float(BS), None, ALU.mult)
                em.ts(pend, incl, float(BS), None, ALU.mult)
                dest = em.sb([128, NT, 32], F32, "dest")
                em.tt(dest, rank, T(pstart.ap.unsqueeze(1).to_broadcast([128, NT, 32]), pstart.buf), ALU.add)
                tmpm = em.sb([128, NT, 32], F32, "tmpm")
                dlo = em.sb([128, NT], F32, "dlo")
                dhi = em.sb([128, NT], F32, "dhi")
                em.ts(tmpm, m, -1.0e6, 1.0e6, ALU.mult, ALU.add)
                em.tt(tmpm, tmpm, dest, ALU.add)
                em.reduce(dlo, tmpm, ALU.min)
                em.tt(tmpm, dest, m, ALU.mult)
                em.tt(tmpm, tmpm, m, ALU.add)
                em.ts(tmpm, tmpm, -1.0, None, ALU.add)
                em.reduce(dhi, tmpm, ALU.max)
                em.tt(tmpm, dest, T(dlo.ap.unsqueeze(2).to_broadcast([128, NT, 32]), dlo.buf), ALU.is_equal)
                em.tt(tmpm, tmpm, Gf, ALU.mult)
                em.reduce(glo, tmpm, ALU.add)
                em.tt(tmpm, dest, T(dhi.ap.unsqueeze(2).to_broadcast([128, NT, 32]), dhi.buf), ALU.is_equal)
                em.tt(tmpm, tmpm, Gf, ALU.mult)
                em.reduce(ghi, tmpm, ALU.add)
                em.copy(dli, dlo)
                em.copy(dhi_i, dhi)
                cmpb = em.sb([128, NBLK, 32], F32, "cmpb")
                em.tt(cmpb, T(pend.ap.unsqueeze(1).to_broadcast([128, NBLK, 32]), pend.buf),
                      T(thr.ap[:, 0:NBLK].unsqueeze(2).to_broadcast([128, NBLK, 32]), thr.buf), ALU.is_le)
                be = em.sb([128, NBLK], F32, "be")
                em.reduce(be, cmpb, ALU.add)
                em.ts(be, be, 31.0, None, ALU.min)
                em.ts(be, be, 128.0, float(l * 32 * 128), ALU.mult, ALU.add)
                em.tt(be, be, T(kp.ap[:, 0:1].to_broadcast([128, NBLK]), kp.buf), ALU.add)
                em.copy(widx, be)
                xtk = [em.sb([128, D], BF16, "xtk") for _ in range(2)]
                for i in range(NT):
                    if l == 1 and (i % NTT) < 2:
                        continue
                    xk = xtk[i % 2]
                    em.dma(xk, h2tokD[i * 128:(i + 1) * 128, :])
                    em.idma(xbD, xk, dli[:, i:i + 1], True, NBLK * BS - 1)
                    em.idma(xbD, xk, dhi_i[:, i:i + 1], True, NBLK * BS - 1)
                em.barrier()
            em.stack = st
            if stage < 2:
                return
            NS = BS // 128
            with ExitStack() as st3:
                em.stack = st3
                NLN = 2

                def blk_lane(li, B):
                    w1s = [em.sb([128, 8, 512], BF16, "w1s") for _ in range(2)]
                    w3s = [em.sb([128, 8, 512], BF16, "w3s") for _ in range(2)]
                    w2s = [em.sb([128, 4, D], BF16, "w2s") for _ in range(2)]
                    xblk = [em.sb([128, NS, D], BF16, "xblk") for _ in range(2)]
                    xT = em.sb([128, 8, BS], BF16, "xT")
                    sil = em.sb([128, BS], BF16, "sil")
                    aT = em.sb([128, 4, BS], BF16, "aT")
                    ysub = [em.sb([128, D], F32, "ysub") for _ in range(2)]
                    blks = list(range(li, NBLK, NLN))

                    def load_blk(n):
                        blk = blks[n]
                        sl = n % 2
                        em.idma(w1s[sl].re("p k n -> p (k n)"), ew1, widx[:, blk:blk + 1], False, 0)
                        em.idma(w3s[sl].re("p k n -> p (k n)"), ew3, widx[:, blk:blk + 1], False, 0)
                        em.idma(w2s[sl].re("p k n -> p (k n)"), ew2, widx[:, blk:blk + 1], False, 0)
                        em.dma(xblk[sl], xbD[blk * BS:(blk + 1) * BS, :].re("(s p) n -> p s n", p=128))

                    load_blk(0)
                    yc = 0
                    for n, blk in enumerate(blks):
                        sl = n % 2
                        if n + 1 < len(blks):
                            load_blk(n + 1)
                        w1b, w3b, w2b_ = w1s[sl], w3s[sl], w2s[sl]
                        for s_ in range(NS):
                            ptb = B[s_ % 2].cast(BF16)
                            for k in range(8):
                                em.tr(ptb[:, k * 128:(k + 1) * 128], xblk[sl][:, s_, k * 128:(k + 1) * 128], identb)
                            em.copy(xT[:, :, s_ * 128:(s_ + 1) * 128], ptb.re("p (a b) -> p a b", a=8), e=("dve" if s_ % 2 else "act"))
                            if s_ % 2:
                                yield
                        for f in range(4):
                            pa = B[2]
                            pb_ = B[3]
                            for k in range(8):
                                em.mm(pa, w1b[:, k, f * 128:(f + 1) * 128], xT[:, k, :], start=(k == 0), stop=(k == 7))
                            for k in range(8):
                                em.mm(pb_, w3b[:, k, f * 128:(f + 1) * 128], xT[:, k, :], start=(k == 0), stop=(k == 7))
                            yield
                            em.act(sil, pa, AF.Silu)
                            em.tt(aT[:, f, :], sil, pb_, ALU.mult)
                        for s_ in range(NS):
                            yb_ = ysub[yc % 2]
                            yc += 1
                            for half in range(2):
                                po = B[half]
                                for f in range(4):
                                    em.mm(po, aT[:, f, s_ * 128:(s_ + 1) * 128], w2b_[:, f, half * 512:(half + 1) * 512],
                                          start=(f == 0), stop=(f == 3))
                            yield
                            for half in range(2):
                                em.copy(yb_[:, half * 512:(half + 1) * 512], B[half], e=("act" if half else "dve"))
                            em.dma(ybD[blk * BS + s_ * 128:blk * BS + (s_ + 1) * 128, :], yb_)

                gens = [blk_lane(li, ps[4 * li:4 * li + 4]) for li in range(NLN)]
                alive = [True] * NLN
                while any(alive):
                    for gi, g in enumerate(gens):
                        if alive[gi]:
                            try:
                                next(g)
                            except StopIteration:
                                alive[gi] = False
                em.barrier()
            em.stack = st
            if stage < 3:
                return
            with ExitStack() as st4:
                em.stack = st4
                g2r = em.sb([128, 3, D], F32, "g2r")
                for w, row in enumerate((2, 0, 1)):
                    bcast_row(g2r[:, w, :], modD[l, row, 5 * D:6 * D])
                lg = em.sb([128, D], F32, "lg2")
                lb = em.sb([128, D], F32, "lb2")
                bcast_row(lg, lnv[l, 2])
                bcast_row(lb, lnv[l, 3])
                xt = [em.sb([128, D], F32, "xt2") for _ in range(2)]
                yl = [em.sb([128, D], F32, "yl") for _ in range(2)]
                yh = [em.sb([128, D], F32, "yh") for _ in range(2)]
                stt_ = em.sb([128, 2, 6], F32, "bst2")
                mv = em.sb([128, 2], F32, "mv2")
                rstd = em.sb([128, 1], F32, "rstd2")
                for i in range(NT):
                    b, tt = i // NTT, i % NTT
                    if l == 1 and tt < 2:
                        continue
                    w = 0 if tt < 2 else 1 + b
                    xx = xt[i % 2]
                    ylo_ = yl[i % 2]
                    yhi_ = yh[i % 2]
                    tsl = slice(tt * 128, (tt + 1) * 128)
                    em.dma(xx, x1D[b, tsl, :])
                    em.idma(ylo_, ybD, dli[:, i:i + 1], False, 0)
                    em.idma(yhi_, ybD, dhi_i[:, i:i + 1], False, 0)
                    u_ = ylo_
                    em.ts(ylo_, ylo_, glo[:, i:i + 1], None, ALU.mult)
                    em.stt(u_, yhi_, ghi[:, i:i + 1], ylo_, ALU.mult, ALU.add)
                    em.tt(u_, u_, g2r[:, w, :], ALU.mult)
                    em.stt(u_, xx, ALPHA, u_, ALU.mult, ALU.add)
                    for half in range(2):
                        em.bn_stats(stt_[:, half, :], u_[:, half * 512:(half + 1) * 512])
                    em.bn_aggr(mv, stt_.re("p a b -> p (a b)"))
                    em.ts(rstd, mv[:, 1:2], LN_EPS, None, ALU.add)
                    em.act(rstd, rstd, AF.Sqrt)
                    em.recip(rstd, rstd)
                    em.ts(u_, u_, mv[:, 0:1], rstd, ALU.subtract, ALU.mult)
                    em.tt(u_, u_, lg, ALU.mult)
                    em.tt(xx, u_, lb, ALU.add)
                    if l == L - 1:
                        em.dma(y_out[b, (tt - 2) * 128:(tt - 1) * 128, :], xx)
                    else:
                        em.dma(sD[b, tsl, :], xx)
                em.barrier()
        em.stack = pstack

    for l in range(nlayers):
        if want("adaln"):
            adaln(l)
        if want("na"):
            na_phase(l)
        if want("rwkv"):
            rwkv_phase(l)
        if SPARSE_MOE:
            for b in range(2):
                if want("post"):
                    post_phase(l, b)
            if want("moe"):
                moe_sparse(l, moe_stage)
        else:
            for b in range(2):
                if want("post"):
                    post_phase(l, b)
                if want("moe"):
                    moe_phase(l, b)
    em.barrier()
    return nc, em


_CONSTS = None


def make_in_maps(inp, ncores=8):
    global _CONSTS
    if _CONSTS is None:
        _CONSTS = _consts_np()
    f = lambda a: np.ascontiguousarray(np.asarray(a, dtype=np.float32))
    shared = {k: f(inp[k]) for k in ("ada_w", "ada_b", "w_in", "rw_w2", "rw_a2", "rw_g2", "w_out", "router_w",
                                     "router_bias")}
    for nm, kc in (("exp_w1", 8), ("exp_w3", 8), ("exp_w2", 4)):
        w = f(inp[nm])
        n = w.shape[-1]
        shared[nm] = np.ascontiguousarray(w.reshape(L, 32, kc, 128, n).transpose(0, 1, 3, 2, 4)).reshape(L * 32 * 128, kc * n)
    shared["vecs"] = _vecs_np(inp)
    shared["gnrow"] = f(np.stack([inp["rw_gn_g"], inp["rw_gn_b"]], 1))
    shared["lnv"] = f(np.stack([inp["ln1_g"], inp["ln1_b"], inp["ln2_g"], inp["ln2_b"]], 1))
    shared["rb"] = _rb_gather(f(inp["na_rpb"]))
    for k, v in _CONSTS.items():
        shared[k] = v
    maps = []
    x = f(inp["x"])
    ctx = f(inp["ctx"])
    c = f(inp["c"])
    cc = f(inp["c_ctx"])
    for i in range(ncores):
        m = dict(shared)
        m["x"] = np.ascontiguousarray(x[2 * i:2 * i + 2])
        m["ctx"] = np.ascontiguousarray(ctx[2 * i:2 * i + 2])
        rows = np.stack([c[2 * i], c[2 * i + 1], cc], 0)
        m["cT"] = np.ascontiguousarray(rows.reshape(3, 8, 128).transpose(2, 1, 0))
        maps.append(m)
    return maps


def kernel(**inputs):
    nc, em = build()
    maps = make_in_maps(inputs, 8)
    res = run_bass_kernel_spmd(nc, maps, core_ids=list(range(8)))
    out = np.concatenate([np.asarray(r["y"], dtype=np.float32) for r in res.results], axis=0)
    return out
```

```python
from contextlib import ExitStack
import numpy as np
import concourse.bass as bass
import concourse.mybir as mybir
from concourse.bass_utils import run_bass_kernel_spmd

F32 = mybir.dt.float32
BF16 = mybir.dt.bfloat16
AF = mybir.ActivationFunctionType
ALU = mybir.AluOpType
AX = mybir.AxisListType

L = 2
D = 1024
S = 2048
CT = 256
TT = S + CT
NTT = TT // 128
DIN = 3296
LAM = float(np.exp(-0.5))
ALPHA = float((2 * L) ** 0.25)
LN_EPS = 1e-6
GN_EPS = 64e-5
NV = 70
DEBUG = False
INV_F32R = True
SPARSE_MOE = True
BS = 512
NBLK = (2 * 2 * TT + BS - 1) // BS + 32
CAST_IDMA = True
I32 = mybir.dt.int32
F32R = mybir.dt.float32r


class Buf:
    __slots__ = ("name", "w", "r")

    def __init__(self, name):
        self.name = name
        self.w = {}
        self.r = []


class T:
    __slots__ = ("ap", "buf")

    def __init__(self, ap, buf):
        self.ap = ap
        self.buf = buf

    def __getitem__(self, idx):
        return T(self.ap[idx], self.buf)

    def re(self, pat, **kw):
        return T(self.ap.rearrange(pat, **kw), self.buf)

    def bc(self, shape):
        return T(self.ap.to_broadcast(shape), self.buf)

    def pbc(self, n):
        return T(self.ap.partition_broadcast(n), self.buf)

    def cast(self, dt):
        return T(self.ap.bitcast(dt), self.buf)


class EM:
    NDS = 12

    def __init__(self, nc):
        self.nc = nc
        self.eng = {"pe": nc.tensor, "dve": nc.vector, "act": nc.scalar, "pool": nc.gpsimd, "sp": nc.sync}
        self.sem = {}
        self.cnt = {}
        self.waited = {e: {} for e in self.eng}
        for e in ("pe", "dve", "act", "pool"):
            self.sem[e] = nc.alloc_semaphore("s_" + e)
            self.cnt[e] = 0
        self.dsem = {}
        self.dcnt = {}
        for q in ("sp", "pool"):
            self.dsem[q] = [nc.alloc_semaphore(f"d_{q}{i}") for i in range(self.NDS)]
            self.dcnt[q] = 0
        self.ntens = 0
        self.ninst = 0
        self.stack = None

    def sb(self, shape, dt=F32, name=None):
        self.ntens += 1
        name = (name or "t") + f"_{self.ntens}"
        h = self.stack.enter_context(self.nc.sbuf_tensor(name, list(shape), dt))
        return T(h.ap(), Buf(name))

    def dram(self, name, shape, dt=F32, kind="Internal"):
        if DEBUG and kind == "Internal":
            kind = "ExternalOutput"
        h = self.nc.dram_tensor(name, list(shape), dt, kind=kind)
        return T(h.ap(), Buf(name))

    def _wait(self, e, tok):
        key = id(tok[0])
        if self.waited[e].get(key, 0) >= tok[1]:
            return
        self.waited[e][key] = tok[1]
        self.eng[e].wait_ge(tok[0], tok[1])
        self.ninst += 1

    def _deps(self, e, reads, writes, is_dma=False):
        for t in reads:
            for tok in t.buf.w.values():
                if e == "pe" and tok[2] == "pe":
                    continue
                self._wait(e, tok)
        for t in writes:
            b = t.buf
            for tok in b.w.values():
                if is_dma and tok[2] == "dma":
                    continue
                if (not is_dma) and tok[2] == e:
                    continue
                self._wait(e, tok)
            for tok in b.r:
                if (not is_dma) and tok[2] == e and e in ("pe", "dve", "act", "pool"):
                    continue
                self._wait(e, tok)

    def _done(self, tok, reads, writes, is_dma=False):
        wb = [t.buf for t in writes]
        for b in wb:
            if is_dma:
                b.w[id(tok[0])] = tok
            else:
                b.w = {id(tok[0]): tok}
            b.r = []
        for t in reads:
            b = t.buf
            if any(b is x for x in wb):
                continue
            b.r.append(tok)
            if len(b.r) > 16:
                best = {}
                for k in b.r:
                    kk = id(k[0])
                    if kk not in best or best[kk][1] < k[1]:
                        best[kk] = k
                b.r = list(best.values())

    def op(self, e, fn, reads=(), writes=()):
        reads = [t for t in reads if isinstance(t, T)]
        writes = [t for t in writes if isinstance(t, T)]
        self._deps(e, reads, writes)
        inst = fn(self.eng[e])
        self.cnt[e] += 1
        inst.then_inc(self.sem[e], 1)
        tok = (self.sem[e], self.cnt[e], e)
        self._done(tok, reads, writes)
        self.ninst += 1
        return tok

    def dma(self, out, in_, q="sp"):
        i = self.dcnt[q]
        self.dcnt[q] += 1
        s = self.dsem[q][i % self.NDS]
        val = 16 * (i // self.NDS + 1)
        if val > 16:
            self._wait(q, (s, val - 16, "dma"))
        self._deps(q, [in_], [out], True)
        self.eng[q].dma_start(out=out.ap, in_=in_.ap).then_inc(s, 16)
        tok = (s, val, "dma")
        self._done(tok, [in_], [out], True)
        self.ninst += 1
        return tok

    def idma(self, out, in_, idx, scatter, bound):
        q = "pool"
        i = self.dcnt[q]
        self.dcnt[q] += 1
        s = self.dsem[q][i % self.NDS]
        val = 16 * (i // self.NDS + 1)
        if val > 16:
            self._wait(q, (s, val - 16, "dma"))
        self._deps(q, [in_, idx], [out], True)
        off = bass.IndirectOffsetOnAxis(ap=idx.ap, axis=0)
        if scatter:
            inst = self.nc.gpsimd.indirect_dma_start(out=out.ap, out_offset=off, in_=in_.ap, in_offset=None)
        else:
            inst = self.nc.gpsimd.indirect_dma_start(out=out.ap, out_offset=None, in_=in_.ap, in_offset=off)
        inst.then_inc(s, 16)
        tok = (s, val, "dma")
        self._done(tok, [in_, idx], [out], True)
        self.ninst += 1
        return tok

    def barrier(self):
        toks = []
        for e in ("pe", "dve", "act", "pool"):
            if self.cnt[e] > 0:
                toks.append((self.sem[e], self.cnt[e], e))
        for q in self.dsem:
            n = self.dcnt[q]
            for j in range(min(n, self.NDS)):
                last = ((n - 1 - j) // self.NDS) if (n - 1 - j) >= 0 else -1
            for si in range(self.NDS):
                k = (n - si + self.NDS - 1) // self.NDS if n > si else 0
                if k > 0:
                    toks.append((self.dsem[q][si], 16 * k, "dma"))
        for e in self.eng:
            for tok in toks:
                if tok[2] == e and e in ("pe", "dve", "act"):
                    continue
                self._wait(e, tok)

    def mm(self, out, lhsT, rhs, start=True, stop=True):
        return self.op("pe", lambda g: g.matmul(out.ap, lhsT.ap, rhs.ap, start=start, stop=stop, skip_group_check=True),
                       [lhsT, rhs], [out])

    def tr(self, out, in_, ident):
        return self.op("pe", lambda g: g.transpose(out.ap, in_.ap, ident.ap), [in_, ident], [out])

    def act(self, out, in_, func, bias=None, scale=None):
        kw = {}
        rd = [in_]
        if bias is not None:
            kw["bias"] = bias.ap if isinstance(bias, T) else bias
            rd.append(bias)
        if scale is not None:
            kw["scale"] = scale.ap if isinstance(scale, T) else scale
            rd.append(scale)
        return self.op("act", lambda g: g.activation(out.ap, in_.ap, func, **kw), rd, [out])

    def tt(self, out, a, b, op, e="dve"):
        return self.op(e, lambda g: g.tensor_tensor(out.ap, a.ap, b.ap, op), [a, b], [out])

    def ts(self, out, a, s1, s2=None, op0=ALU.mult, op1=None, e="dve"):
        rd = [a, s1, s2]
        A1 = s1.ap if isinstance(s1, T) else s1
        A2 = s2.ap if isinstance(s2, T) else s2
        if op1 is None:
            return self.op(e, lambda g: g.tensor_scalar(out.ap, a.ap, A1, None, op0), rd, [out])
        return self.op(e, lambda g: g.tensor_scalar(out.ap, a.ap, A1, A2, op0, op1), rd, [out])

    def stt(self, out, a, s, b, op0, op1):
        rd = [a, b, s]
        Sx = s.ap if isinstance(s, T) else s
        return self.op("dve", lambda g: g.scalar_tensor_tensor(out.ap, a.ap, Sx, b.ap, op0, op1), rd, [out])

    def copy(self, out, in_, e="dve"):
        if e == "act":
            return self.op(e, lambda g: g.copy(out.ap, in_.ap), [in_], [out])
        return self.op(e, lambda g: g.tensor_copy(out.ap, in_.ap), [in_], [out])

    def memset(self, out, val, e="dve"):
        return self.op(e, lambda g: g.memset(out.ap, val), [], [out])

    def recip(self, out, in_):
        return self.op("dve", lambda g: g.reciprocal(out.ap, in_.ap), [in_], [out])

    def scan(self, out, d0, d1, init, op0, op1):
        return self.op("dve", lambda g: g.tensor_tensor_scan(out.ap, d0.ap, d1.ap, init, op0, op1), [d0, d1], [out])

    def reduce(self, out, in_, op, axis=AX.X):
        return self.op("dve", lambda g: g.tensor_reduce(out.ap, in_.ap, axis, op), [in_], [out])

    def bn_stats(self, out, in_):
        return self.op("dve", lambda g: g.bn_stats(out.ap, in_.ap), [in_], [out])

    def bn_aggr(self, out, in_):
        return self.op("dve", lambda g: g.bn_aggr(out.ap, in_.ap), [in_], [out])


def _na_variants():
    types = {0: [0, 1, 2, 3], 1: [-1, 0, 1, 2], 2: [-2, -1, 0, 1, 2], 3: [-2, -1, 0, 1], 4: [-3, -2, -1, 0]}
    rep = {0: 0, 1: 1, 2: 5, 3: 14, 4: 15}
    var = []
    for ty in range(5):
        for dc in types[ty]:
            var.append((ty, dc))
    return types, rep, var


def _qtype(qc):
    return {0: 0, 1: 1, 14: 3, 15: 4}.get(qc, 2)


def _consts_np():
    c = {}
    idx = np.arange(128)
    c["ident"] = np.eye(128, dtype=np.float32)
    blk = (idx[:, None] // 64 == idx[None, :] // 64).astype(np.float32)
    c["blockones"] = blk
    rm = np.zeros((128, 128), np.float32)
    for h in range(2):
        for m in range(64):
            q = m // 16
            if q in (0, 2):
                rm[h * 64 + m + 16, h * 64 + m] = -1.0
            else:
                rm[h * 64 + m - 16, h * 64 + m] = 1.0
    c["rmat"] = rm
    LT = (idx[:, None] < idx[None, :]).astype(np.float32)
    LE = (idx[:, None] <= idx[None, :]).astype(np.float32)
    GT = (idx[:, None] > idx[None, :]).astype(np.float32)
    GE = (idx[:, None] >= idx[None, :]).astype(np.float32)
    c["maskA"] = np.stack([np.concatenate([LT, LE], 1), np.concatenate([GT, GE], 1)], 1).astype(np.float32)
    c["maskN"] = np.stack([GT, GT, LT, LT], 1).astype(np.float32)
    t = np.arange(S)
    row = (t // 64).astype(np.float32)
    col = (t % 64).astype(np.float32)
    inv = (10000.0 ** (-np.arange(16, dtype=np.float32) / 16)).astype(np.float32)
    ar = row[:, None] * inv
    ac = col[:, None] * inv
    ang = np.concatenate([ar, ar, ac, ac], -1).astype(np.float32)
    cs = np.cos(ang).astype(np.float32).T
    sn = np.sin(ang).astype(np.float32).T
    c["cos"] = np.ascontiguousarray(np.concatenate([cs, cs], 0))
    c["sin"] = np.ascontiguousarray(np.concatenate([sn, sn], 0))
    types, rep, var = _na_variants()
    mk = np.zeros((128, len(var), 128), np.float32)
    for v, (ty, dc) in enumerate(var):
        qc = rep[ty]
        for kr in range(2):
            krow = 2 * (qc + dc) + kr
            for qr in range(2):
                i = 2 * qc + qr
                rs = min(max(i - 4, 0), 24)
                if not (rs <= krow < rs + 8):
                    continue
                for qcol in range(64):
                    cst = min(max(qcol - 8, 0), 48)
                    mk[kr * 64 + cst: kr * 64 + cst + 16, v, qr * 64 + qcol] = 1.0
    c["namask"] = mk
    c["thr"] = np.tile((float(BS) * np.arange(128, dtype=np.float32))[None, :], (128, 1))
    c["kp"] = np.tile(np.arange(128, dtype=np.float32)[:, None], (1, 8))
    return c


def _rb_gather(rpb):
    kr = np.arange(128)[:, None] // 64
    kc = np.arange(128)[:, None] % 64
    qr = np.arange(128)[None, :] // 64
    qc = np.arange(128)[None, :] % 64
    out = np.zeros((L, 7, 128, 8, 128), np.float32)
    ci = np.clip(kc - qc + 15, 0, 30)
    for d in range(7):
        ri = np.clip(2 * (d - 3) + kr - qr + 7, 0, 14)
        g = rpb[:, :, ri, ci]
        out[:, d] = np.transpose(g, (0, 2, 1, 3))
    return out


def _vecs_np(inp):
    v = np.zeros((L, 128, NV), np.float32)
    groups = [(g * 128, 128) for g in range(12)] + [(1536, 32), (1568, 32), (1600, 32), (1632, 32), (1664, 96)]
    for l in range(L):
        mp = inp["rw_mu_prev"][l]
        mn = inp["rw_mu_next"][l]
        for g, (a, n) in enumerate(groups):
            v[l, :n, g] = mp[a:a + n]
            v[l, :n, 17 + g] = mn[a:a + n]
        for d in range(2):
            for j in range(4):
                v[l, :, 34 + d * 4 + j] = inp["rw_w0"][l, d, j * 128:(j + 1) * 128]
                v[l, :, 42 + d * 4 + j] = inp["rw_a0"][l, d, j * 128:(j + 1) * 128]
        rk = inp["rw_r_k"][l].reshape(512)
        for j in range(4):
            sl = slice(j * 128, (j + 1) * 128)
            v[l, :, 50 + j] = inp["rw_k_k"][l, sl]
            v[l, :, 54 + j] = inp["rw_k_a"][l, sl]
            v[l, :, 58 + j] = rk[sl]
            v[l, :, 62 + j] = inp["rw_gn_g"][l, sl]
            v[l, :, 66 + j] = inp["rw_gn_b"][l, sl]
    return v


def build(nlayers=L, phases=None, na_stage=9, rw_stage=9, moe_stage=9):
    nc = bass.Bass("TRN2", target_bir_lowering=False)
    em = EM(nc)
    types, rep, variants = _na_variants()
    NVAR = len(variants)

    def din(name, shape, dt=F32):
        return em.dram(name, shape, dt, kind="ExternalInput")

    x_in = din("x", [2, S, D])
    ctx_in = din("ctx", [2, CT, D])
    cT_in = din("cT", [128, 8, 3])
    ada_w = din("ada_w", [L, D, 6 * D])
    ada_b = din("ada_b", [L, 6 * D])
    w_in = din("w_in", [L, D, DIN])
    vecs = din("vecs", [L, 128, NV])
    rw_w2 = din("rw_w2", [L, 2, 32, 512])
    rw_a2 = din("rw_a2", [L, 2, 32, 512])
    rw_g2 = din("rw_g2", [L, 96, 512])
    gnrow = din("gnrow", [L, 2, 512])
    w_out = din("w_out", [L, D, D])
    lnv = din("lnv", [L, 4, D])
    router_w = din("router_w", [D, 32])
    rbias = din("router_bias", [32])
    ew1 = din("exp_w1", [L * 32 * 128, 8 * 512])
    ew3 = din("exp_w3", [L * 32 * 128, 8 * 512])
    ew2 = din("exp_w2", [L * 32 * 128, 4 * D])
    c_ident = din("ident", [128, 128])
    c_blk = din("blockones", [128, 128])
    c_rmat = din("rmat", [128, 128])
    c_maskA = din("maskA", [128, 2, 256])
    c_maskN = din("maskN", [128, 4, 128])
    c_cos = din("cos", [128, S])
    c_sin = din("sin", [128, S])
    c_namask = din("namask", [128, NVAR, 128])
    c_thr = din("thr", [128, 128])
    c_kp = din("kp", [128, 8])
    rb_in = din("rb", [L, 7, 128, 8, 128])
    y_out = em.dram("y", [2, S, D], F32, kind="ExternalOutput")

    modD = em.dram("modD", [L, 3, 6 * D])
    sD = em.dram("sD", [2, TT, D])
    x1D = em.dram("x1D", [2, TT, D])
    mixD = em.dram("mixD", [2, 128, 8, TT], BF16)
    h2D = em.dram("h2D", [2, 128, 8, TT], BF16)
    hTD = em.dram("hTD", [2, 128, 8, TT], BF16)
    h2tokD = em.dram("h2tokD", [2 * TT, D], BF16)
    xbD = em.dram("xbD", [NBLK * BS, D], BF16)
    ybD = em.dram("ybD", [NBLK * BS, D], F32)
    KRD = em.dram("KRD", [2, 2, 4, 128, NTT, 2, 128], BF16)
    BKD = em.dram("BKD", [2, 2, 4, 128, NTT, 2, 128], BF16)
    KHD = em.dram("KHD", [2, 2, 4, 128, NTT, 2, 128], BF16)
    VFD = em.dram("VFD", [2, 4, 128, TT], BF16)
    BOND = em.dram("BOND", [2, 4, 128, TT], F32)
    GCD = em.dram("GCD", [2, 2, 4, 128, NTT], F32)

    pst = nc.alloc_psum_tensor("psum", [128, 8, 512], F32)
    ps = [T(pst.ap()[:, i, :], Buf(f"ps{i}")) for i in range(8)]

    def seq_tile(l, b, tt):
        if l == 0:
            if tt < 2:
                return ctx_in[b, tt * 128:(tt + 1) * 128, :]
            return x_in[b, (tt - 2) * 128:(tt - 1) * 128, :]
        return sD[b, tt * 128:(tt + 1) * 128, :]

    def want(name):
        return phases is None or name in phases

    pstack = ExitStack()
    em.stack = pstack
    ident = em.sb([128, 128], F32, "ident")
    em.dma(ident, c_ident)
    identb = em.sb([128, 128], BF16, "identb")
    em.copy(identb, ident)
    G_all2 = em.sb([128, 2, NTT, 32], F32, "G_all")

    def bcast_row(dst, row_ap_T):
        em.dma(dst, row_ap_T.pbc(128))

    def adaln(l):
        with ExitStack() as st:
            em.stack = st
            cTs = em.sb([128, 8, 3])
            em.dma(cTs, cT_in)
            sc = em.sb([128, 8, 3])
            em.act(sc, cTs, AF.Silu)
            adab = em.sb([3, 6 * D])
            em.dma(adab, ada_b[l].pbc(3))
            modsb = em.sb([3, 6 * D])
            wbuf = [em.sb([128, 8, 512]) for _ in range(2)]
            awv = ada_w[l].re("(k p) n -> p k n", p=128)
            for blk in range(12):
                wb = wbuf[blk % 2]
                em.dma(wb, awv[:, :, blk * 512:(blk + 1) * 512])
                pp = ps[blk % 2][0:3, :]
                for k in range(8):
                    em.mm(pp, sc[:, k, :], wb[:, k, :], start=(k == 0), stop=(k == 7))
                em.tt(modsb[:, blk * 512:(blk + 1) * 512], pp, adab[:, blk * 512:(blk + 1) * 512], ALU.add)
            em.dma(modD[l], modsb)
            em.barrier()
        em.stack = pstack

    def make_hT(l, b, hT, sc1, sh1):
        xt = [em.sb([128, D], F32, "xt") for _ in range(2)]
        for tt in range(NTT):
            xx = xt[tt % 2]
            em.dma(xx, seq_tile(l, b, tt))
            w = 0 if tt < 2 else 1
            em.tt(xx, xx, sc1[:, w, :], ALU.mult)
            em.tt(xx, xx, sh1[:, w, :], ALU.add)
            for half in range(2):
                pp = ps[6 + half]
                for kk in range(4):
                    k = half * 4 + kk
                    em.tr(pp[:, kk * 128:(kk + 1) * 128], xx[:, k * 128:(k + 1) * 128], ident)
                dst = hT[:, half * 4:(half + 1) * 4, tt * 128:(tt + 1) * 128]
                src = pp.re("p (a b) -> p a b", a=4)
                if half == 0:
                    em.copy(dst, src, e="act")
                else:
                    em.copy(dst, src, e="dve")

    def load_mod1(l, b):
        sc1 = em.sb([128, 2, D], F32, "sc1")
        sh1 = em.sb([128, 2, D], F32, "sh1")
        for w, row in enumerate((2, b)):
            bcast_row(sh1[:, w, :], modD[l, row, 0:D])
            bcast_row(sc1[:, w, :], modD[l, row, D:2 * D])
        em.ts(sc1, sc1, 1.0, None, ALU.add)
        return sc1, sh1

    def na_phase(l):
        with ExitStack() as st:
            em.stack = st
            Tb = em.sb([128, NVAR, 2, 4, 128], BF16, "Tb")
            with ExitStack() as st2:
                em.stack = st2
                mk = em.sb([128, NVAR, 128], F32, "mk")
                em.dma(mk, c_namask)
                for d in range(7):
                    rbt = em.sb([128, 8, 128], F32, "rbt")
                    em.dma(rbt, rb_in[l, d])
                    em.act(rbt, rbt, AF.Exp)
                    for v, (ty, dc) in enumerate(variants):
                        if dc + 3 == d:
                            for par in range(2):
                                em.tt(Tb[:, v, par], rbt.re("p (a two) q -> p a two q", two=2)[:, :, par, :],
                                      mk[:, v:v + 1, :].bc([128, 4, 128]), ALU.mult)
                em.barrier()
            em.stack = st
            ones_b = em.sb([128, 64], BF16, "ones_b")
            em.memset(ones_b, 1.0)
            wq = em.sb([128, 8, 1536], BF16, "wq")
            em.dma(wq, w_in[l].re("(k p) n -> p k n", p=128)[:, :, 0:1536], q="pool")
            for b in range(2):
                with ExitStack() as st3:
                    em.stack = st3
                    qT = em.sb([128, 4, TT], BF16, "qT")
                    kT = em.sb([128, 4, TT], BF16, "kT")
                    Vt = em.sb([128, NTT, 512], BF16, "Vt")
                    with ExitStack() as st4:
                        em.stack = st4
                        sc1, sh1 = load_mod1(l, b)
                        hT = em.sb([128, 8, TT], BF16, "hT")
                        make_hT(l, b, hT, sc1, sh1)
                        em.dma(hTD[b], hT)
                        cnt = 0
                        for ci in range(8):
                            dst = qT if ci < 4 else kT
                            for t0 in range(0, TT, 512):
                                w = min(512, TT - t0)
                                pp = ps[cnt % 2]
                                for k in range(8):
                                    em.mm(pp[:, 0:w], wq[:, k, ci * 128:(ci + 1) * 128], hT[:, k, t0:t0 + w],
                                          start=(k == 0), stop=(k == 7))
                                em.copy(dst[:, ci % 4, t0:t0 + w], pp[:, 0:w], e=("act" if cnt % 2 else "dve"))
                                cnt += 1
                        for tt in range(NTT):
                            pp = ps[cnt % 2]
                            for k in range(8):
                                em.mm(pp, hT[:, k, tt * 128:(tt + 1) * 128], wq[:, k, 1024:1536],
                                      start=(k == 0), stop=(k == 7))
                            em.copy(Vt[:, tt, :], pp, e=("act" if cnt % 2 else "dve"))
                            cnt += 1
                        em.barrier()
                    em.stack = st3
                    nab = [em.sb([128, 4, 128], BF16, "nab") for _ in range(2)]
                    if na_stage < 2:
                        em.barrier()
                        continue
                    Eb = [em.sb([128, 4, 128], BF16, "Eb") for _ in range(3)]
                    rec = em.sb([128, 512], F32, "rec")
                    qlist = [(2 + qc, True) for qc in range(16)]
                    if l == 0:
                        qlist += [(0, False), (1, False)]
                    its = []
                    for qi, (qt, is_x) in enumerate(qlist):
                        if is_x:
                            qc = qt - 2
                            ty = _qtype(qc)
                            keys = [(2 + qc + dc, variants.index((ty, dc))) for dc in types[ty]] + [(0, None), (1, None)]
                        else:
                            keys = [(0, None), (1, None)]
                        for ki, (kt, v) in enumerate(keys):
                            for hg in range(2):
                                its.append((qi, qt, ki, kt, v, hg, len(keys)))
                    NIT = len(its)

                    def emit_S(n):
                        qi, qt, ki, kt, v, hg, nk = its[n]
                        sp_ = ps[n % 4]
                        rows = slice(hg * 64, hg * 64 + 64)
                        for hh in range(4):
                            em.mm(sp_[:, hh * 128:(hh + 1) * 128], kT[rows, hh, kt * 128:(kt + 1) * 128],
                                  qT[rows, hh, qt * 128:(qt + 1) * 128])

                    def emit_E(n):
                        qi, qt, ki, kt, v, hg, nk = its[n]
                        E = Eb[n % 3]
                        em.act(E, ps[n % 4].re("p (a b) -> p a b", a=4), AF.Exp, scale=0.125)
                        if v is not None:
                            em.tt(E, E, Tb[:, v, hg, :, :], ALU.mult)

                    def emit_PV(n):
                        qi, qt, ki, kt, v, hg, nk = its[n]
                        E = Eb[n % 3]
                        num = ps[4 + 2 * (qi % 2)]
                        den = ps[5 + 2 * (qi % 2)]
                        rows = slice(hg * 64, hg * 64 + 64)
                        for hh in range(4):
                            h = 2 * hh + hg
                            cs = slice(hh * 128, hh * 128 + 128)
                            first = (ki == 0 and hh == 0)
                            em.mm(num[rows, cs], Vt[:, kt, h * 64:(h + 1) * 64], E[:, hh, :], start=first, stop=(ki == nk - 1))
                        em.mm(den[rows, :], ones_b, E.re("p a b -> p (a b)"), start=(ki == 0), stop=(ki == nk - 1))
                        if ki == nk - 1 and hg == 1:
                            em.recip(rec, den)
                            nb_ = nab[qi % 2]
                            em.tt(nb_, num.re("p (a b) -> p a b", a=4), rec.re("p (a b) -> p a b", a=4), ALU.mult)
                            em.dma(mixD[b, :, 0:4, qt * 128:(qt + 1) * 128], nb_)

                    emit_S(0)
                    emit_S(1)
                    emit_E(0)
                    for n in range(NIT):
                        if n + 2 < NIT:
                            emit_S(n + 2)
                        if n + 1 < NIT:
                            emit_E(n + 1)
                        emit_PV(n)
                    em.barrier()
            em.stack = st
            em.barrier()
        em.stack = pstack

    SEGS = [(0, 256, False, 0, 0)] + [(256 + i * 512, 512, True, int(i > 0), int(i < 3)) for i in range(4)]
    REV_ORDER = [1, 0] + list(range(NTT - 1, 1, -1))

    def rwkv_phase(l):
        with ExitStack() as st:
            em.stack = st
            vc = em.sb([128, NV], F32, "vc")
            em.dma(vc, vecs[l])
            c0v = em.sb([128, 17], F32, "c0v")
            em.tt(c0v, vc[:, 0:17], vc[:, 17:34], ALU.add)
            em.ts(c0v, c0v, -1.0, 1.0, ALU.mult, ALU.add)
            omka = em.sb([128, 4], F32, "omka")
            em.ts(omka, vc[:, 54:58], -1.0, 1.0, ALU.mult, ALU.add)
            blk = em.sb([128, 128], F32, "blk")
            em.dma(blk, c_blk)
            rmat = em.sb([128, 128], F32, "rmat")
            em.dma(rmat, c_rmat)
            blkrk = em.sb([128, 4, 128], F32, "blkrk")
            for j in range(4):
                em.ts(blkrk[:, j, :], blk, vc[:, 58 + j:59 + j], None, ALU.mult)
            maskA = em.sb([128, 2, 256], BF16, "maskA")
            maskN = em.sb([128, 4, 128], BF16, "maskN")
            w2b = em.sb([32, 2, 512], BF16, "w2b")
            a2b = em.sb([32, 2, 512], BF16, "a2b")
            g2b = em.sb([96, 512], BF16, "g2b")
            em.dma(maskA, c_maskA, q="pool")
            em.dma(maskN, c_maskN, q="pool")
            em.dma(w2b, rw_w2[l].re("d r c -> r d c"), q="pool")
            em.dma(a2b, rw_a2[l].re("d r c -> r d c"), q="pool")
            em.dma(g2b, rw_g2[l], q="pool")
            sgs = [em.sb([96, TT], BF16, "sg") for _ in range(2)]
            maskA32 = em.sb([128, 2, 128], F32, "maskA32")
            maskN32 = em.sb([128, 4, 128], F32, "maskN32")
            em.copy(maskA32, maskA[:, :, 0:128])
            em.copy(maskN32, maskN)
            stw = ExitStack()
            em.stack = stw
            wr = em.sb([128, 8, 1760], BF16, "wr")
            wv_ = w_in[l].re("(k p) n -> p k n", p=128)
            em.dma(wr[:, :, 1536:1760], wv_[:, :, 3072:DIN], q="pool")
            for j_ in range(4):
                for part in range(3):
                    c_ = part * 512 + j_ * 128
                    em.dma(wr[:, :, c_:c_ + 128], wv_[:, :, 1536 + c_:1536 + c_ + 128], q="pool")
            for b in range(2):
                with ExitStack() as st2:
                    em.stack = st2
                    sg = sgs[b]
                    rstm = em.sb([128, 512], F32, "rstm")
                    em.memset(rstm, 1.0)
                    em.memset(rstm.re("p (n t) -> p n t", t=128)[:, :, 0:1], 0.0)
                    tl = em.sb([32, 2, TT], BF16, "tl")
                    la = em.sb([32, 2, TT], BF16, "la")
                    hT = em.sb([128, 8, TT], BF16, "hT")
                    if want("na"):
                        em.dma(hT, hTD[b])
                    else:
                        with ExitStack() as st5:
                            em.stack = st5
                            sc1, sh1 = load_mod1(l, b)
                            make_hT(l, b, hT, sc1, sh1)
                            em.barrier()
                        em.stack = st2
                    cosb = em.sb([128, 512], F32, "cosb")
                    sinb = em.sb([128, 512], F32, "sinb")

                    class Lane:
                        pass

                    def mk_lane(li):
                        ln = Lane()
                        ln.B = ps[4 * li:4 * li + 4]
                        ln.c0 = 0
                        ln.c1 = 0
                        ln.Pb = em.sb([128, 514], F32, "Pb")
                        ln.tmp = [em.sb([128, 512], F32, f"tmp{i}") for i in range(3)]
                        for nm in ("r_", "k_", "v_", "kk_", "lw_", "a_", "cum_", "kd_", "be_", "rks"):
                            setattr(ln, nm, em.sb([128, 512], F32, nm))
                        ln.ob = [em.sb([128, 512], BF16, f"ob{i}") for i in range(7)]
                        ln.gct = em.sb([128, 4], F32, "gct")
                        return ln

                    def bankA(ln):
                        ln.c0 += 1
                        return ln.B[ln.c0 % 2]

                    def bankB(ln):
                        ln.c1 += 1
                        return ln.B[2 + ln.c1 % 2]

                    def proj_shift(ln, dst, g, m, c0, seg):
                        t0, n, rope, hl, hr = seg
                        Pb = ln.Pb
                        ta = t0 - hl
                        tb = t0 + n + hr
                        if not hl:
                            em.memset(Pb[0:m, 0:1], 0.0)
                        if not hr:
                            em.memset(Pb[0:m, n + 1:n + 2], 0.0)
                        for s0 in range(ta, tb, 512):
                            w = min(512, tb - s0)
                            pp = bankA(ln)
                            for k in range(8):
                                em.mm(pp[0:m, 0:w], wr[:, k, c0:c0 + m], hT[:, k, s0:s0 + w], start=(k == 0), stop=(k == 7))
                            o = 1 - hl + (s0 - ta)
                            em.copy(Pb[0:m, o:o + w], pp[0:m, 0:w], e="act")
                        em.ts(dst, Pb[0:m, 1:n + 1], c0v[0:m, g:g + 1], None, ALU.mult)
                        em.stt(dst, Pb[0:m, 0:n], vc[0:m, g:g + 1], dst, ALU.mult, ALU.add)
                        em.stt(dst, Pb[0:m, 2:n + 2], vc[0:m, 17 + g:18 + g], dst, ALU.mult, ALU.add)

                    lanes = [mk_lane(0), mk_lane(1)]
                    for si, seg in enumerate(SEGS):
                        t0, n = seg[0], seg[1]
                        ln = lanes[si % 2]
                        t_ = ln.tmp[0]
                        for d in range(2):
                            proj_shift(ln, t_[0:32, 0:n], 12 + d, 32, 1536 + d * 32, seg)
                            em.act(tl[:, d, t0:t0 + n], t_[0:32, 0:n], AF.Tanh)
                            proj_shift(ln, t_[0:32, 0:n], 14 + d, 32, 1600 + d * 32, seg)
                            em.copy(la[:, d, t0:t0 + n], t_[0:32, 0:n])
                        proj_shift(ln, t_[0:96, 0:n], 16, 96, 1664, seg)
                        em.act(sg[:, t0:t0 + n], t_[0:96, 0:n], AF.Sigmoid)

                    def rope_apply(ln, z, n):
                        pp = bankB(ln)
                        em.mm(pp[:, 0:n], rmat, z[:, 0:n])
                        em.tt(ln.tmp[0][:, 0:n], pp[:, 0:n], sinb[:, 0:n], ALU.mult)
                        em.tt(z[:, 0:n], z[:, 0:n], cosb[:, 0:n], ALU.mult)
                        em.tt(z[:, 0:n], z[:, 0:n], ln.tmp[0][:, 0:n], ALU.add)

                    def pair_gen(ln, seg, j):
                        t0, n, rope, hl, hr = seg
                        nch = n // 128
                        n0 = t0 // 128
                        tmp = ln.tmp
                        ob = ln.ob
                        r = ln.r_[:, 0:n]
                        k = ln.k_[:, 0:n]
                        v = ln.v_[:, 0:n]
                        kk = ln.kk_[:, 0:n]
                        rks = ln.rks
                        proj_shift(ln, r, j, 128, j * 128, seg)
                        yield
                        proj_shift(ln, k, 4 + j, 128, 512 + j * 128, seg)
                        yield
                        proj_shift(ln, v, 8 + j, 128, 1024 + j * 128, seg)
                        yield
                        if rope:
                            rope_apply(ln, ln.r_, n)
                            yield
                            rope_apply(ln, ln.k_, n)
                            yield
                        em.copy(ob[6][:, 0:n], v)
                        em.dma(VFD[b, j, :, t0:t0 + n], ob[6][:, 0:n])
                        em.ts(kk, k, vc[:, 50 + j:51 + j], None, ALU.mult)
                        em.tt(tmp[0][:, 0:n], kk, kk, ALU.mult)
                        pp = bankB(ln)
                        em.mm(pp[:, 0:n], blk, tmp[0][:, 0:n])
                        yield
                        em.act(tmp[1][:, 0:n], pp[:, 0:n], AF.Sqrt)
                        yield
                        em.ts(tmp[1][:, 0:n], tmp[1][:, 0:n], 1e-12, None, ALU.max)
                        em.recip(tmp[1][:, 0:n], tmp[1][:, 0:n])
                        em.tt(kk, kk, tmp[1][:, 0:n], ALU.mult)
                        yield
                        for d in range(2):
                            lw = ln.lw_[:, 0:n]
                            a = ln.a_[:, 0:n]
                            cum = ln.cum_[:, 0:n]
                            kd = ln.kd_[:, 0:n]
                            be = ln.be_[:, 0:n]
                            pp = bankB(ln)
                            em.mm(pp[:, 0:n], w2b[:, d, j * 128:(j + 1) * 128], tl[:, d, t0:t0 + n])
                            pp2 = bankB(ln)
                            em.mm(pp2[:, 0:n], a2b[:, d, j * 128:(j + 1) * 128], la[:, d, t0:t0 + n])
                            yield
                            em.act(lw, pp[:, 0:n], AF.Sigmoid, bias=vc[:, 34 + d * 4 + j:35 + d * 4 + j])
                            em.act(a, pp2[:, 0:n], AF.Sigmoid, bias=vc[:, 42 + d * 4 + j:43 + d * 4 + j])
                            yield
                            em.scan(tmp[0][:, 0:n], rstm[:, 0:n], lw, 0.0, ALU.mult, ALU.add)
                            pre3 = tmp[0][:, 0:n].re("p (n t) -> p n t", t=128)
                            totb = pre3[:, :, 127:128].bc([128, nch, 128])
                            cum3 = cum.re("p (n t) -> p n t", t=128)
                            if d == 0:
                                em.copy(cum, tmp[0][:, 0:n])
                            else:
                                em.tt(cum3, totb, pre3, ALU.subtract)
                                em.tt(cum, cum, lw, ALU.add)
                            em.act(ln.gct[:, 0:nch], pre3[:, :, 127], AF.Exp, scale=-LAM)
                            em.dma(GCD[b, d, j, :, n0:n0 + nch], ln.gct[:, 0:nch])
                            em.ts(tmp[1][:, 0:n], a, vc[:, 54 + j:55 + j], omka[:, j:j + 1], ALU.mult, ALU.add)
                            em.tt(kd, tmp[1][:, 0:n], k, ALU.mult)
                            em.tt(be, kk, a, ALU.mult)
                            if d == 0:
                                em.tt(rks[:, 0:n], r, kd, ALU.mult)
                            else:
                                em.tt(tmp[1][:, 0:n], r, kd, ALU.mult)
                                em.tt(rks[:, 0:n], rks[:, 0:n], tmp[1][:, 0:n], ALU.add)
                            yield
                            e1 = tmp[1][:, 0:n]
                            e2 = tmp[2][:, 0:n]
                            em.act(e1, cum, AF.Exp, scale=-LAM)
                            em.tt(e2, cum, lw, ALU.subtract)
                            yield
                            em.tt(ob[1][:, 0:n], r, e1, ALU.mult)
                            em.act(e1, cum, AF.Exp, scale=LAM)
                            em.act(e2, e2, AF.Exp, scale=-LAM)
                            yield
                            em.tt(ob[3][:, 0:n], kd, e1, ALU.mult)
                            em.stt(ob[2][:, 0:n], be, -1.0, e1, ALU.mult, ALU.mult)
                            em.tt(ob[0][:, 0:n], kk, e2, ALU.mult)
                            em.tt(e2.re("p (n t) -> p n t", t=128), totb, cum3, ALU.subtract)
                            em.act(e2, e2, AF.Exp, scale=-LAM)
                            yield
                            em.tt(ob[4][:, 0:n], kd, e2, ALU.mult)
                            em.stt(ob[5][:, 0:n], be, -1.0, e2, ALU.mult, ALU.mult)
                            for idx, (dst, slot) in enumerate(((KRD[b], 0), (KRD[b], 1), (BKD[b], 0), (BKD[b], 1), (KHD[b], 0), (KHD[b], 1))):
                                em.dma(dst[d, j, :, n0:n0 + nch, slot, :], ob[idx][:, 0:n].re("p (n t) -> p n t", t=128))
                            yield
                        pp = bankB(ln)
                        em.mm(pp[:, 0:n], blkrk[:, j, :], rks[:, 0:n])
                        yield
                        em.tt(tmp[0][:, 0:n], pp[:, 0:n], v, ALU.mult)
                        em.dma(BOND[b, j, :, t0:t0 + n], tmp[0][:, 0:n])

                    for seg in SEGS:
                        t0, n, rope, hl, hr = seg
                        if rope:
                            em.dma(cosb[:, 0:n], c_cos[:, t0 - CT:t0 - CT + n])
                            em.dma(sinb[:, 0:n], c_sin[:, t0 - CT:t0 - CT + n])
                        for jj in range(0, 4, 2):
                            gens = [pair_gen(lanes[0], seg, jj), pair_gen(lanes[1], seg, jj + 1)]
                            alive = [True, True]
                            while any(alive):
                                for gi, g in enumerate(gens):
                                    if alive[gi]:
                                        try:
                                            next(g)
                                        except StopIteration:
                                            alive[gi] = False
                    em.barrier()
                em.stack = stw
            stw.close()
            em.stack = st
            def rr(t):
                return t.cast(F32R) if INV_F32R else t

            zero_reg = nc.gpsimd.to_reg(0.0)

            def asel(out, in_, pattern, cmp, base, cm):
                return em.op("pool", lambda g: g.affine_select(out.ap, in_.ap, pattern, cmp, zero_reg, base=base,
                                                               channel_multiplier=cm), [in_], [out])

            def scan_gen(b, j, B, gi):
                a_, c_ = B
                sg = sgs[b]
                KRt = [em.sb([64, 4, 256], BF16, "KRt") for _ in range(2)]
                BKt = [em.sb([64, 4, 256], BF16, "BKt") for _ in range(2)]
                KHt = [em.sb([128, 2, 256], BF16, "KHt") for _ in range(2)]
                Vf = [em.sb([128, 2, 128], BF16, "Vf") for _ in range(2)]
                gC = em.sb([64, 4, NTT], F32, "gC")
                for d in range(2):
                    em.dma(gC[:, d * 2:d * 2 + 2, :], GCD[b, d, j].re("(h c) n -> c h n", h=2))
                TM = em.sb([128, 6, 128], BF16, "TM")
                A1 = em.sb([128, 4, 256], BF16, "A1")
                A2 = em.sb([128, 4, 256], BF16, "A2")
                PY = em.sb([128, 4, 2, 128], F32, "PY")
                Pq = PY[:, :, 0, :]
                Yq = PY[:, :, 1, :]
                Qq = em.sb([128, 4, 128], F32, "Qq")
                TTb = em.sb([128, 4, 128], BF16, "TTb")
                RH = em.sb([128, 4, 64], BF16, "RH")
                Ub = em.sb([128, 4, 64], BF16, "Ub")
                Zf = em.sb([64, 4, 64], F32, "Zf")
                Zb = em.sb([64, 4, 64], BF16, "Zb")
                ztmp = em.sb([64, 4, 64], F32, "ztmp")
                ytm = em.sb([128, NTT, 128], F32, "ytm")
                em.memset(Zf, 0.0)
                em.memset(Zb, 0.0)
                em.memset(ytm, 0.0, e="pool")
                e0 = "dve"
                e1 = "act"
                v4 = lambda t: t.re("p (a b) -> p a b", a=4)
                v2 = lambda t: t.re("p (a b) -> p a b", a=2)

                def load_step(s):
                    sl = s % 2
                    for d in range(2):
                        n = s if d == 0 else REV_ORDER[s]
                        em.dma(KRt[sl][:, d * 2:d * 2 + 2, :], KRD[b, d, j, :, n].re("(h c) s t -> c h (s t)", h=2))
                        em.dma(BKt[sl][:, d * 2:d * 2 + 2, :], BKD[b, d, j, :, n].re("(h c) s t -> c h (s t)", h=2))
                        em.dma(KHt[sl][:, d, :], KHD[b, d, j, :, n].re("p s t -> p (s t)"))
                        em.dma(Vf[sl][:, d, :], VFD[b, j, :, n * 128:(n + 1) * 128])

                def mmA(bank, d, lo):
                    for hp in range(2):
                        u = d * 2 + hp
                        em.mm(bank[:, hp * 256:(hp + 1) * 256], BKt[sl][:, u, lo:lo + 128], KRt[sl][:, u, :])

                load_step(0)
                yield
                for s in range(NTT):
                    sl = s % 2
                    if s + 1 < NTT:
                        load_step(s + 1)
                    ns = [s, REV_ORDER[s]]
                    pt = a_.cast(BF16)
                    for d in range(2):
                        em.tr(pt[:, (2 * d) * 128:(2 * d + 1) * 128], KHt[sl][:, d, 0:128], identb)
                        em.tr(pt[:, (2 * d + 1) * 128:(2 * d + 2) * 128], KHt[sl][:, d, 128:256], identb)
                        em.tr(pt[:, (4 + d) * 128:(5 + d) * 128], Vf[sl][:, d, :], identb)
                    mmA(c_, 0, 0)
                    yield
                    em.copy(TM, pt[:, 0:768].re("p (a b) -> p a b", a=6), e="act")
                    em.copy(A1[:, 0:2, 128:256], v2(c_)[:, :, 128:256], e="act")
                    em.copy(rr(Pq[:, 0:2, :]), v2(c_)[:, :, 0:128], e="act")
                    asel(A1[:, 0:2, 128:256], A1[:, 0:2, 128:256], [[0, 2], [1, 128]], ALU.is_ge, 0, -1)
                    asel(rr(Pq[:, 0:2, :]), Pq[:, 0:2, :], [[0, 2], [1, 128]], ALU.is_gt, 0, -1)
                    mmA(a_, 1, 0)
                    for u in range(4):
                        em.mm(c_[:, u * 128:(u + 1) * 128], KRt[sl][:, u, 0:128], BKt[sl][:, u, 0:128])
                    yield
                    em.copy(A1[:, 2:4, 128:256], v2(a_)[:, :, 128:256], e="act")
                    em.copy(rr(Pq[:, 2:4, :]), v2(a_)[:, :, 0:128], e="act")
                    em.copy(rr(Qq), v4(c_), e="act")
                    asel(A1[:, 2:4, 128:256], A1[:, 2:4, 128:256], [[0, 2], [-1, 128]], ALU.is_ge, 0, 1)
                    asel(rr(Pq[:, 2:4, :]), Pq[:, 2:4, :], [[0, 2], [-1, 128]], ALU.is_gt, 0, 1)
                    asel(rr(Qq[:, 0:2, :]), Qq[:, 0:2, :], [[0, 2], [-1, 128]], ALU.is_gt, 0, 1)
                    asel(rr(Qq[:, 2:4, :]), Qq[:, 2:4, :], [[0, 2], [1, 128]], ALU.is_gt, 0, -1)
                    em.tt(rr(Yq), Pq, T(ident.ap.unsqueeze(1).to_broadcast([128, 4, 128]), ident.buf), ALU.add)
                    mmA(a_, 0, 128)
                    mmA(c_, 1, 128)
                    yield
                    em.copy(A2[:, 0:2, :], v2(a_), e="act")
                    em.copy(A2[:, 2:4, :], v2(c_), e="act")
                    asel(A2[:, 0:2, :], A2[:, 0:2, :], [[0, 2], [1, 2], [1, 128]], ALU.is_ge, -1, -1)
                    asel(A2[:, 2:4, :], A2[:, 2:4, :], [[0, 2], [1, 2], [-1, 128]], ALU.is_ge, -1, 1)
                    for u in range(4):
                        em.mm(a_[:, u * 128:(u + 1) * 128], rr(Qq[:, u, :]), rr(Pq[:, u, :]))
                    for u in range(4):
                        em.mm(c_[:, u * 128:(u + 1) * 128], rr(Pq[:, u, :]), rr(Qq[:, u, :]))
                    yield
                    em.copy(rr(Pq), v4(a_), e=e0)
                    em.copy(rr(Qq), v4(c_), e=e1)
                    for lev in range(1, 6):
                        for u in (0, 1):
                            em.mm(a_[:, u * 256:(u + 1) * 256], rr(Qq[:, u, :]), rr(PY[:, u, :, :]))
                        for u in range(4):
                            em.mm(c_[:, u * 128:(u + 1) * 128], rr(Pq[:, u, :]), rr(Qq[:, u, :]))
                        yield
                        a3 = a_.re("p (u s t) -> p u s t", u=2, s=2)
                        em.tt(rr(Yq[:, 0:2, :]), a3[:, :, 1, :], Yq[:, 0:2, :], ALU.add)
                        em.copy(rr(Pq[:, 0:2, :]), a3[:, :, 0, :], e=e0)
                        for u in (2, 3):
                            em.mm(a_[:, (u - 2) * 256:(u - 1) * 256], rr(Qq[:, u, :]), rr(PY[:, u, :, :]))
                        yield
                        em.tt(rr(Yq[:, 2:4, :]), a3[:, :, 1, :], Yq[:, 2:4, :], ALU.add)
                        em.copy(rr(Pq[:, 2:4, :]), a3[:, :, 0, :], e=e0)
                        em.copy(rr(Qq), v4(c_), e=e1)
                    for u in range(4):
                        em.mm(a_[:, u * 128:(u + 1) * 128], rr(Qq[:, u, :]), rr(Yq[:, u, :]))
                    yield
                    em.tt(rr(Yq), v4(a_), Yq, ALU.add)
                    em.copy(TTb, Yq, e="act")
                    for u in range(4):
                        d, hp = u // 2, u % 2
                        vs = TM[:, 4 + d, hp * 64:(hp + 1) * 64]
                        em.mm(c_[:, u * 64:(u + 1) * 64], KRt[sl][:, u, 0:128], Zb[:, u, :], start=True, stop=False)
                        em.mm(c_[:, u * 64:(u + 1) * 64], A2[:, u, 0:128], vs, start=False, stop=True)
                    yield
                    em.copy(RH, v4(c_[:, 0:256]), e="act")
                    for u in range(4):
                        em.mm(a_[:, u * 64:(u + 1) * 64], TTb[:, u, :], RH[:, u, :])
                    yield
                    em.copy(Ub, v4(a_[:, 0:256]), e="act")
                    for u in range(4):
                        d, hp = u // 2, u % 2
                        vs = TM[:, 4 + d, hp * 64:(hp + 1) * 64]
                        yo = c_[:, u * 64:(u + 1) * 64]
                        em.mm(yo, KRt[sl][:, u, 128:256], Zb[:, u, :], start=True, stop=False)
                        em.mm(yo, A2[:, u, 128:256], vs, start=False, stop=False)
                        em.mm(yo, A1[:, u, 128:256], Ub[:, u, :], start=False, stop=True)
                        zo = a_[0:64, 256 + u * 64:256 + (u + 1) * 64]
                        em.mm(zo, TM[:, 2 * d, hp * 64:(hp + 1) * 64], vs, start=True, stop=False)
                        em.mm(zo, TM[:, 2 * d + 1, hp * 64:(hp + 1) * 64], Ub[:, u, :], start=False, stop=True)
                    yield
                    for d in range(2):
                        n = ns[d]
                        em.tt(ytm[:, n, :], ytm[:, n, :], c_[:, d * 128:(d + 1) * 128], ALU.add)
                        em.tt(ztmp[:, d * 2:d * 2 + 2, :], Zf[:, d * 2:d * 2 + 2, :],
                              gC[:, d * 2:d * 2 + 2, n:n + 1].bc([64, 2, 64]), ALU.mult)
                    em.tt(Zf, ztmp, v4(a_[0:64, 256:512]), ALU.add)
                    em.copy(Zb, Zf, e="act")
                st_s = em.sb([128, NTT * 2], F32, "st_s")
                st_q = em.sb([128, NTT * 2], F32, "st_q")
                msq = em.sb([128, NTT * 2], F32, "msq")
                fin = em.sb([128, 512], F32, "fin")
                bon = em.sb([128, 512], F32, "bon")
                rwc = em.sb([128, 512], BF16, "rwc")
                y4 = ytm.re("p n (h v) -> p (n h) v", h=2)
                em.reduce(st_s, y4, ALU.add)
                for q4 in range(0, NTT, 4):
                    nt = min(4, NTT - q4)
                    f3 = fin[:, 0:nt * 128].re("p (n c) -> p n c", c=128)
                    em.tt(f3, ytm[:, q4:q4 + nt, :], ytm[:, q4:q4 + nt, :], ALU.mult)
                    em.reduce(st_q[:, q4 * 2:(q4 + nt) * 2], fin[:, 0:nt * 128].re("p (n v) -> p n v", v=64), ALU.add)
                yield
                em.ts(st_s, st_s, 1.0 / 64, None, ALU.mult)
                em.ts(st_q, st_q, 1.0 / 64, None, ALU.mult)
                em.tt(msq, st_s, st_s, ALU.mult)
                em.tt(st_q, st_q, msq, ALU.subtract)
                em.ts(st_q, st_q, GN_EPS, None, ALU.add)
                em.act(st_q, st_q, AF.Sqrt)
                em.recip(st_q, st_q)
                yield
                em.tt(y4, y4, T(st_s.ap.unsqueeze(2).to_broadcast([128, NTT * 2, 64]), st_s.buf), ALU.subtract)
                em.tt(y4, y4, T(st_q.ap.unsqueeze(2).to_broadcast([128, NTT * 2, 64]), st_q.buf), ALU.mult)
                yield
                for q4 in range(0, NTT, 4):
                    nt = min(4, NTT - q4)
                    w = nt * 128
                    tsl = slice(q4 * 128, q4 * 128 + w)
                    em.dma(bon[:, 0:w], BOND[b, j, :, tsl])
                    for i in range(nt):
                        em.tr(a_[:, i * 128:(i + 1) * 128], ytm[:, q4 + i, :], ident)
                    em.mm(c_[:, 0:w], g2b[:, j * 128:(j + 1) * 128], sg[:, tsl])
                    yield
                    em.ts(fin[:, 0:w], a_[:, 0:w], vc[:, 62 + j:63 + j], vc[:, 66 + j:67 + j], ALU.mult, ALU.add)
                    em.tt(fin[:, 0:w], fin[:, 0:w], bon[:, 0:w], ALU.add)
                    em.tt(rwc[:, 0:w], fin[:, 0:w], c_[:, 0:w], ALU.mult)
                    em.dma(mixD[b, :, 4 + j, tsl], rwc[:, 0:w])

            for jj in range(0, 4 if rw_stage >= 2 else 0, 2):
                with ExitStack() as st3:
                    em.stack = st3
                    gens = []
                    for gi, (b_, j_) in enumerate(((0, jj), (1, jj), (0, jj + 1), (1, jj + 1))):
                        gens.append(scan_gen(b_, j_, (ps[2 * gi], ps[2 * gi + 1]), gi))
                    alive = [True] * len(gens)
                    while any(alive):
                        for gi, g in enumerate(gens):
                            if alive[gi]:
                                try:
                                    next(g)
                                except StopIteration:
                                    alive[gi] = False
                    em.barrier()
                em.stack = st
            em.barrier()
        em.stack = pstack

    def post_phase(l, b):
        G_all = G_all2[:, b]
        with ExitStack() as st:
            em.stack = st
            ntiles = NTT if l == 0 else NTT
            t_lo = 0 if l == 0 else 2
            wo = em.sb([128, 8, D], BF16, "wo")
            em.dma(wo, w_out[l].re("(k p) n -> p k n", p=128), q="pool")
            rw32 = em.sb([128, 8, 32], F32, "rw32")
            em.dma(rw32, router_w.re("(k p) n -> p k n", p=128))
            rb = em.sb([128, 32], F32, "rb")
            bcast_row(rb, rbias)
            g1 = em.sb([128, 2, D], F32, "g1")
            sc2 = em.sb([128, 2, D], F32, "sc2")
            sh2 = em.sb([128, 2, D], F32, "sh2")
            for w, row in enumerate((2, b)):
                bcast_row(g1[:, w, :], modD[l, row, 2 * D:3 * D])
                bcast_row(sh2[:, w, :], modD[l, row, 3 * D:4 * D])
                bcast_row(sc2[:, w, :], modD[l, row, 4 * D:5 * D])
            em.ts(sc2, sc2, 1.0, None, ALU.add)
            lg = em.sb([128, D], F32, "lg")
            lb = em.sb([128, D], F32, "lb")
            bcast_row(lg, lnv[l, 0])
            bcast_row(lb, lnv[l, 1])
            PAIRS = [(0, 1), (0, 2), (0, 3), (1, 2), (1, 3), (2, 3)]
            NLANE = 4

            def lane_gen(li, B):
                m = em.sb([128, 8, 128], BF16, "mt")
                xx = em.sb([128, D], F32, "xt")
                u_ = em.sb([128, D], F32, "u_")
                h2 = em.sb([128, D], F32, "h2")
                h2f = em.sb([128, 8, 128], F32, "h2f")
                hb = em.sb([128, 8, 128], BF16, "h2b")
                h2t = em.sb([128, D], BF16, "h2t")
                stt_ = em.sb([128, 2, 6], F32, "bst")
                mv = em.sb([128, 2], F32, "mv")
                rstd = em.sb([128, 1], F32, "rstd")
                sc_ = em.sb([128, 32], F32, "sc_")
                sel = em.sb([128, 32], F32, "sel")
                p6 = em.sb([128, 8, 6], F32, "p6")
                m6 = em.sb([128, 8, 6], F32, "m6")
                gs = em.sb([128, 8], F32, "gs")
                sec = em.sb([128, 8], F32, "sec")
                gmx = em.sb([128, 1], F32, "gmx")
                gmk = em.sb([128, 8], F32, "gmk")
                emk = em.sb([128, 32], F32, "emk")
                gsum = em.sb([128, 1], F32, "gsum")
                for tt in range(t_lo + li, NTT, NLANE):
                    w = 0 if tt < 2 else 1
                    tsl = slice(tt * 128, (tt + 1) * 128)
                    em.dma(m, mixD[b, :, :, tsl])
                    em.dma(xx, seq_tile(l, b, tt))
                    for half in range(2):
                        for k in range(8):
                            em.mm(B[half], m[:, k, :], wo[:, k, half * 512:(half + 1) * 512], start=(k == 0), stop=(k == 7))
                    yield
                    for half in range(2):
                        em.tt(u_[:, half * 512:(half + 1) * 512], B[half], g1[:, w, half * 512:(half + 1) * 512], ALU.mult)
                    em.stt(u_, xx, ALPHA, u_, ALU.mult, ALU.add)
                    for half in range(2):
                        em.bn_stats(stt_[:, half, :], u_[:, half * 512:(half + 1) * 512])
                    em.bn_aggr(mv, stt_.re("p a b -> p (a b)"))
                    em.ts(rstd, mv[:, 1:2], LN_EPS, None, ALU.add)
                    em.act(rstd, rstd, AF.Sqrt)
                    yield
                    em.recip(rstd, rstd)
                    em.ts(u_, u_, mv[:, 0:1], rstd, ALU.subtract, ALU.mult)
                    em.tt(u_, u_, lg, ALU.mult)
                    em.tt(u_, u_, lb, ALU.add)
                    em.dma(x1D[b, tsl, :], u_)
                    em.tt(h2, u_, sc2[:, w, :], ALU.mult, e="pool")
                    em.tt(h2, h2, sh2[:, w, :], ALU.add, e="pool")
                    if SPARSE_MOE:
                        em.copy(h2t, h2, e="pool")
                        em.dma(h2tokD[b * TT + tt * 128:b * TT + (tt + 1) * 128, :], h2t)
                    yield
                    for half in range(2):
                        pp = B[half]
                        for kk in range(4):
                            k = half * 4 + kk
                            em.tr(pp[:, kk * 128:(kk + 1) * 128], h2[:, k * 128:(k + 1) * 128], ident)
                    yield
                    for half in range(2):
                        em.copy(h2f[:, half * 4:(half + 1) * 4, :], B[half].re("p (a b) -> p a b", a=4), e="act")
                    em.copy(hb, h2f, e="pool")
                    em.dma(h2D[b, :, :, tsl], hb)
                    lgt = B[0][:, 0:32]
                    for k in range(8):
                        em.mm(lgt, h2f[:, k, :], rw32[:, k, :], start=(k == 0), stop=(k == 7))
                    yield
                    em.act(sc_, lgt, AF.Sigmoid)
                    em.tt(sel, sc_, rb, ALU.add)
                    s3 = sel.re("p (g e) -> p g e", e=4)
                    for pi, (i0_, i1_) in enumerate(PAIRS):
                        em.tt(p6[:, :, pi], s3[:, :, i0_], s3[:, :, i1_], ALU.add)
                        em.tt(m6[:, :, pi], s3[:, :, i0_], s3[:, :, i1_], ALU.min)
                    em.reduce(gs, p6, ALU.max)
                    em.reduce(sec, m6, ALU.max)
                    em.reduce(gmx, gs, ALU.max)
                    em.ts(gmk, gs, gmx, None, ALU.is_ge)
                    e3 = emk.re("p (g e) -> p g e", e=4)
                    em.tt(e3, s3, T(sec.ap.unsqueeze(2).to_broadcast([128, 8, 4]), sec.buf), ALU.is_ge)
                    em.tt(e3, e3, T(gmk.ap.unsqueeze(2).to_broadcast([128, 8, 4]), gmk.buf), ALU.mult)
                    em.tt(emk, emk, sc_, ALU.mult)
                    em.reduce(gsum, emk, ALU.add)
                    em.recip(gsum, gsum)
                    em.ts(G_all[:, tt, :], emk, gsum, None, ALU.mult)
                    yield

            gens = [lane_gen(li, ps[2 * li:2 * li + 2]) for li in range(NLANE)]
            alive = [True] * NLANE
            for li in range(1, NLANE):
                for _ in range(2 * li):
                    pass
            while any(alive):
                for gi, g in enumerate(gens):
                    if alive[gi]:
                        try:
                            next(g)
                        except StopIteration:
                            alive[gi] = False
            em.barrier()
        em.stack = pstack

    def moe_phase(l, b):
        G_all = G_all2[:, b]
        with ExitStack() as st:
            em.stack = st
            t_lo = 0 if l == 0 else 2
            tok0 = t_lo * 128
            NTOK = TT - tok0
            hT2 = em.sb([128, 8, TT], BF16, "hT2")
            em.dma(hT2[:, :, tok0:], h2D[b, :, :, tok0:])
            yacc = em.sb([128, NTT, D], F32, "yacc")
            em.memset(yacc[:, :, 0:512], 0.0)
            em.memset(yacc[:, :, 512:1024], 0.0, e="pool")
            w1b = [em.sb([128, 8, 512], BF16, "w1b") for _ in range(2)]
            w3b = [em.sb([128, 8, 512], BF16, "w3b") for _ in range(2)]
            w2b_ = [em.sb([128, 4, D], BF16, "w2b_") for _ in range(2)]
            sil = [em.sb([128, 512], BF16, "sil") for _ in range(2)]
            actT = [em.sb([128, 4, 512], BF16, "actT") for _ in range(2)]

            def load_w(e):
                sl = e % 2
                em.dma(w1b[sl], ew1[l, e].re("(k p) n -> p k n", p=128), q="pool")
                em.dma(w3b[sl], ew3[l, e].re("(k p) n -> p k n", p=128), q="pool")
                em.dma(w2b_[sl], ew2[l, e].re("(k p) n -> p k n", p=128), q="pool")

            load_w(0)
            it = 0
            for e in range(32):
                sl = e % 2
                if e + 1 < 32:
                    load_w(e + 1)
                for t0 in range(tok0, TT, 512):
                    w = min(512, TT - t0)
                    aT = actT[it % 2]
                    for f in range(4):
                        pa = ps[(it * 4 + f) % 2]
                        pb_ = ps[2 + (it * 4 + f) % 2]
                        for k in range(8):
                            em.mm(pa[:, 0:w], w1b[sl][:, k, f * 128:(f + 1) * 128], hT2[:, k, t0:t0 + w], start=(k == 0), stop=(k == 7))
                        for k in range(8):
                            em.mm(pb_[:, 0:w], w3b[sl][:, k, f * 128:(f + 1) * 128], hT2[:, k, t0:t0 + w], start=(k == 0), stop=(k == 7))
                        sb_ = sil[f % 2]
                        em.act(sb_[:, 0:w], pa[:, 0:w], AF.Silu)
                        em.tt(aT[:, f, 0:w], sb_[:, 0:w], pb_[:, 0:w], ALU.mult)
                    for sub in range(w // 128):
                        tt = (t0 + sub * 128) // 128
                        for half in range(2):
                            po = ps[4 + (sub * 2 + half) % 4]
                            for f in range(4):
                                em.mm(po, aT[:, f, sub * 128:(sub + 1) * 128], w2b_[sl][:, f, half * 512:(half + 1) * 512],
                                      start=(f == 0), stop=(f == 3))
                            ya = yacc[:, tt, half * 512:(half + 1) * 512]
                            em.stt(ya, po, G_all[:, tt, e:e + 1], ya, ALU.mult, ALU.add)
                    it += 1
            g2r = em.sb([128, 2, D], F32, "g2r")
            for w, row in enumerate((2, b)):
                bcast_row(g2r[:, w, :], modD[l, row, 5 * D:6 * D])
            lg = em.sb([128, D], F32, "lg2")
            lb = em.sb([128, D], F32, "lb2")
            bcast_row(lg, lnv[l, 2])
            bcast_row(lb, lnv[l, 3])
            xt = [em.sb([128, D], F32, "xt2") for _ in range(2)]
            stt_ = em.sb([128, 2, 6], F32, "bst2")
            mv = em.sb([128, 2], F32, "mv2")
            rstd = em.sb([128, 1], F32, "rstd2")
            for tt in range(t_lo, NTT):
                w = 0 if tt < 2 else 1
                xx = xt[tt % 2]
                tsl = slice(tt * 128, (tt + 1) * 128)
                em.dma(xx, x1D[b, tsl, :])
                u_ = yacc[:, tt, :]
                em.tt(u_, u_, g2r[:, w, :], ALU.mult)
                em.stt(u_, xx, ALPHA, u_, ALU.mult, ALU.add)
                for half in range(2):
                    em.bn_stats(stt_[:, half, :], u_[:, half * 512:(half + 1) * 512])
                em.bn_aggr(mv, stt_.re("p a b -> p (a b)"))
                em.ts(rstd, mv[:, 1:2], LN_EPS, None, ALU.add)
                em.act(rstd, rstd, AF.Sqrt)
                em.recip(rstd, rstd)
                em.ts(u_, u_, mv[:, 0:1], rstd, ALU.subtract, ALU.mult)
                em.tt(u_, u_, lg, ALU.mult)
                em.tt(xx, u_, lb, ALU.add)
                if l == L - 1:
                    em.dma(y_out[b, (tt - 2) * 128:(tt - 1) * 128, :], xx)
                else:
                    em.dma(sD[b, tsl, :], xx)
            em.barrier()
        em.stack = pstack

    def moe_sparse(l, stage=9):
        NT = 2 * NTT
        with ExitStack() as st:
            em.stack = st
            Gf = G_all2.re("p b t e -> p (b t) e")
            ones32 = em.sb([128, 128], F32, "ones32")
            em.memset(ones32, 1.0)
            lt32 = em.sb([128, 128], F32, "lt32")
            em.dma(lt32, c_maskA[:, 0, 0:128])
            thr = em.sb([128, 128], F32, "thr")
            em.dma(thr, c_thr)
            kp = em.sb([128, 8], F32, "kp")
            em.dma(kp, c_kp)
            glo = em.sb([128, NT], F32, "glo")
            ghi = em.sb([128, NT], F32, "ghi")
            dli = em.sb([128, NT], I32, "dli")
            dhi_i = em.sb([128, NT], I32, "dhi_i")
            widx = em.sb([128, NBLK], I32, "widx")
            with ExitStack() as st2:
                em.stack = st2
                m = em.sb([128, NT, 32], F32, "m")
                em.ts(m, Gf, 0.0, None, ALU.is_gt)
                if l == 1:
                    for b in range(2):
                        em.memset(m[:, b * NTT:b * NTT + 2, :], 0.0)
                rank = em.sb([128, NT, 32], F32, "rank")
                cnt = em.sb([128, 32], F32, "cnt")
                em.memset(cnt, 0.0)
                for i in range(NT):
                    em.mm(ps[i // 16][:, (i % 16) * 32:(i % 16 + 1) * 32], lt32, m[:, i, :])
                    em.mm(ps[3 + i // 16][:, (i % 16) * 32:(i % 16 + 1) * 32], ones32, m[:, i, :])
                csum = em.sb([128, NT, 32], F32, "csum")
                for bk in range(3):
                    n_ = min(16, NT - bk * 16)
                    em.copy(rank[:, bk * 16:bk * 16 + n_, :], ps[bk][:, 0:n_ * 32].re("p (a b) -> p a b", b=32), e="act")
                    em.copy(csum[:, bk * 16:bk * 16 + n_, :], ps[3 + bk][:, 0:n_ * 32].re("p (a b) -> p a b", b=32), e="dve")
                for i in range(NT):
                    if i > 0:
                        em.tt(rank[:, i, :], rank[:, i, :], cnt, ALU.add)
                    em.tt(cnt, cnt, csum[:, i, :], ALU.add)
                cmp = em.sb([128, 32, 40], F32, "cmp")
                em.tt(cmp, T(cnt.ap.unsqueeze(2).to_broadcast([128, 32, 40]), cnt.buf),
                      T(thr.ap[:, 0:40].unsqueeze(1).to_broadcast([128, 32, 40]), thr.buf), ALU.is_gt)
                nblk = em.sb([128, 32], F32, "nblk")
                em.reduce(nblk, cmp, ALU.add)
                incl = em.sb([128, 32], F32, "incl")
                em.scan(incl, ones32[:, 0:32], nblk, 0.0, ALU.mult, ALU.add)
                pstart = em.sb([128, 32], F32, "pstart")
                pend = em.sb([128, 32], F32, "pend")
                em.tt(pstart, incl, nblk, ALU.subtract)
                em.ts(pstart, pstart, float(BS), None, ALU.mult)
                em.ts(pend, incl, float(BS), None, ALU.mult)
                dest = em.sb([128, NT, 32], F32, "dest")
                em.tt(dest, rank, T(pstart.ap.unsqueeze(1).to_broadcast([128, NT, 32]), pstart.buf), ALU.add)
                tmpm = em.sb([128, NT, 32], F32, "tmpm")
                dlo = em.sb([128, NT], F32, "dlo")
                dhi = em.sb([128, NT], F32, "dhi")
                em.ts(tmpm, m, -1.0e6, 1.0e6, ALU.mult, ALU.add)
                em.tt(tmpm, tmpm, dest, ALU.add)
                em.reduce(dlo, tmpm, ALU.min)
                em.tt(tmpm, dest, m, ALU.mult)
                em.tt(tmpm, tmpm, m, ALU.add)
                em.ts(tmpm, tmpm, -1.0, None, ALU.add)
                em.reduce(dhi, tmpm, ALU.max)
                em.tt(tmpm, dest, T(dlo.ap.unsqueeze(2).to_broadcast([128, NT, 32]), dlo.buf), ALU.is_equal)
                em.tt(tmpm, tmpm, Gf, ALU.mult)
                em.reduce(glo, tmpm, ALU.add)
                em.tt(tmpm, dest, T(dhi.ap.unsqueeze(2).to_broadcast([128, NT, 32]), dhi.buf), ALU.is_equal)
                em.tt(tmpm, tmpm, Gf, ALU.mult)
                em.reduce(ghi, tmpm, ALU.add)
                em.copy(dli, dlo)
                em.copy(dhi_i, dhi)
                cmpb = em.sb([128, NBLK, 32], F32, "cmpb")
                em.tt(cmpb, T(pend.ap.unsqueeze(1).to_broadcast([128, NBLK, 32]), pend.buf),
                      T(thr.ap[:, 0:NBLK].unsqueeze(2).to_broadcast([128, NBLK, 32]), thr.buf), ALU.is_le)
                be = em.sb([128, NBLK], F32, "be")
                em.reduce(be, cmpb, ALU.add)
                em.ts(be, be, 31.0, None, ALU.min)
                em.ts(be, be, 128.0, float(l * 32 * 128), ALU.mult, ALU.add)
                em.tt(be, be, T(kp.ap[:, 0:1].to_broadcast([128, NBLK]), kp.buf), ALU.add)
                em.copy(widx, be)
                xtk = [em.sb([128, D], BF16, "xtk") for _ in range(2)]
                for i in range(NT):
                    if l == 1 and (i % NTT) < 2:
                        continue
                    xk = xtk[i % 2]
                    em.dma(xk, h2tokD[i * 128:(i + 1) * 128, :])
                    em.idma(xbD, xk, dli[:, i:i + 1], True, NBLK * BS - 1)
                    em.idma(xbD, xk, dhi_i[:, i:i + 1], True, NBLK * BS - 1)
                em.barrier()
            em.stack = st
            if stage < 2:
                return
            NS = BS // 128
            with ExitStack() as st3:
                em.stack = st3
                NLN = 2

                def blk_lane(li, B):
                    w1s = [em.sb([128, 8, 512], BF16, "w1s") for _ in range(2)]
                    w3s = [em.sb([128, 8, 512], BF16, "w3s") for _ in range(2)]
                    w2s = [em.sb([128, 4, D], BF16, "w2s") for _ in range(2)]
                    xblk = [em.sb([128, NS, D], BF16, "xblk") for _ in range(2)]
                    xT = em.sb([128, 8, BS], BF16, "xT")
                    sil = em.sb([128, BS], BF16, "sil")
                    aT = em.sb([128, 4, BS], BF16, "aT")
                    ysub = [em.sb([128, D], F32, "ysub") for _ in range(2)]
                    blks = list(range(li, NBLK, NLN))

                    def load_blk(n):
                        blk = blks[n]
                        sl = n % 2
                        em.idma(w1s[sl].re("p k n -> p (k n)"), ew1, widx[:, blk:blk + 1], False, 0)
                        em.idma(w3s[sl].re("p k n -> p (k n)"), ew3, widx[:, blk:blk + 1], False, 0)
                        em.idma(w2s[sl].re("p k n -> p (k n)"), ew2, widx[:, blk:blk + 1], False, 0)
                        em.dma(xblk[sl], xbD[blk * BS:(blk + 1) * BS, :].re("(s p) n -> p s n", p=128))

                    load_blk(0)
                    yc = 0
                    for n, blk in enumerate(blks):
                        sl = n % 2
                        if n + 1 < len(blks):
                            load_blk(n + 1)
                        w1b, w3b, w2b_ = w1s[sl], w3s[sl], w2s[sl]
                        for s_ in range(NS):
                            ptb = B[s_ % 2].cast(BF16)
                            for k in range(8):
                                em.tr(ptb[:, k * 128:(k + 1) * 128], xblk[sl][:, s_, k * 128:(k + 1) * 128], identb)
                            em.copy(xT[:, :, s_ * 128:(s_ + 1) * 128], ptb.re("p (a b) -> p a b", a=8), e=("dve" if s_ % 2 else "act"))
                            if s_ % 2:
                                yield
                        for f in range(4):
                            pa = B[2]
                            pb_ = B[3]
                            for k in range(8):
                                em.mm(pa, w1b[:, k, f * 128:(f + 1) * 128], xT[:, k, :], start=(k == 0), stop=(k == 7))
                            for k in range(8):
                                em.mm(pb_, w3b[:, k, f * 128:(f + 1) * 128], xT[:, k, :], start=(k == 0), stop=(k == 7))
                            yield
                            em.act(sil, pa, AF.Silu)
                            em.tt(aT[:, f, :], sil, pb_, ALU.mult)
                        for s_ in range(NS):
                            yb_ = ysub[yc % 2]
                            yc += 1
                            for half in range(2):
                                po = B[half]
                                for f in range(4):
                                    em.mm(po, aT[:, f, s_ * 128:(s_ + 1) * 128], w2b_[:, f, half * 512:(half + 1) * 512],
                                          start=(f == 0), stop=(f == 3))
                            yield
                            for half in range(2):
                                em.copy(yb_[:, half * 512:(half + 1) * 512], B[half], e=("act" if half else "dve"))
                            em.dma(ybD[blk * BS + s_ * 128:blk * BS + (s_ + 1) * 128, :], yb_)

                gens = [blk_lane(li, ps[4 * li:4 * li + 4]) for li in range(NLN)]
                alive = [True] * NLN
                while any(alive):
                    for gi, g in enumerate(gens):
                        if alive[gi]:
                            try:
                                next(g)
                            except StopIteration:
                                alive[gi] = False
                em.barrier()
            em.stack = st
            if stage < 3:
                return
            with ExitStack() as st4:
                em.stack = st4
                g2r = em.sb([128, 3, D], F32, "g2r")
                for w, row in enumerate((2, 0, 1)):
                    bcast_row(g2r[:, w, :], modD[l, row, 5 * D:6 * D])
                lg = em.sb([128, D], F32, "lg2")
                lb = em.sb([128, D], F32, "lb2")
                bcast_row(lg, lnv[l, 2])
                bcast_row(lb, lnv[l, 3])
                xt = [em.sb([128, D], F32, "xt2") for _ in range(2)]
                yl = [em.sb([128, D], F32, "yl") for _ in range(2)]
                yh = [em.sb([128, D], F32, "yh") for _ in range(2)]
                stt_ = em.sb([128, 2, 6], F32, "bst2")
                mv = em.sb([128, 2], F32, "mv2")
                rstd = em.sb([128, 1], F32, "rstd2")
                for i in range(NT):
                    b, tt = i // NTT, i % NTT
                    if l == 1 and tt < 2:
                        continue
                    w = 0 if tt < 2 else 1 + b
                    xx = xt[i % 2]
                    ylo_ = yl[i % 2]
                    yhi_ = yh[i % 2]
                    tsl = slice(tt * 128, (tt + 1) * 128)
                    em.dma(xx, x1D[b, tsl, :])
                    em.idma(ylo_, ybD, dli[:, i:i + 1], False, 0)
                    em.idma(yhi_, ybD, dhi_i[:, i:i + 1], False, 0)
                    u_ = ylo_
                    em.ts(ylo_, ylo_, glo[:, i:i + 1], None, ALU.mult)
                    em.stt(u_, yhi_, ghi[:, i:i + 1], ylo_, ALU.mult, ALU.add)
                    em.tt(u_, u_, g2r[:, w, :], ALU.mult)
                    em.stt(u_, xx, ALPHA, u_, ALU.mult, ALU.add)
                    for half in range(2):
                        em.bn_stats(stt_[:, half, :], u_[:, half * 512:(half + 1) * 512])
                    em.bn_aggr(mv, stt_.re("p a b -> p (a b)"))
                    em.ts(rstd, mv[:, 1:2], LN_EPS, None, ALU.add)
                    em.act(rstd, rstd, AF.Sqrt)
                    em.recip(rstd, rstd)
                    em.ts(u_, u_, mv[:, 0:1], rstd, ALU.subtract, ALU.mult)
                    em.tt(u_, u_, lg, ALU.mult)
                    em.tt(xx, u_, lb, ALU.add)
                    if l == L - 1:
                        em.dma(y_out[b, (tt - 2) * 128:(tt - 1) * 128, :], xx)
                    else:
                        em.dma(sD[b, tsl, :], xx)
                em.barrier()
        em.stack = pstack

    for l in range(nlayers):
        if want("adaln"):
            adaln(l)
        if want("na"):
            na_phase(l)
        if want("rwkv"):
            rwkv_phase(l)
        if SPARSE_MOE:
            for b in range(2):
                if want("post"):
                    post_phase(l, b)
            if want("moe"):
                moe_sparse(l, moe_stage)
        else:
            for b in range(2):
                if want("post"):
                    post_phase(l, b)
                if want("moe"):
                    moe_phase(l, b)
    em.barrier()
    return nc, em


_CONSTS = None


def make_in_maps(inp, ncores=8):
    global _CONSTS
    if _CONSTS is None:
        _CONSTS = _consts_np()
    f = lambda a: np.ascontiguousarray(np.asarray(a, dtype=np.float32))
    shared = {k: f(inp[k]) for k in ("ada_w", "ada_b", "w_in", "rw_w2", "rw_a2", "rw_g2", "w_out", "router_w",
                                     "router_bias")}
    for nm, kc in (("exp_w1", 8), ("exp_w3", 8), ("exp_w2", 4)):
        w = f(inp[nm])
        n = w.shape[-1]
        shared[nm] = np.ascontiguousarray(w.reshape(L, 32, kc, 128, n).transpose(0, 1, 3, 2, 4)).reshape(L * 32 * 128, kc * n)
    shared["vecs"] = _vecs_np(inp)
    shared["gnrow"] = f(np.stack([inp["rw_gn_g"], inp["rw_gn_b"]], 1))
    shared["lnv"] = f(np.stack([inp["ln1_g"], inp["ln1_b"], inp["ln2_g"], inp["ln2_b"]], 1))
    shared["rb"] = _rb_gather(f(inp["na_rpb"]))
    for k, v in _CONSTS.items():
        shared[k] = v
    maps = []
    x = f(inp["x"])
    ctx = f(inp["ctx"])
    c = f(inp["c"])
    cc = f(inp["c_ctx"])
    for i in range(ncores):
        m = dict(shared)
        m["x"] = np.ascontiguousarray(x[2 * i:2 * i + 2])
        m["ctx"] = np.ascontiguousarray(ctx[2 * i:2 * i + 2])
        rows = np.stack([c[2 * i], c[2 * i + 1], cc], 0)
        m["cT"] = np.ascontiguousarray(rows.reshape(3, 8, 128).transpose(2, 1, 0))
        maps.append(m)
    return maps


def kernel(**inputs):
    nc, em = build()
    maps = make_in_maps(inputs, 8)
    res = run_bass_kernel_spmd(nc, maps, core_ids=list(range(8)))
    out = np.concatenate([np.asarray(r["y"], dtype=np.float32) for r in res.results], axis=0)
    return out
```
